# Optimizing a Trainium2 kernel written in Bass

```python
import math
import jax
import jax.numpy as jnp
from jax import lax
import numpy as np

D_MODEL = 1024
BATCH = 16
SEQ = 2048
DEPTH = 4

N_MIXERS = 2
N_A = (DEPTH + 1) // 2
N_B = DEPTH // 2

GDN_DK = 128
GDN_DV = 128
GDN_HK = D_MODEL // 128
GDN_HV = 2 * GDN_HK
QK_DIM = GDN_HK * GDN_DK
V_DIM = GDN_HV * GDN_DV
CONV_W = 5
CONV_CH = 2 * QK_DIM + V_DIM
A_IN_DIM = CONV_CH + V_DIM + 4 * GDN_HV
CHUNK = 64

ATT_DH = 64
ATT_HQ = D_MODEL // ATT_DH
ATT_HKV = 4
ATT_G = ATT_HQ // ATT_HKV
Q_DIM = ATT_HQ * ATT_DH
KV_DIM = ATT_HKV * ATT_DH
B_IN_DIM = Q_DIM + 2 * KV_DIM
WINDOW = 128
WB = 128

N_EXPERTS = 32
TOP_K = 4
D_FF = D_MODEL
SWIGLU_LIMIT = 7.0
SWIGLU_ALPHA = 1.702
MOE_BLOCK = 256

ALPHA_DN = (2 * DEPTH) ** 0.25
BETA_DN = (8 * DEPTH) ** -0.25
LN_EPS = 1e-5
RMS_EPS = 1e-6

kernel_name = 'hybrid_gdn_swa_moe_deepnorm'


def layer_norm(x, g, b):
    xf = x.astype(jnp.float32)
    mu = jnp.mean(xf, axis=-1, keepdims=True)
    xc = xf - mu
    var = jnp.mean(xc * xc, axis=-1, keepdims=True)
    y = xc * lax.rsqrt(var + LN_EPS) * g.astype(jnp.float32) + b.astype(jnp.float32)
    return y.astype(x.dtype)


def l2_normalize(x):
    return x * lax.rsqrt(jnp.sum(x * x, axis=-1, keepdims=True) + RMS_EPS)


def short_conv(u, w):
    c = u.shape[-1]
    return lax.conv_general_dilated(
        u, w.astype(u.dtype)[:, None, :], window_strides=(1,),
        padding=[(CONV_W // 2, CONV_W // 2)],
        dimension_numbers=('NWC', 'WIO', 'NWC'), feature_group_count=c)


def gated_delta_rule(q, k, v, g, beta):
    bsz, h, t, _ = k.shape
    n = t // CHUNK
    q = q.reshape(bsz, h, n, CHUNK, GDN_DK)
    k = k.reshape(bsz, h, n, CHUNK, GDN_DK)
    v = v.reshape(bsz, h, n, CHUNK, GDN_DV)
    g = g.reshape(bsz, h, n, CHUNK)
    beta = beta.reshape(bsz, h, n, CHUNK)
    gc = jnp.cumsum(g, axis=-1)
    lower = jnp.tril(jnp.ones((CHUNK, CHUNK), dtype=bool))
    decay = jnp.exp(jnp.where(lower, gc[..., :, None] - gc[..., None, :], -jnp.inf))
    eye = jnp.eye(CHUNK, dtype=k.dtype)
    kb = k * beta[..., None]
    strict = jnp.einsum('bhnid,bhnjd->bhnij', kb, k) * decay * (1.0 - eye)
    lhs = eye + strict
    rhs = jnp.concatenate([v * beta[..., None], kb * jnp.exp(gc)[..., None]], axis=-1)
    sol = lax.linalg.triangular_solve(lhs, rhs, left_side=True, lower=True, unit_diagonal=True)
    u, w = sol[..., :GDN_DV], sol[..., GDN_DV:]
    qk = jnp.einsum('bhnid,bhnjd->bhnij', q, k) * decay
    qg = q * jnp.exp(gc)[..., None]
    kd = k * jnp.exp(gc[..., -1:] - gc)[..., None]
    gl = jnp.exp(gc[..., -1])
    xs = (jnp.moveaxis(u, 2, 0), jnp.moveaxis(w, 2, 0), jnp.moveaxis(qk, 2, 0),
          jnp.moveaxis(qg, 2, 0), jnp.moveaxis(kd, 2, 0), jnp.moveaxis(gl, 2, 0))

    def step(state, inp):
        u_c, w_c, qk_c, qg_c, kd_c, gl_c = inp
        v_new = u_c - jnp.einsum('bhcd,bhde->bhce', w_c, state)
        o_c = jnp.einsum('bhcd,bhde->bhce', qg_c, state) + jnp.einsum('bhcs,bhse->bhce', qk_c, v_new)
        state = state * gl_c[..., None, None] + jnp.einsum('bhcd,bhce->bhde', kd_c, v_new)
        return state, o_c

    s0 = jnp.zeros((bsz, h, GDN_DK, GDN_DV), k.dtype)
    _, o = lax.scan(step, s0, xs)
    return jnp.moveaxis(o, 0, 2).reshape(bsz, h, t, GDN_DV)


def deltanet_mixer(x, w_in, conv_w, a_log, dt_bias, norm_w, w_out):
    f32 = jnp.float32
    bsz, s, _ = x.shape
    hcat = x @ w_in
    qkv = jax.nn.silu(short_conv(hcat[..., :CONV_CH], conv_w))
    z = hcat[..., CONV_CH:CONV_CH + V_DIM].reshape(bsz, s, GDN_HV, GDN_DV)
    gates = hcat[..., CONV_CH + V_DIM:].astype(f32).reshape(bsz, s, 2, 2, GDN_HV)
    q = qkv[..., :QK_DIM].astype(f32).reshape(bsz, s, GDN_HK, GDN_DK)
    k = qkv[..., QK_DIM:2 * QK_DIM].astype(f32).reshape(bsz, s, GDN_HK, GDN_DK)
    v = qkv[..., 2 * QK_DIM:].astype(f32).reshape(bsz, s, GDN_HV, GDN_DV)
    q = l2_normalize(q) * (GDN_DK ** -0.5)
    k = l2_normalize(k)
    rep = GDN_HV // GDN_HK
    q = jnp.transpose(jnp.repeat(q, rep, axis=2), (0, 2, 1, 3))
    k = jnp.transpose(jnp.repeat(k, rep, axis=2), (0, 2, 1, 3))
    v = jnp.transpose(v, (0, 2, 1, 3))
    beta = jax.nn.sigmoid(gates[:, :, :, 0])
    g = -jnp.exp(a_log.astype(f32)) * jax.nn.softplus(gates[:, :, :, 1] + dt_bias.astype(f32))
    beta = jnp.transpose(beta, (2, 0, 3, 1))
    g = jnp.transpose(g, (2, 0, 3, 1))
    o_fwd = gated_delta_rule(q, k, v, g[0], beta[0])
    flip = lambda a: jnp.flip(a, axis=2)
    o_bwd = flip(gated_delta_rule(flip(q), flip(k), flip(v), flip(g[1]), flip(beta[1])))
    o = jnp.transpose(o_fwd + o_bwd, (0, 2, 1, 3))
    o = o * lax.rsqrt(jnp.mean(o * o, axis=-1, keepdims=True) + RMS_EPS) * norm_w.astype(f32)
    o = o * jax.nn.silu(z.astype(f32))
    return o.astype(x.dtype).reshape(bsz, s, V_DIM) @ w_out


def alibi_slopes():
    return 2.0 ** (-8.0 * jnp.arange(1, ATT_HQ + 1, dtype=jnp.float32) / ATT_HQ)


def window_attention_mixer(x, w_in, b_in, sinks, w_out, b_out):
    f32 = jnp.float32
    bsz, s, _ = x.shape
    nb = s // WB
    hcat = x @ w_in + b_in
    q = hcat[..., :Q_DIM].reshape(bsz, nb, WB, ATT_HKV, ATT_G, ATT_DH)
    k = hcat[..., Q_DIM:Q_DIM + KV_DIM].reshape(bsz, s, ATT_HKV, ATT_DH)
    v = hcat[..., Q_DIM + KV_DIM:].reshape(bsz, s, ATT_HKV, ATT_DH)
    pad = ((0, 0), (WB, WB), (0, 0), (0, 0))
    kp, vp = jnp.pad(k, pad), jnp.pad(v, pad)
    idx = jnp.arange(nb)[:, None] * WB + jnp.arange(3 * WB)[None, :]
    kw = jnp.moveaxis(kp[:, idx], 1, 0)
    vw = jnp.moveaxis(vp[:, idx], 1, 0)
    qb = jnp.moveaxis(q, 1, 0)
    slopes = alibi_slopes().reshape(ATT_HKV, ATT_G)
    sink = sinks.astype(f32).reshape(ATT_HKV, ATT_G)[:, :, None]
    qi = jnp.arange(WB)
    sj = jnp.arange(3 * WB)
    dist = jnp.abs(qi[:, None] + WB - sj[None, :])
    bias = -slopes[:, :, None, None] * dist.astype(f32)
    scale = ATT_DH ** -0.5

    def block(args):
        qj, kj, vj, j = args
        key_pos = j * WB - WB + sj
        valid = (dist <= WINDOW) & ((key_pos >= 0) & (key_pos < s))[None, :]
        sc = jnp.einsum('bqhgd,bshd->bhgqs', qj, kj).astype(f32) * scale + bias
        sc = jnp.where(valid, sc, -jnp.inf)
        m = jnp.maximum(jnp.max(sc, axis=-1), sink)
        p = jnp.exp(sc - m[..., None])
        denom = jnp.sum(p, axis=-1) + jnp.exp(sink - m)
        o = jnp.einsum('bhgqs,bshd->bqhgd', p, vj.astype(f32))
        o = o / jnp.transpose(denom, (0, 3, 1, 2))[..., None]
        return o.astype(x.dtype)

    o = lax.map(block, (qb, kw, vw, jnp.arange(nb)))
    o = jnp.moveaxis(o, 0, 1).reshape(bsz, s, Q_DIM)
    return o @ w_out + b_out


def moe_ffn(x, router_w, router_b, w_up, b_up, w_down, b_down):
    bsz, s, d = x.shape
    n_tok = bsz * s
    n_rows = n_tok * TOP_K
    xf = x.reshape(n_tok, d)
    logits = (xf @ router_w + router_b).astype(jnp.float32)
    top_v, top_e = lax.top_k(logits, TOP_K)
    gates = jax.nn.softmax(top_v, axis=-1)
    e_flat = top_e.reshape(-1)
    g_flat = gates.reshape(-1)
    t_flat = jnp.arange(n_rows, dtype=jnp.int32) // TOP_K
    order = jnp.argsort(e_flat)
    e_sorted = e_flat[order]
    counts = jnp.bincount(e_flat, length=N_EXPERTS)
    padded = (counts + MOE_BLOCK - 1) // MOE_BLOCK * MOE_BLOCK
    starts = jnp.cumsum(counts) - counts
    pends = jnp.cumsum(padded)
    pstarts = pends - padded
    dest = pstarts[e_sorted] + (jnp.arange(n_rows) - starts[e_sorted])
    n_blocks = (n_rows + MOE_BLOCK - 1) // MOE_BLOCK + N_EXPERTS
    n_pad = n_blocks * MOE_BLOCK
    row_tok = jnp.full((n_pad,), n_tok, dtype=jnp.int32).at[dest].set(t_flat[order])
    row_gate = jnp.zeros((n_pad,), jnp.float32).at[dest].set(g_flat[order])
    blk_e = jnp.minimum(jnp.searchsorted(pends, jnp.arange(n_blocks) * MOE_BLOCK, side='right'),
                        N_EXPERTS - 1)
    x_pad = jnp.concatenate([xf, jnp.zeros((1, d), xf.dtype)], axis=0)

    def expert_block(args):
        tok, e = args
        hb = x_pad[tok] @ w_up[e] + b_up[e]
        gate = jnp.minimum(hb[:, :D_FF], SWIGLU_LIMIT)
        up = jnp.clip(hb[:, D_FF:], -SWIGLU_LIMIT, SWIGLU_LIMIT)
        act = gate * jax.nn.sigmoid(SWIGLU_ALPHA * gate) * (up + 1.0)
        return act @ w_down[e] + b_down[e]

    y = lax.map(expert_block, (row_tok.reshape(n_blocks, MOE_BLOCK), blk_e))
    y = y.reshape(n_pad, d).astype(jnp.float32) * row_gate[:, None]
    out = jax.ops.segment_sum(y, row_tok, num_segments=n_tok + 1)[:n_tok]
    return out.astype(x.dtype).reshape(bsz, s, d)


def setup_inputs(seed: int = 0) -> dict:
    key = jax.random.key(seed)
    ks = jax.random.split(key, 20)
    f32 = jnp.float32

    def nrm(k, shape, scale):
        return jax.random.normal(k, shape, f32) * scale

    x = nrm(ks[0], (BATCH, SEQ, D_MODEL), 1.0)
    a_w_in = nrm(ks[1], (N_A, D_MODEL, A_IN_DIM), D_MODEL ** -0.5)
    a_conv_w = nrm(ks[2], (N_A, CONV_W, CONV_CH), CONV_W ** -0.5)
    a_A_log = jnp.log(jax.random.uniform(ks[3], (N_A, 2, GDN_HV), f32, 1.0, 16.0))
    dt = jnp.exp(jax.random.uniform(ks[4], (N_A, 2, GDN_HV), f32, math.log(1e-3), math.log(1e-1)))
    a_dt_bias = dt + jnp.log(-jnp.expm1(-dt))
    a_norm_w = 1.0 + nrm(ks[5], (N_A, GDN_DV), 0.02)
    a_w_out = nrm(ks[6], (N_A, V_DIM, D_MODEL), (V_DIM ** -0.5) * BETA_DN)
    b_w_in = nrm(ks[7], (N_B, D_MODEL, B_IN_DIM), D_MODEL ** -0.5)
    b_b_in = nrm(ks[8], (N_B, B_IN_DIM), 0.02)
    b_sinks = nrm(ks[9], (N_B, ATT_HQ), 1.0)
    b_w_out = nrm(ks[10], (N_B, Q_DIM, D_MODEL), (Q_DIM ** -0.5) * BETA_DN)
    b_b_out = nrm(ks[11], (N_B, D_MODEL), 0.02)
    router_w = nrm(ks[12], (DEPTH, D_MODEL, N_EXPERTS), D_MODEL ** -0.5)
    router_b = nrm(ks[13], (DEPTH, N_EXPERTS), 0.01)
    exp_w_up = nrm(ks[14], (DEPTH, N_EXPERTS, D_MODEL, 2 * D_FF), D_MODEL ** -0.5)
    exp_b_up = nrm(ks[15], (DEPTH, N_EXPERTS, 2 * D_FF), 0.02)
    exp_w_down = nrm(ks[16], (DEPTH, N_EXPERTS, D_FF, D_MODEL), (D_FF ** -0.5) * BETA_DN)
    exp_b_down = nrm(ks[17], (DEPTH, N_EXPERTS, D_MODEL), 0.02)
    ln_g = 1.0 + nrm(ks[18], (DEPTH, 2, D_MODEL), 0.02)
    ln_b = nrm(ks[19], (DEPTH, 2, D_MODEL), 0.02)
    return {'x': x, 'a_w_in': a_w_in, 'a_conv_w': a_conv_w, 'a_A_log': a_A_log,
            'a_dt_bias': a_dt_bias, 'a_norm_w': a_norm_w, 'a_w_out': a_w_out,
            'b_w_in': b_w_in, 'b_b_in': b_b_in, 'b_sinks': b_sinks, 'b_w_out': b_w_out,
            'b_b_out': b_b_out, 'router_w': router_w, 'router_b': router_b,
            'exp_w_up': exp_w_up, 'exp_b_up': exp_b_up, 'exp_w_down': exp_w_down,
            'exp_b_down': exp_b_down, 'ln_g': ln_g, 'ln_b': ln_b}


def reference(x, a_w_in, a_conv_w, a_A_log, a_dt_bias, a_norm_w, a_w_out,
              b_w_in, b_b_in, b_sinks, b_w_out, b_b_out,
              router_w, router_b, exp_w_up, exp_b_up, exp_w_down, exp_b_down,
              ln_g, ln_b):
    for i in range(DEPTH):
        j = i // N_MIXERS
        if i % N_MIXERS == 0:
            h = deltanet_mixer(x, a_w_in[j], a_conv_w[j], a_A_log[j], a_dt_bias[j],
                               a_norm_w[j], a_w_out[j])
        else:
            h = window_attention_mixer(x, b_w_in[j], b_b_in[j], b_sinks[j], b_w_out[j], b_b_out[j])
        x = layer_norm(ALPHA_DN * x + h, ln_g[i, 0], ln_b[i, 0])
        h = moe_ffn(x, router_w[i], router_b[i], exp_w_up[i], exp_b_up[i],
                    exp_w_down[i], exp_b_down[i])
        x = layer_norm(ALPHA_DN * x + h, ln_g[i, 1], ln_b[i, 1])
    return x
```

```python
from contextlib import ExitStack
import numpy as np
import concourse.bass as bass
import concourse.mybir as mybir
from concourse.bass_utils import run_bass_kernel_spmd

F32 = mybir.dt.float32
BF16 = mybir.dt.bfloat16
U32 = mybir.dt.uint32
I32 = mybir.dt.int32
AF = mybir.ActivationFunctionType
ALU = mybir.AluOpType
AX = mybir.AxisListType

DMA_RING = 8


class Prog:
    def __init__(self, nc):
        self.nc = nc
        self.ops = []
        self.last_w = {}
        self.readers = {}

    def add(self, eng, fn, reads=(), writes=(), dma=False):
        idx = len(self.ops)
        deps = set()
        for k in reads:
            w = self.last_w.get(k)
            if w is not None:
                deps.add(w)
        for k in writes:
            w = self.last_w.get(k)
            if w is not None:
                deps.add(w)
            rs = self.readers.get(k)
            if rs:
                deps.update(rs)
        for k in reads:
            self.readers.setdefault(k, []).append(idx)
        for k in writes:
            self.last_w[k] = idx
            self.readers[k] = []
        self.ops.append((eng, fn, deps, dma))
        return idx

    def pe(self, fn, reads=(), writes=()):
        return self.add('pe', fn, reads, writes)

    def dve(self, fn, reads=(), writes=()):
        return self.add('dve', fn, reads, writes)

    def act(self, fn, reads=(), writes=()):
        return self.add('act', fn, reads, writes)

    def pool(self, fn, reads=(), writes=()):
        return self.add('pool', fn, reads, writes)

    def dma(self, fn, reads=(), writes=(), q='sp'):
        return self.add(q, fn, reads, writes, dma=True)

    def emit(self, stack):
        nc = self.nc
        ops = self.ops
        n = len(ops)
        needed = [False] * n
        for (eng, fn, deps, dma) in ops:
            for d in deps:
                needed[d] = True
        engs = ('pe', 'dve', 'act', 'pool', 'sp')
        esem = {e: stack.enter_context(nc.semaphore('s_' + e)) for e in engs}
        dsem = {e: [stack.enter_context(nc.semaphore('d_%s%d' % (e, i))) for i in range(DMA_RING)]
                for e in ('sp', 'act', 'pool')}
        ecount = {e: 0 for e in engs}
        dcount = {e: 0 for e in dsem}
        event = [None] * n
        extra_wait = [None] * n
        dma_last = {}
        for i, (eng, fn, deps, dma) in enumerate(ops):
            if dma:
                k = dcount[eng]
                dcount[eng] += 1
                slot = k % DMA_RING
                val = 16 * (k // DMA_RING + 1)
                sem = dsem[eng][slot]
                event[i] = (('d', eng, slot), sem, val, 16)
                if k >= DMA_RING:
                    extra_wait[i] = (('d', eng, slot), sem, val - 16)
                dma_last[(eng, slot)] = (('d', eng, slot), sem, val)
                needed[i] = True
            elif needed[i]:
                ecount[eng] += 1
                event[i] = (('e', eng), esem[eng], ecount[eng], 1)
        per_eng = {e: [] for e in engs}
        seen = {e: {} for e in engs}
        for i, (eng, fn, deps, dma) in enumerate(ops):
            waits = []
            cand = []
            if extra_wait[i] is not None:
                cand.append(extra_wait[i])
            for d in sorted(deps):
                deng, _, _, ddma = ops[d]
                if deng == 'pe' and eng == 'pe' and not ddma and not dma:
                    continue
                ev = event[d]
                cand.append((ev[0], ev[1], ev[2]))
            best = {}
            for (sid, sem, val) in cand:
                if seen[eng].get(sid, 0) >= val:
                    continue
                if sid not in best or best[sid][1] < val:
                    best[sid] = (sem, val)
            for sid, (sem, val) in best.items():
                seen[eng][sid] = val
                waits.append((sem, val))
            per_eng[eng].append((fn, waits, event[i] if needed[i] else None))
        final_waits = []
        for (eng, slot), (sid, sem, val) in dma_last.items():
            if seen['sp'].get(sid, 0) < val:
                final_waits.append((sem, val))
        self.stats = {e: len(per_eng[e]) for e in engs}

        with nc.Block() as block:
            def run(e_obj, lst, tail=()):
                for fn, waits, ev in lst:
                    for sem, val in waits:
                        e_obj.wait_ge(sem, val)
                    inst = fn(e_obj)
                    if ev is not None:
                        inst.then_inc(ev[1], ev[3])
                for sem, val in tail:
                    e_obj.wait_ge(sem, val)

            @block.tensor
            def _(e):
                run(e, per_eng['pe'])

            @block.vector
            def _(e):
                run(e, per_eng['dve'])

            @block.scalar
            def _(e):
                run(e, per_eng['act'])

            @block.gpsimd
            def _(e):
                run(e, per_eng['pool'])

            @block.sync
            def _(e):
                run(e, per_eng['sp'], final_waits)


GRAN = 256


class Buf:
    __slots__ = ('ap', 'keys')

    def __init__(self, ap, keys):
        self.ap = ap
        self.keys = keys


def _flat(items):
    out = []
    for k in items:
        if isinstance(k, Buf):
            out.extend(k.keys)
        else:
            out.append(k)
    return out


_ESZ = {F32: 4, BF16: 2, U32: 4, I32: 4}


class Arena:
    def __init__(self, nc, st, nbytes, name="arena"):
        self.t = st.enter_context(nc.sbuf_tensor(name, [128, nbytes // 4], F32))
        self.nbytes = nbytes

    def buf(self, off, shape, dtype, parts=128):
        nel = 1
        for s in shape:
            nel *= s
        nb = nel * _ESZ[dtype]
        assert off % 4 == 0 and off + nb <= self.nbytes, (off, nb, self.nbytes)
        v = self.t[0:parts, off // 4:(off + nb + 3) // 4]
        if dtype != F32:
            v = v.bitcast(dtype)
        if len(shape) == 2:
            v = v.rearrange("p (a b) -> p a b", a=shape[0])
        elif len(shape) == 3:
            v = v.rearrange("p (a b c) -> p a b c", a=shape[0], b=shape[1])
        keys = [('sb', g) for g in range(off // GRAN, (off + nb - 1) // GRAN + 1)]
        return Buf(v, keys)


class Alloc:
    def __init__(self, arena, start, end=None):
        self.arena = arena
        self.off = start
        self.end = end if end is not None else arena.nbytes

    def new(self, shape, dtype, parts=128):
        nel = 1
        for s in shape:
            nel *= s
        nb = nel * _ESZ[dtype]
        b = self.arena.buf(self.off, shape, dtype, parts)
        self.off += (nb + GRAN - 1) // GRAN * GRAN
        assert self.off <= self.end, ("SBUF overflow", self.off, self.end)
        return b


D = 1024
NTOK = 4096
NT = NTOK // 128
SEQ = 2048
NE = 32
CAP = 768
NB = 256
NBLK = CAP // NB
NST = NB // 128
DEPTH = 4
ALPHA = float(8 ** 0.25)
LN_EPS = 1e-5


class Ctx:
    pass


def P_add(P, eng, fn, reads=(), writes=(), dma=False):
    return P.add(eng, fn, _flat(reads), _flat(writes), dma)


def dve(P, fn, r=(), w=()):
    return P.add('dve', fn, _flat(r), _flat(w))


def act(P, fn, r=(), w=()):
    return P.add('act', fn, _flat(r), _flat(w))


def pool(P, fn, r=(), w=()):
    return P.add('pool', fn, _flat(r), _flat(w))


def pe(P, fn, r=(), w=()):
    return P.add('pe', fn, _flat(r), _flat(w))


def dma(P, fn, r=(), w=(), q='sp'):
    return P.add(q, fn, _flat(r), _flat(w), True)


def _bc_reg(C, e):
    if getattr(C, 'bc_reg', None) is None:
        C.bc_reg = e.to_reg(NE * CAP - 1)
    return C.bc_reg


def barrier(P, C, key):
    dve(P, lambda e: e.memset(C.junk1.ap, 0.0), r=[], w=[C.junk1, key])


def setup_consts(P, C, al):
    C.ident_f = al.new([128], F32)
    C.ident_b = al.new([128], BF16)
    C.ones_b = al.new([128], BF16)
    C.su_b = al.new([128], BF16)
    C.ecap = al.new([NE], F32)
    C.capmax = al.new([NE], F32)
    C.junk1 = al.new([8], F32)
    tmp = al.new([128], F32)
    pool(P, lambda e: e.memset(C.ident_f.ap, 1.0), w=[C.ident_f])
    pool(P, lambda e: e.affine_select(out=C.ident_f.ap, in_=C.ident_f.ap, pattern=[[-1, 128]],
                                       compare_op=ALU.is_equal, fill=0.0, base=0, channel_multiplier=1),
         r=[C.ident_f], w=[C.ident_f])
    dve(P, lambda e: e.tensor_copy(out=C.ident_b.ap, in_=C.ident_f.ap), r=[C.ident_f], w=[C.ident_b])
    dve(P, lambda e: e.memset(C.ones_b.ap, 1.0), w=[C.ones_b])
    pool(P, lambda e: e.memset(tmp.ap, 1.0), w=[tmp])
    pool(P, lambda e: e.affine_select(out=tmp.ap, in_=tmp.ap, pattern=[[1, 128]],
                                       compare_op=ALU.is_gt, fill=0.0, base=0, channel_multiplier=-1),
         r=[tmp], w=[tmp])
    dve(P, lambda e: e.tensor_copy(out=C.su_b.ap, in_=tmp.ap), r=[tmp], w=[C.su_b])
    pool(P, lambda e: e.iota(C.ecap.ap, pattern=[[CAP, NE]], base=0, channel_multiplier=0,
                              allow_small_or_imprecise_dtypes=True), w=[C.ecap])
    dve(P, lambda e: e.tensor_scalar(out=C.capmax.ap, in0=C.ecap.ap, scalar1=float(CAP - 1), scalar2=None,
                                      op0=ALU.add), r=[C.ecap], w=[C.capmax])


def ln_tile(P, xa, ha, gbc, bbc, sm):
    st, mv, sd, rstd = sm
    dve(P, lambda e: e.scalar_tensor_tensor(out=xa.ap, in0=xa.ap, scalar=ALPHA, in1=ha.ap,
                                            op0=ALU.mult, op1=ALU.add), r=[xa, ha], w=[xa])
    dve(P, lambda e: e.bn_stats(out=st.ap[:, 0:6], in_=xa.ap[:, 0:512]), r=[xa], w=[st])
    dve(P, lambda e: e.bn_stats(out=st.ap[:, 6:12], in_=xa.ap[:, 512:1024]), r=[xa, st], w=[st])
    dve(P, lambda e: e.bn_aggr(out=mv.ap, in_=st.ap), r=[st], w=[mv])
    dve(P, lambda e: e.tensor_scalar(out=sd.ap, in0=mv.ap[:, 1:2], scalar1=LN_EPS, scalar2=None, op0=ALU.add),
        r=[mv], w=[sd])
    act(P, lambda e: e.activation(out=sd.ap, in_=sd.ap, func=AF.Sqrt), r=[sd], w=[sd])
    dve(P, lambda e: e.reciprocal(out=rstd.ap, in_=sd.ap), r=[sd], w=[rstd])
    dve(P, lambda e: e.tensor_scalar(out=xa.ap, in0=xa.ap, scalar1=mv.ap[:, 0:1], scalar2=rstd.ap[:, 0:1],
                                     op0=ALU.subtract, op1=ALU.mult), r=[xa, mv, rstd], w=[xa])
    pool(P, lambda e: e.tensor_tensor(out=xa.ap, in0=xa.ap, in1=gbc.ap, op=ALU.mult), r=[xa, gbc], w=[xa])
    pool(P, lambda e: e.tensor_tensor(out=xa.ap, in0=xa.ap, in1=bbc.ap, op=ALU.add), r=[xa, bbc], w=[xa])


def moe_stage(P, C, li, Xin, Xmid, Xout, H, W):
    nc = C.nc
    (Xin_ap, Xin_k), (Xmid_ap, Xmid_k), (Xout_ap, Xout_k), (H_ap, H_k) = Xin, Xmid, Xout, H
    XS, Y = C.XS, C.Y
    banks = C.banks
    al = Alloc(C.arena, C.dyn_start)
    destf = al.new([NT, 4], F32)
    desti = al.new([NT, 4], U32)
    gates = al.new([NT, 4], F32)
    g1 = al.new([D], F32)
    b1 = al.new([D], F32)
    g2, b2 = g1, b1
    rw = al.new([8, NE], F32)
    rb = al.new([NE], F32)
    bupT = al.new([16, NE], F32)
    cnt = [al.new([NE], F32), al.new([NE], F32)]
    sm = [(al.new([12], F32), al.new([2], F32), al.new([1], F32), al.new([1], F32)) for _ in range(2)]
    lg = [al.new([NE], F32) for _ in range(2)]
    mx8 = [al.new([8], F32) for _ in range(2)]
    nv1 = [al.new([1], F32) for _ in range(2)]
    e4 = [al.new([4], F32) for _ in range(2)]
    ssum = [al.new([1], F32) for _ in range(2)]
    maskb = [al.new([NE], BF16) for _ in range(2)]
    slot = [al.new([NE], F32) for _ in range(2)]
    oh = [al.new([NE], F32) for _ in range(2)]
    junk = [al.new([NE], F32) for _ in range(2)]
    big0 = al.off
    wu0 = [al.new([2048], BF16) for dc in range(8)]
    off_wu1 = al.off
    wu1 = [al.new([2048], BF16) for dc in range(8)]
    wu = [wu0, wu1]
    wd = [[al.new([1024], BF16) for fc in range(8)] for _ in range(2)]
    bd = [al.new([D], F32) for _ in range(2)]
    xs = [al.new([NST, D], BF16) for _ in range(2)]
    xsT = [[al.new([2, NB], BF16) for _ in range(4)] for _ in range(2)]
    actT = [[al.new([NB], BF16) for fc in range(8)] for _ in range(2)]
    gsb = [al.new([NB], F32) for _ in range(2)]
    sgb = [al.new([NB], F32) for _ in range(2)]
    usb = [al.new([NB], F32) for _ in range(2)]
    gsm = [al.new([NB], F32) for _ in range(2)]
    ysb = [al.new([D], F32) for _ in range(2)]
    a1 = Alloc(C.arena, off_wu1, off_wu1 + 32768)
    xa = [a1.new([D], F32) for _ in range(2)]
    ha = [a1.new([D], F32) for _ in range(2)]
    xT = [a1.new([D], F32) for _ in range(2)]
    xb = [a1.new([D], BF16) for _ in range(2)]
    bup_raw = Alloc(C.arena, off_wu1).new([2048], F32, parts=NE)
    a3 = Alloc(C.arena, big0)
    xa3 = [a3.new([D], F32) for _ in range(2)]
    yk = [[a3.new([D], F32) for k in range(4)] for _ in range(2)]
    acc = [a3.new([D], F32) for _ in range(2)]

    lw = W
    dma(P, lambda e: e.dma_start(out=g1.ap, in_=lw['ln_g'][li, 0:1, :].partition_broadcast(128)), w=[g1])
    dma(P, lambda e: e.dma_start(out=b1.ap, in_=lw['ln_b'][li, 0:1, :].partition_broadcast(128)), w=[b1])
    dma(P, lambda e: e.dma_start(out=rw.ap, in_=lw['router_w'][li].rearrange("(c p) n -> p c n", p=128)), w=[rw])
    dma(P, lambda e: e.dma_start(out=rb.ap, in_=lw['router_b'][li:li + 1, :].partition_broadcast(128)), w=[rb])
    dma(P, lambda e: e.dma_start(out=bup_raw.ap, in_=lw['exp_b_up'][li]), w=[bup_raw])
    for c in range(16):
        bk = banks[2]
        pe(P, lambda e, c=c, bk=bk: e.transpose(bk.ap[:, 0:NE], bup_raw.ap[:, c * 128:(c + 1) * 128],
                                                 C.ident_f.ap[0:NE, 0:NE]), r=[bup_raw, C.ident_f], w=[bk])
        if c < 8:
            dve(P, lambda e, c=c, bk=bk: e.tensor_copy(out=bupT.ap[:, c, :], in_=bk.ap[:, 0:NE]), r=[bk], w=[bupT])
        else:
            dve(P, lambda e, c=c, bk=bk: e.tensor_scalar(out=bupT.ap[:, c, :], in0=bk.ap[:, 0:NE], scalar1=1.0,
                                                          scalar2=None, op0=ALU.add), r=[bk], w=[bupT])
    dve(P, lambda e: e.tensor_copy(out=cnt[0].ap, in_=C.ecap.ap), r=[C.ecap], w=[cnt[0]])

    def load_w(e_):
        ws = e_ % 2
        for dc in range(8):
            dma(P, lambda e, dc=dc: e.dma_start(out=wu[ws][dc].ap, in_=lw['exp_w_up'][li, e_, dc * 128:(dc + 1) * 128, :]),
                w=[wu[ws][dc]], q='pool')
        for fc in range(8):
            dma(P, lambda e, fc=fc: e.dma_start(out=wd[ws][fc].ap, in_=lw['exp_w_down'][li, e_, fc * 128:(fc + 1) * 128, :]),
                w=[wd[ws][fc]], q='pool')
        dma(P, lambda e: e.dma_start(out=bd[ws].ap, in_=lw['exp_b_down'][li, e_:e_ + 1, :].partition_broadcast(128)),
            w=[bd[ws]])

    barrier(P, C, 'K_XS')
    load_w(0)
    def m1_tile(t):
        s = t % 2
        rows = slice(t * 128, (t + 1) * 128)
        dma(P, lambda e, s=s, rows=rows: e.dma_start(out=xa[s].ap, in_=Xin_ap[rows, :]), r=[(Xin_k, t)], w=[xa[s]])
        dma(P, lambda e, s=s, rows=rows: e.dma_start(out=ha[s].ap, in_=H_ap[rows, :]), r=[(H_k, t)], w=[ha[s]])
        ln_tile(P, xa[s], ha[s], g1, b1, sm[s])
        dma(P, lambda e, s=s, rows=rows: e.dma_start(out=Xmid_ap[rows, :], in_=xa[s].ap), r=[xa[s]], w=[(Xmid_k, t)])
        act(P, lambda e, s=s: e.copy(out=xb[s].ap, in_=xa[s].ap), r=[xa[s]], w=[xb[s]])
        bA, bB = (banks[0], banks[1]) if s == 0 else (banks[5], banks[6])
        for c in range(8):
            bk = bA if c < 4 else bB
            pe(P, lambda e, c=c, bk=bk, s=s: e.transpose(bk.ap[:, (c % 4) * 128:(c % 4 + 1) * 128],
                                                       xa[s].ap[:, c * 128:(c + 1) * 128], C.ident_f.ap),
               r=[xa[s], C.ident_f], w=[bk])
        act(P, lambda e, s=s, bk=bA: e.copy(out=xT[s].ap[:, 0:512], in_=bk.ap), r=[bA], w=[xT[s]])
        act(P, lambda e, s=s, bk=bB: e.copy(out=xT[s].ap[:, 512:1024], in_=bk.ap), r=[bB, xT[s]], w=[xT[s]])
        for c in range(8):
            pe(P, lambda e, c=c, s=s: e.matmul(banks[2].ap[:, 0:NE], lhsT=xT[s].ap[:, c * 128:(c + 1) * 128],
                                               rhs=rw.ap[:, c, :], start=(c == 0), stop=(c == 7)),
               r=[xT[s], rw], w=[banks[2]])
        dve(P, lambda e, s=s: e.tensor_tensor(out=lg[s].ap, in0=banks[2].ap[:, 0:NE], in1=rb.ap, op=ALU.add),
            r=[banks[2], rb], w=[lg[s]])
        dve(P, lambda e, s=s: e.max(out=mx8[s].ap, in_=lg[s].ap), r=[lg[s]], w=[mx8[s]])
        dve(P, lambda e, s=s: e.tensor_scalar(out=nv1[s].ap, in0=mx8[s].ap[:, 0:1], scalar1=-1.0, scalar2=None,
                                              op0=ALU.mult), r=[mx8[s]], w=[nv1[s]])
        act(P, lambda e, s=s: e.activation(out=e4[s].ap, in_=mx8[s].ap[:, 0:4], func=AF.Exp, bias=nv1[s].ap[:, 0:1],
                                           scale=1.0, accum_out=ssum[s].ap), r=[mx8[s], nv1[s]], w=[e4[s], ssum[s]])
        dve(P, lambda e, s=s: e.reciprocal(out=ssum[s].ap, in_=ssum[s].ap), r=[ssum[s]], w=[ssum[s]])
        dve(P, lambda e, s=s, t=t: e.tensor_scalar(out=gates.ap[:, t, :], in0=e4[s].ap, scalar1=ssum[s].ap[:, 0:1],
                                                   scalar2=None, op0=ALU.mult), r=[e4[s], ssum[s]], w=[gates])
        dve(P, lambda e, s=s: e.tensor_scalar(out=maskb[s].ap, in0=lg[s].ap, scalar1=mx8[s].ap[:, 3:4], scalar2=None,
                                              op0=ALU.is_ge), r=[lg[s], mx8[s]], w=[maskb[s]])
        pe(P, lambda e, s=s: e.matmul(banks[3].ap[:, 0:NE], lhsT=C.su_b.ap, rhs=maskb[s].ap, start=True, stop=True),
           r=[C.su_b, maskb[s]], w=[banks[3]])
        pe(P, lambda e, s=s: e.matmul(banks[4].ap[:, 0:NE], lhsT=C.ones_b.ap, rhs=maskb[s].ap, start=True, stop=True),
           r=[C.ones_b, maskb[s]], w=[banks[4]])
        cur, nxt = cnt[t % 2], cnt[(t + 1) % 2]
        dve(P, lambda e, s=s, cur=cur: e.tensor_tensor(out=slot[s].ap, in0=banks[3].ap[:, 0:NE], in1=cur.ap, op=ALU.add),
            r=[banks[3], cur], w=[slot[s]])
        dve(P, lambda e, s=s: e.tensor_tensor(out=slot[s].ap, in0=slot[s].ap, in1=C.capmax.ap, op=ALU.min),
            r=[slot[s], C.capmax], w=[slot[s]])
        dve(P, lambda e, cur=cur, nxt=nxt: e.tensor_tensor(out=nxt.ap, in0=banks[4].ap[:, 0:NE], in1=cur.ap, op=ALU.add),
            r=[banks[4], cur], w=[nxt])
        for k in range(4):
            dve(P, lambda e, s=s, k=k: e.tensor_scalar(out=oh[s].ap, in0=lg[s].ap, scalar1=mx8[s].ap[:, k:k + 1],
                                                       scalar2=None, op0=ALU.is_equal), r=[lg[s], mx8[s]], w=[oh[s]])
            dve(P, lambda e, s=s, k=k, t=t: e.scalar_tensor_tensor(out=junk[s].ap, in0=oh[s].ap, scalar=1.0, in1=slot[s].ap,
                                                                   op0=ALU.mult, op1=ALU.mult,
                                                                   accum_out=destf.ap[:, t, k:k + 1]),
                r=[oh[s], slot[s]], w=[junk[s], destf])
        dve(P, lambda e, t=t: e.tensor_copy(out=desti.ap[:, t, :], in_=destf.ap[:, t, :]), r=[destf], w=[desti])
        for k in range(4):
            dma(P, lambda e, s=s, k=k, t=t: e.indirect_dma_start(
                out=XS[:, :], out_offset=bass.IndirectOffsetOnAxis(ap=desti.ap[:, t, k:k + 1], axis=0),
                in_=xb[s].ap, in_offset=None, bounds_check=_bc_reg(C, e), oob_is_err=False),
                r=[xb[s], desti, 'K_XS'], w=[], q='pool')
    for t in range(NT):
        m1_tile(t)
    if getattr(C, 'dbg', None):
        dma(P, lambda e: e.dma_start(out=C.dbg['destf'], in_=destf.ap), r=[destf], w=['dbg1'])
        dma(P, lambda e: e.dma_start(out=C.dbg['gates'], in_=gates.ap), r=[gates], w=['dbg2'])
    barrier(P, C, 'K_XS')
    barrier(P, C, 'K_Y')
    nblk_total = NE * NBLK

    def load_xs(n):
        e_, b_ = divmod(n, NBLK)
        r0 = e_ * CAP + b_ * NB
        bs = n % 2
        dma(P, lambda e: e.dma_start(out=xs[bs].ap, in_=XS[r0:r0 + NB, :].rearrange("(s p) d -> p s d", p=128)),
            r=['K_XS'], w=[xs[bs]])

    load_xs(0)
    def m2_block(e_, b_):
        if True:
            ws = e_ % 2
            n = e_ * NBLK + b_
            bs = n % 2
            if n + 1 < nblk_total:
                load_xs(n + 1)
            for dcp in range(4):
                bk = banks[dcp % 2]
                bkb = bk.ap.bitcast(BF16)
                for j in range(2):
                    dc = dcp * 2 + j
                    for st_ in range(NST):
                        pe(P, lambda e, dc=dc, st_=st_, j=j, bkb=bkb: e.transpose(
                            bkb[:, j * NB + st_ * 128: j * NB + (st_ + 1) * 128],
                            xs[bs].ap[:, st_, dc * 128:(dc + 1) * 128], C.ident_b.ap),
                           r=[xs[bs], C.ident_b], w=[bk])
                act(P, lambda e, dcp=dcp, bkb=bkb: e.copy(out=xsT[bs][dcp].ap.rearrange("p a b -> p (a b)"),
                                                         in_=bkb[:, 0:2 * NB]), r=[bk], w=[xsT[bs][dcp]])
            for fc in range(8):
                pgk = banks[2 + fc % 2]
                puk = banks[4 + fc % 2]
                for dc in range(8):
                    pe(P, lambda e, dc=dc, fc=fc, pgk=pgk: e.matmul(
                        pgk.ap[:, 0:NB], lhsT=wu[ws][dc].ap[:, fc * 128:(fc + 1) * 128],
                        rhs=xsT[bs][dc // 2].ap[:, dc % 2, :], start=(dc == 0), stop=(dc == 7)),
                       r=[wu[ws][dc], xsT[bs][dc // 2]], w=[pgk])
                for dc in range(8):
                    pe(P, lambda e, dc=dc, fc=fc, puk=puk: e.matmul(
                        puk.ap[:, 0:NB], lhsT=wu[ws][dc].ap[:, 1024 + fc * 128:1024 + (fc + 1) * 128],
                        rhs=xsT[bs][dc // 2].ap[:, dc % 2, :], start=(dc == 0), stop=(dc == 7)),
                       r=[wu[ws][dc], xsT[bs][dc // 2]], w=[puk])
                q = fc % 2
                dve(P, lambda e, fc=fc, pgk=pgk, q=q: e.tensor_scalar(
                    out=gsb[q].ap, in0=pgk.ap[:, 0:NB], scalar1=bupT.ap[:, fc, e_:e_ + 1], scalar2=7.0,
                    op0=ALU.add, op1=ALU.min), r=[pgk, bupT], w=[gsb[q]])
                act(P, lambda e, q=q: e.activation(out=sgb[q].ap, in_=gsb[q].ap, func=AF.Sigmoid, scale=1.702),
                    r=[gsb[q]], w=[sgb[q]])
                dve(P, lambda e, fc=fc, puk=puk, q=q: e.tensor_scalar(
                    out=usb[q].ap, in0=puk.ap[:, 0:NB], scalar1=bupT.ap[:, 8 + fc, e_:e_ + 1], scalar2=8.0,
                    op0=ALU.add, op1=ALU.min), r=[puk, bupT], w=[usb[q]])
                pool(P, lambda e, q=q: e.tensor_tensor(out=gsm[q].ap, in0=gsb[q].ap, in1=sgb[q].ap, op=ALU.mult),
                     r=[gsb[q], sgb[q]], w=[gsm[q]])
                dve(P, lambda e, fc=fc, q=q: e.scalar_tensor_tensor(
                    out=actT[bs][fc].ap, in0=usb[q].ap, scalar=-6.0, in1=gsm[q].ap, op0=ALU.max, op1=ALU.mult),
                    r=[usb[q], gsm[q]], w=[actT[bs][fc]])
            for st_ in range(NST):
                ys = (n * NST + st_) % 2
                for half in range(2):
                    pyk = banks[6 + half]
                    for fc in range(8):
                        pe(P, lambda e, fc=fc, half=half, st_=st_, pyk=pyk: e.matmul(
                            pyk.ap, lhsT=actT[bs][fc].ap[:, st_ * 128:(st_ + 1) * 128],
                            rhs=wd[ws][fc].ap[:, half * 512:(half + 1) * 512], start=(fc == 0), stop=(fc == 7)),
                           r=[actT[bs][fc], wd[ws][fc]], w=[pyk])
                    dve(P, lambda e, half=half, pyk=pyk, ys=ys: e.tensor_tensor(
                        out=ysb[ys].ap[:, half * 512:(half + 1) * 512], in0=pyk.ap,
                        in1=bd[ws].ap[:, half * 512:(half + 1) * 512], op=ALU.add),
                        r=[pyk, bd[ws], ysb[ys]], w=[ysb[ys]])
                r0 = e_ * CAP + b_ * NB + st_ * 128
                dma(P, lambda e, r0=r0, ys=ys: e.dma_start(out=Y[r0:r0 + 128, :], in_=ysb[ys].ap),
                    r=[ysb[ys], 'K_Y'], w=[])
    for e_ in range(NE):
        if e_ + 1 < NE:
            load_w(e_ + 1)
        for b_ in range(NBLK):
            m2_block(e_, b_)
    barrier(P, C, 'K_Y')
    dma(P, lambda e: e.dma_start(out=g2.ap, in_=lw['ln_g'][li, 1:2, :].partition_broadcast(128)), w=[g2])
    dma(P, lambda e: e.dma_start(out=b2.ap, in_=lw['ln_b'][li, 1:2, :].partition_broadcast(128)), w=[b2])
    def m3_tile(t):
        s = t % 2
        rows = slice(t * 128, (t + 1) * 128)
        dma(P, lambda e, s=s, rows=rows: e.dma_start(out=xa3[s].ap, in_=Xmid_ap[rows, :]), r=[(Xmid_k, t)], w=[xa3[s]])
        for k in range(4):
            dma(P, lambda e, s=s, k=k, t=t: e.indirect_dma_start(
                out=yk[s][k].ap, out_offset=None, in_=Y[:, :],
                in_offset=bass.IndirectOffsetOnAxis(ap=desti.ap[:, t, k:k + 1], axis=0),
                bounds_check=_bc_reg(C, e), oob_is_err=False), r=['K_Y', desti], w=[yk[s][k]], q='pool')
        dve(P, lambda e, s=s, t=t: e.tensor_scalar(out=acc[s].ap, in0=yk[s][0].ap, scalar1=gates.ap[:, t, 0:1],
                                                   scalar2=None, op0=ALU.mult), r=[yk[s][0], gates], w=[acc[s]])
        for k in range(1, 4):
            dve(P, lambda e, s=s, t=t, k=k: e.scalar_tensor_tensor(
                out=acc[s].ap, in0=yk[s][k].ap, scalar=gates.ap[:, t, k:k + 1], in1=acc[s].ap,
                op0=ALU.mult, op1=ALU.add), r=[yk[s][k], gates, acc[s]], w=[acc[s]])
        ln_tile(P, xa3[s], acc[s], g2, b2, sm[s])
        dma(P, lambda e, s=s, rows=rows: e.dma_start(out=Xout_ap[rows, :], in_=xa3[s].ap), r=[xa3[s]], w=[(Xout_k, t)])
    for t in range(NT):
        m3_tile(t)


def load_xT(P, C, X, sq, xT, xa):
    X_ap, X_k = X
    banks = C.banks
    for tt in range(16):
        t = sq * 16 + tt
        s = tt % 2
        rows = slice(t * 128, (t + 1) * 128)
        dma(P, lambda e, s=s, rows=rows: e.dma_start(out=xa[s].ap, in_=X_ap[rows, :]), r=[(X_k, t)], w=[xa[s]])
        bA, bB = (banks[0], banks[1]) if s == 0 else (banks[2], banks[3])
        for c in range(8):
            bk = bA if c < 4 else bB
            pe(P, lambda e, c=c, bk=bk, s=s: e.transpose(bk.ap[:, (c % 4) * 128:(c % 4 + 1) * 128],
                                                       xa[s].ap[:, c * 128:(c + 1) * 128], C.ident_f.ap),
               r=[xa[s], C.ident_f], w=[bk])
        for c in range(8):
            bk = bA if c < 4 else bB
            eng = act if c % 2 == 0 else dve
            if False:
                act(P, lambda e, c=c, bk=bk, tt=tt: e.copy(out=xT[c].ap[:, tt * 128:(tt + 1) * 128],
                                                        in_=bk.ap[:, (c % 4) * 128:(c % 4 + 1) * 128]),
                    r=[bk], w=[xT[c]])
            else:
                dve(P, lambda e, c=c, bk=bk, tt=tt: e.tensor_copy(out=xT[c].ap[:, tt * 128:(tt + 1) * 128],
                                                               in_=bk.ap[:, (c % 4) * 128:(c % 4 + 1) * 128]),
                    r=[bk], w=[xT[c]])


def row_to_cols(P, C, row, out, n, bank):
    for c in range(n):
        pe(P, lambda e, c=c: e.transpose(bank.ap[:, c:c + 1], row.ap[0:1, c * 128:(c + 1) * 128],
                                         C.ident_f.ap[0:1, 0:1]), r=[row, C.ident_f], w=[bank])
    dve(P, lambda e: e.tensor_copy(out=out.ap[:, 0:n], in_=bank.ap[:, 0:n]), r=[bank], w=[out])


ATT_SLOPES = [float(2.0 ** (-8.0 * (h + 1) / 16)) for h in range(16)]


def attn_stage(P, C, j, X, H, W):
    nc = C.nc
    banks = C.banks
    H_ap, H_k = H
    al = Alloc(C.arena, C.dyn_start)
    xT = [al.new([SEQ], BF16) for _ in range(8)]
    oT2 = xT
    QT2 = [al.new([SEQ], BF16) for _ in range(8)]
    KT2 = [al.new([SEQ], BF16) for _ in range(4)]
    V = [al.new([256], BF16) for _ in range(16)]
    win = [al.new([1536], BF16) for _ in range(8)]
    wk2 = [al.new([4, 128], BF16) for _ in range(8)]
    wo = [al.new([D], BF16) for _ in range(8)]
    bqk = al.new([12], F32)
    bq8 = al.new([8], F32)
    bk2 = al.new([4], F32)
    bv_bc = al.new([256], F32)
    bo_bc = al.new([D], F32)
    sinkbc = al.new([16], F32)
    Dm = al.new([384], F32)
    dtmp = al.new([384], F32)
    xa_off = al.off
    xa = [al.new([D], F32) for _ in range(2)]
    ysb = xa
    _a = Alloc(C.arena, xa_off)
    brow = _a.new([1536], F32, parts=1)
    bkrow2 = _a.new([512], F32, parts=1)
    R = 3
    Sb = [al.new([384], F32) for _ in range(R)]
    Pe = [al.new([384], F32) for _ in range(R)]
    Pn = [al.new([384], BF16) for _ in range(R)]
    PTs = [al.new([384], BF16) for _ in range(R)]
    mr = [al.new([1], F32) for _ in range(R)]
    negm = [al.new([1], F32) for _ in range(R)]
    rsum = [al.new([1], F32) for _ in range(R)]
    es = [al.new([1], F32) for _ in range(R)]
    rr = [al.new([1], F32) for _ in range(R)]

    for dc in range(8):
        dma(P, lambda e, dc=dc: e.dma_start(out=win[dc].ap, in_=W['b_w_in'][j, dc * 128:(dc + 1) * 128, :]),
            w=[win[dc]], q='pool')
        dma(P, lambda e, dc=dc: e.dma_start(out=wo[dc].ap, in_=W['b_w_out'][j, dc * 128:(dc + 1) * 128, :]),
            w=[wo[dc]], q='pool')
    for dc in range(8):
        for half in range(2):
            act(P, lambda e, dc=dc, half=half: e.copy(
                out=wk2[dc].ap[:, :, half * 64:(half + 1) * 64],
                in_=win[dc].ap[:, 1024:1280].rearrange("p (g d) -> p g d", g=4)), r=[win[dc]], w=[wk2[dc]])
    def _nc_dma(e, out, in_):
        with nc.allow_non_contiguous_dma(reason="small per-partition bias load"):
            return e.dma_start(out=out, in_=in_)

    dma(P, lambda e: _nc_dma(e, bqk.ap[:, 0:8], W['b_b_in'][j, 0:1024].rearrange("(c p) -> p c", p=128)), w=[bqk])
    dve(P, lambda e: e.tensor_scalar(out=bq8.ap, in0=bqk.ap[:, 0:8], scalar1=0.125, scalar2=None, op0=ALU.mult),
        r=[bqk], w=[bq8])
    for half in range(2):
        dma(P, lambda e, half=half: _nc_dma(e, bk2.ap[half * 64:(half + 1) * 64, :],
                                            W['b_b_in'][j, 1024:1280].rearrange("(g d) -> d g", d=64)), r=[bk2], w=[bk2])
    dma(P, lambda e: e.dma_start(out=bv_bc.ap, in_=W['b_b_in'][j:j + 1, 1280:1536].partition_broadcast(128)), w=[bv_bc])
    dma(P, lambda e: e.dma_start(out=bo_bc.ap, in_=W['b_b_out'][j:j + 1, :].partition_broadcast(128)), w=[bo_bc])
    dma(P, lambda e: e.dma_start(out=sinkbc.ap, in_=W['b_sinks'][j:j + 1, :].partition_broadcast(128)), w=[sinkbc])
    pool(P, lambda e: e.iota(Dm.ap, pattern=[[-1, 384]], base=128, channel_multiplier=1,
                             allow_small_or_imprecise_dtypes=True), w=[Dm])
    dve(P, lambda e: e.tensor_scalar(out=dtmp.ap, in0=Dm.ap, scalar1=-1.0, scalar2=None, op0=ALU.mult), r=[Dm], w=[dtmp])
    dve(P, lambda e: e.tensor_tensor(out=Dm.ap, in0=Dm.ap, in1=dtmp.ap, op=ALU.max), r=[Dm, dtmp], w=[Dm])
    dve(P, lambda e: e.tensor_scalar(out=dtmp.ap, in0=Dm.ap, scalar1=128.0, scalar2=1.0e6, op0=ALU.is_gt, op1=ALU.mult),
        r=[Dm], w=[dtmp])
    dve(P, lambda e: e.tensor_tensor(out=Dm.ap, in0=Dm.ap, in1=dtmp.ap, op=ALU.add), r=[Dm, dtmp], w=[Dm])

    stop = getattr(C, 'attn_stop', 99)
    if stop <= 1:
        return

    def seq_body(sq):
        load_xT(P, C, X, sq, xT, xa)
        if stop <= 2:
            return
        for hp in range(8):
            for tq in range(4):
                bk = banks[4 + (hp * 4 + tq) % 2]
                for dc in range(8):
                    pe(P, lambda e, dc=dc, hp=hp, tq=tq, bk=bk: e.matmul(
                        bk.ap, lhsT=win[dc].ap[:, hp * 128:(hp + 1) * 128], rhs=xT[dc].ap[:, tq * 512:(tq + 1) * 512],
                        start=(dc == 0), stop=(dc == 7)), r=[win[dc], xT[dc]], w=[bk])
                act(P, lambda e, hp=hp, tq=tq, bk=bk: e.activation(
                    out=QT2[hp].ap[:, tq * 512:(tq + 1) * 512], in_=bk.ap, func=AF.Identity,
                    bias=bq8.ap[:, hp:hp + 1], scale=0.125), r=[bk, bq8], w=[QT2[hp]])
        for g in range(4):
            for tq in range(4):
                bk = banks[4 + (g * 4 + tq) % 2]
                for dc in range(8):
                    pe(P, lambda e, dc=dc, g=g, tq=tq, bk=bk: e.matmul(
                        bk.ap, lhsT=wk2[dc].ap[:, g, :], rhs=xT[dc].ap[:, tq * 512:(tq + 1) * 512],
                        start=(dc == 0), stop=(dc == 7)), r=[wk2[dc], xT[dc]], w=[bk])
                act(P, lambda e, g=g, tq=tq, bk=bk: e.activation(
                    out=KT2[g].ap[:, tq * 512:(tq + 1) * 512], in_=bk.ap, func=AF.Identity,
                    bias=bk2.ap[:, g:g + 1], scale=1.0), r=[bk, bk2], w=[KT2[g]])
        for tt in range(16):
            bk = banks[4 + tt % 2]
            for dc in range(8):
                pe(P, lambda e, dc=dc, tt=tt, bk=bk: e.matmul(
                    bk.ap[:, 0:256], lhsT=xT[dc].ap[:, tt * 128:(tt + 1) * 128], rhs=win[dc].ap[:, 1280:1536],
                    start=(dc == 0), stop=(dc == 7)), r=[win[dc], xT[dc]], w=[bk])
            dve(P, lambda e, tt=tt, bk=bk: e.tensor_tensor(out=V[tt].ap, in0=bk.ap[:, 0:256], in1=bv_bc.ap, op=ALU.add),
                r=[bk, bv_bc], w=[V[tt]])
        if stop <= 3:
            return
        units = [(h, jb) for jb in range(16) for h in range(16)]
        if stop == 4:
            units = units[:16]

        def geom(jb):
            kb0 = max(0, jb - 1)
            kb1 = min(15, jb + 1)
            nkb = kb1 - kb0 + 1
            off = 128 if jb == 0 else 0
            return kb0, nkb, off

        def stage0(u):
            h, jb = units[u]
            kb0, nkb, off = geom(jb)
            nk = nkb * 128
            g, hb, hp = h // 4, (h % 2) * 64, h // 2
            rs = u % R
            bk = banks[0 + u % 2]
            pe(P, lambda e: e.matmul(bk.ap[:, 0:nk], lhsT=QT2[hp].ap[hb:hb + 64, jb * 128:(jb + 1) * 128],
                                     rhs=KT2[g].ap[hb:hb + 64, kb0 * 128:kb0 * 128 + nk], start=True, stop=True),
               r=[QT2[hp], KT2[g]], w=[bk])
            dve(P, lambda e: e.scalar_tensor_tensor(out=Sb[rs].ap[:, 0:nk], in0=Dm.ap[:, off:off + nk],
                                                    scalar=-ATT_SLOPES[h], in1=bk.ap[:, 0:nk],
                                                    op0=ALU.mult, op1=ALU.add), r=[Dm, bk], w=[Sb[rs]])
            dve(P, lambda e: e.tensor_reduce(out=mr[rs].ap, in_=Sb[rs].ap[:, 0:nk], axis=AX.X, op=ALU.max),
                r=[Sb[rs]], w=[mr[rs]])
            dve(P, lambda e: e.tensor_scalar(out=negm[rs].ap, in0=mr[rs].ap, scalar1=sinkbc.ap[:, h:h + 1],
                                             scalar2=-1.0, op0=ALU.max, op1=ALU.mult),
                r=[mr[rs], sinkbc], w=[negm[rs]])
            act(P, lambda e: e.activation(out=Pe[rs].ap[:, 0:nk], in_=Sb[rs].ap[:, 0:nk], func=AF.Exp,
                                          bias=negm[rs].ap[:, 0:1], scale=1.0, accum_out=rsum[rs].ap),
                r=[Sb[rs], negm[rs]], w=[Pe[rs], rsum[rs]])
            act(P, lambda e: e.activation(out=es[rs].ap, in_=sinkbc.ap[:, h:h + 1], func=AF.Exp,
                                          bias=negm[rs].ap[:, 0:1], scale=1.0), r=[sinkbc, negm[rs]], w=[es[rs]])
            dve(P, lambda e: e.tensor_tensor(out=rr[rs].ap, in0=rsum[rs].ap, in1=es[rs].ap, op=ALU.add),
                r=[rsum[rs], es[rs]], w=[rr[rs]])
            dve(P, lambda e: e.reciprocal(out=rr[rs].ap, in_=rr[rs].ap), r=[rr[rs]], w=[rr[rs]])
            dve(P, lambda e: e.tensor_scalar(out=Pn[rs].ap[:, 0:nk], in0=Pe[rs].ap[:, 0:nk], scalar1=rr[rs].ap[:, 0:1],
                                             scalar2=None, op0=ALU.mult), r=[Pe[rs], rr[rs]], w=[Pn[rs]])

        def stage1(u):
            h, jb = units[u]
            kb0, nkb, off = geom(jb)
            nk = nkb * 128
            rs = u % R
            bk = banks[2 + u % 2]
            bkb = bk.ap.bitcast(BF16)
            for kb in range(nkb):
                pe(P, lambda e, kb=kb: e.transpose(bkb[:, kb * 128:(kb + 1) * 128], Pn[rs].ap[:, kb * 128:(kb + 1) * 128],
                                                   C.ident_b.ap), r=[Pn[rs], C.ident_b], w=[bk])
            act(P, lambda e: e.copy(out=PTs[rs].ap[:, 0:nk], in_=bkb[:, 0:nk]), r=[bk], w=[PTs[rs]])

        def stage2(u):
            h, jb = units[u]
            kb0, nkb, off = geom(jb)
            g, hb, hp = h // 4, (h % 2) * 64, h // 2
            rs = u % R
            bk = banks[6 + (u // 2) % 2]
            for kb in range(nkb):
                pe(P, lambda e, kb=kb: e.matmul(bk.ap[hb:hb + 64, 0:128], lhsT=V[kb0 + kb].ap[:, g * 64:(g + 1) * 64],
                                                rhs=PTs[rs].ap[:, kb * 128:(kb + 1) * 128],
                                                start=(kb == 0), stop=(kb == nkb - 1)),
                   r=[V[kb0 + kb], PTs[rs]], w=[(bk.keys[0], hb)])
            act(P, lambda e: e.copy(out=oT2[hp].ap[hb:hb + 64, jb * 128:(jb + 1) * 128], in_=bk.ap[hb:hb + 64, 0:128]),
                r=[(bk.keys[0], hb)], w=[oT2[hp]])

        U = len(units)
        for i in range(U + 2):
            if i < U:
                stage0(i)
            if 0 <= i - 1 < U:
                stage1(i - 1)
            if 0 <= i - 2 < U:
                stage2(i - 2)
        if stop <= 5:
            return
        for tt in range(16):
            t = sq * 16 + tt
            ys = tt % 2
            for half in range(2):
                bk = banks[4 + half]
                for hp in range(8):
                    pe(P, lambda e, hp=hp, half=half, tt=tt, bk=bk: e.matmul(
                        bk.ap, lhsT=oT2[hp].ap[:, tt * 128:(tt + 1) * 128], rhs=wo[hp].ap[:, half * 512:(half + 1) * 512],
                        start=(hp == 0), stop=(hp == 7)), r=[oT2[hp], wo[hp]], w=[bk])
                dve(P, lambda e, half=half, bk=bk, ys=ys: e.tensor_tensor(
                    out=ysb[ys].ap[:, half * 512:(half + 1) * 512], in0=bk.ap, in1=bo_bc.ap[:, half * 512:(half + 1) * 512],
                    op=ALU.add), r=[bk, bo_bc, ysb[ys]], w=[ysb[ys]])
            dma(P, lambda e, t=t, ys=ys: e.dma_start(out=H_ap[t * 128:(t + 1) * 128, :], in_=ysb[ys].ap),
                r=[ysb[ys]], w=[(H_k, t)])

    for sq in range(2):
        seq_body(sq)


def bc_mid(ap2, n):
    return ap2.unsqueeze(2).to_broadcast([ap2.shape[0], ap2.shape[1], n])


def bc_heads(ap2, nh):
    return ap2.unsqueeze(1).to_broadcast([ap2.shape[0], nh, ap2.shape[1]])


def setup_gdn_consts(P, C, al):
    f = lambda: al.new([128], F32)
    C.SC, C.Li, C.Ui = f(), f(), f()
    C.nLi, C.nUi = f(), f()
    C.NM_Ls, C.NM_Us, C.NM_Ui, C.NM_Li = f(), f(), f(), f()
    C.OnesF, C.NegOnesF, C.Half0, C.Half1 = f(), f(), f(), f()
    pool(P, lambda e: e.memset(C.SC.ap, 0.0), w=[C.SC])
    pool(P, lambda e: e.memset(C.SC.ap[0:64, 0:64], 1.0), r=[C.SC], w=[C.SC])
    pool(P, lambda e: e.memset(C.SC.ap[64:128, 64:128], 1.0), r=[C.SC], w=[C.SC])
    pool(P, lambda e: e.memset(C.OnesF.ap, 1.0), w=[C.OnesF])
    pool(P, lambda e: e.memset(C.NegOnesF.ap, -1.0), w=[C.NegOnesF])
    pool(P, lambda e: e.memset(C.Half0.ap, 0.0), w=[C.Half0])
    pool(P, lambda e: e.memset(C.Half0.ap[0:64, :], 1.0), r=[C.Half0], w=[C.Half0])
    pool(P, lambda e: e.memset(C.Half1.ap, 0.0), w=[C.Half1])
    pool(P, lambda e: e.memset(C.Half1.ap[64:128, :], 1.0), r=[C.Half1], w=[C.Half1])

    def sel(dst, pat, cm, op):
        pool(P, lambda e: e.affine_select(out=dst.ap, in_=C.SC.ap, pattern=[[pat, 128]], compare_op=op, fill=0.0,
                                          base=0, channel_multiplier=cm), r=[C.SC], w=[dst])

    def negmask(dst):
        dve(P, lambda e: e.tensor_scalar(out=dst.ap, in0=dst.ap, scalar1=-1.0, scalar2=1.0e4, op0=ALU.add, op1=ALU.mult),
            r=[dst], w=[dst])

    sel(C.Li, -1, 1, ALU.is_ge)
    sel(C.Ui, 1, -1, ALU.is_ge)
    sel(C.NM_Ls, -1, 1, ALU.is_gt)
    sel(C.NM_Us, 1, -1, ALU.is_gt)
    sel(C.NM_Li, -1, 1, ALU.is_ge)
    sel(C.NM_Ui, 1, -1, ALU.is_ge)
    for m in (C.NM_Ls, C.NM_Us, C.NM_Li, C.NM_Ui):
        negmask(m)
    dve(P, lambda e: e.tensor_scalar(out=C.nLi.ap, in0=C.Li.ap, scalar1=-1.0, scalar2=None, op0=ALU.mult), r=[C.Li], w=[C.nLi])
    dve(P, lambda e: e.tensor_scalar(out=C.nUi.ap, in0=C.Ui.ap, scalar1=-1.0, scalar2=None, op0=ALU.mult), r=[C.Ui], w=[C.nUi])


def gdn_stage(P, C, jl, X, H, W):
    nc = C.nc
    banks = C.banks
    X_ap, X_k = X
    H_ap, H_k = H
    OF, ON = C.OF, C.ON
    Win = W['a_w_in']
    al = Alloc(C.arena, C.dyn_start)
    setup_gdn_consts(P, C, al)
    gdn_dyn = al.off
    xT = [al.new([SEQ], BF16) for _ in range(8)]
    graw = al.new([16, 64], F32)
    Gt = al.new([16, 2, 16], F32)
    Bt = al.new([16, 2, 16], F32)
    wg = al.new([8, 64], F32)
    cw = al.new([32, 5], F32)
    negA = al.new([2, 16], F32)
    dtb = al.new([2, 16], F32)
    nw_bc = al.new([128], F32)
    qT = [al.new([SEQ], BF16) for _ in range(2)]
    kT = [al.new([SEQ], BF16) for _ in range(2)]
    ktm = [al.new([2, 128], BF16) for _ in range(16)]
    vtm = [al.new([4, 128], BF16) for _ in range(16)]
    wz = [al.new([512], BF16) for _ in range(8)]
    Sf = [al.new([4, 128], F32) for _ in range(2)]
    Sb = [al.new([4, 128], BF16) for _ in range(2)]
    regR = al.off
    a = Alloc(C.arena, regR)
    xa = [a.new([D], F32) for _ in range(2)]
    xTf = [a.new([D], F32) for _ in range(2)]
    wblk = [[a.new([128], BF16) for dc in range(8)] for _ in range(2)]
    hT = [a.new([SEQ + 4], BF16) for _ in range(2)]
    dg = [[a.new([128], BF16) for jt in range(5)] for _ in range(2)]
    qf = [a.new([512], F32) for _ in range(2)]
    sqb = [a.new([512], F32) for _ in range(2)]
    t1b = [a.new([512], F32) for _ in range(2)]
    ss4 = [a.new([4], F32) for _ in range(2)]
    g1_end = a.off
    a = Alloc(C.arena, regR)

    class S_:
        pass
    sets = []
    for d in range(2):
        s_ = S_()
        s_.XA, s_.XB, s_.XTA, s_.XTB, s_.PTA, s_.PTB = [a.new([4, 128], F32) for _ in range(6)]
        s_.Gb, s_.RG = s_.XB, s_.XTB
        s_.dS = s_.PTB
        s_.dT = a.new([4, 128], F32)
        s_.u = s_.dT
        s_.otmp = a.new([4, 128], F32)
        s_.Tb = a.new([4, 128], BF16)
        s_.vb = a.new([4, 128], BF16)
        s_.kbg = a.new([4, 128], BF16)
        s_.kd = a.new([4, 128], BF16)
        s_.wT = a.new([4, 128], BF16)
        s_.qkT = a.new([4, 128], BF16)
        s_.vn = a.new([4, 128], BF16)
        s_.osb = a.new([4, 128], F32)
        s_.gc = a.new([4], F32)
        s_.tot = a.new([4], F32)
        s_.egc = a.new([4], F32)
        s_.bge = a.new([4], F32)
        s_.ekd = a.new([4], F32)
        s_.nbeta = a.new([4], F32)
        s_.gl = a.new([2, 4], F32)
        s_.bk = banks[4 * d:4 * d + 4]
        sets.append(s_)
    scan_end = a.off
    a = Alloc(C.arena, regR)
    of_t = [a.new([512], F32) for _ in range(2)]
    ob_t = [a.new([512], F32) for _ in range(2)]
    zs = [a.new([512], F32) for _ in range(2)]
    sq5 = [a.new([512], F32) for _ in range(2)]
    ms5 = [a.new([4], F32) for _ in range(2)]
    onb = [a.new([512], BF16) for _ in range(2)]

    jl_ = jl
    dma(P, lambda e: e.dma_start(out=wg.ap, in_=Win[jl_, :, 6144:6208].rearrange("(c p) n -> p c n", p=128)), w=[wg])
    dma(P, lambda e: e.dma_start(out=negA.ap.rearrange("p a b -> p (a b)"),
                                 in_=W['a_A_log'][jl_:jl_ + 1].rearrange("o a b -> o (a b)").partition_broadcast(128)), w=[negA])
    dma(P, lambda e: e.dma_start(out=dtb.ap.rearrange("p a b -> p (a b)"),
                                 in_=W['a_dt_bias'][jl_:jl_ + 1].rearrange("o a b -> o (a b)").partition_broadcast(128)), w=[dtb])
    dma(P, lambda e: e.dma_start(out=nw_bc.ap, in_=W['a_norm_w'][jl_:jl_ + 1, :].partition_broadcast(128)), w=[nw_bc])
    act(P, lambda e: e.activation(out=negA.ap, in_=negA.ap, func=AF.Exp), r=[negA], w=[negA])
    dve(P, lambda e: e.tensor_scalar(out=negA.ap, in0=negA.ap, scalar1=-1.0, scalar2=None, op0=ALU.mult), r=[negA], w=[negA])
    cwrow = Alloc(C.arena, regR).new([4096], F32, parts=5)
    dma(P, lambda e: e.dma_start(out=cwrow.ap, in_=W['a_conv_w'][jl_]), w=[cwrow])
    for blk in range(32):
        pe(P, lambda e, blk=blk: e.transpose(banks[7].ap[:, blk * 5:(blk + 1) * 5], cwrow.ap[0:5, blk * 128:(blk + 1) * 128],
                                             C.ident_f.ap[0:5, 0:5]), r=[cwrow, C.ident_f], w=[banks[7]])
    dve(P, lambda e: e.tensor_copy(out=cw.ap.rearrange("p a b -> p (a b)"), in_=banks[7].ap[:, 0:160]), r=[banks[7]], w=[cw])

    def seq_body(sq):
        for tt in range(16):
            t = sq * 16 + tt
            s = tt % 2
            rows = slice(t * 128, (t + 1) * 128)
            dma(P, lambda e, s=s, rows=rows: e.dma_start(out=xa[s].ap, in_=X_ap[rows, :]), r=[(X_k, t)], w=[xa[s]])
            bA, bB = (banks[0], banks[1]) if s == 0 else (banks[2], banks[3])
            for c in range(8):
                bk = bA if c < 4 else bB
                pe(P, lambda e, c=c, bk=bk, s=s: e.transpose(bk.ap[:, (c % 4) * 128:(c % 4 + 1) * 128],
                                                           xa[s].ap[:, c * 128:(c + 1) * 128], C.ident_f.ap),
                   r=[xa[s], C.ident_f], w=[bk])
            act(P, lambda e, s=s, bk=bA: e.copy(out=xTf[s].ap[:, 0:512], in_=bk.ap), r=[bA, xTf[s]], w=[xTf[s]])
            act(P, lambda e, s=s, bk=bB: e.copy(out=xTf[s].ap[:, 512:1024], in_=bk.ap), r=[bB, xTf[s]], w=[xTf[s]])
            for c in range(8):
                dve(P, lambda e, c=c, s=s, tt=tt: e.tensor_copy(out=xT[c].ap[:, tt * 128:(tt + 1) * 128],
                                                              in_=xTf[s].ap[:, c * 128:(c + 1) * 128]),
                    r=[xTf[s]], w=[xT[c]])
            bg = banks[4 + s]
            for c in range(8):
                pe(P, lambda e, c=c, s=s, bg=bg: e.matmul(bg.ap[:, 0:64], lhsT=xTf[s].ap[:, c * 128:(c + 1) * 128],
                                                        rhs=wg.ap[:, c, :], start=(c == 0), stop=(c == 7)),
                   r=[xTf[s], wg], w=[bg])
            dve(P, lambda e, tt=tt, bg=bg: e.tensor_copy(out=graw.ap[:, tt, :], in_=bg.ap[:, 0:64]), r=[bg], w=[graw])
        g4 = graw.ap.rearrange("p t (d b h) -> p t d b h", d=2, b=2)
        for d in range(2):
            act(P, lambda e, d=d: e.activation(out=Bt.ap[:, :, d, :], in_=g4[:, :, d, 0, :], func=AF.Sigmoid),
                r=[graw], w=[Bt])
            dve(P, lambda e, d=d: e.tensor_tensor(out=Gt.ap[:, :, d, :], in0=g4[:, :, d, 1, :],
                                                  in1=bc_heads(dtb.ap[:, d, :], 16), op=ALU.add), r=[graw, dtb], w=[Gt])
        act(P, lambda e: e.activation(out=Gt.ap, in_=Gt.ap, func=AF.Exp), r=[Gt], w=[Gt])
        act(P, lambda e: e.activation(out=Gt.ap, in_=Gt.ap, func=AF.Ln, bias=1.0, scale=1.0), r=[Gt], w=[Gt])
        for d in range(2):
            dve(P, lambda e, d=d: e.tensor_tensor(out=Gt.ap[:, :, d, :], in0=Gt.ap[:, :, d, :],
                                                  in1=bc_heads(negA.ap[:, d, :], 16), op=ALU.mult), r=[Gt, negA], w=[Gt])
        for hg in range(4):
            hg_body(sq, hg)

    def hg_body(sq, hg):
        blocks = [('q', 0, 2 * hg, 2 * hg * 128), ('q', 1, 2 * hg + 1, (2 * hg + 1) * 128),
                  ('k', 0, 8 + 2 * hg, 1024 + 2 * hg * 128), ('k', 1, 8 + 2 * hg + 1, 1024 + (2 * hg + 1) * 128)]
        for vi in range(4):
            blocks.append(('v', vi, 16 + 4 * hg + vi, 2048 + (4 * hg + vi) * 128))
        for dc in range(8):
            dma(P, lambda e, dc=dc: e.dma_start(out=wz[dc].ap, in_=Win[jl_, dc * 128:(dc + 1) * 128,
                                                                    4096 + hg * 512:4096 + (hg + 1) * 512]),
                w=[wz[dc]], q='pool')
        for hb_ in range(2):
            dve(P, lambda e, hb_=hb_: e.memset(hT[hb_].ap[:, 0:2], 0.0), r=[hT[hb_]], w=[hT[hb_]])
            dve(P, lambda e, hb_=hb_: e.memset(hT[hb_].ap[:, SEQ + 2:SEQ + 4], 0.0), r=[hT[hb_]], w=[hT[hb_]])
        for bi, (kind, li_, blk, col0) in enumerate(blocks):
            ws = bi % 2
            for dc in range(8):
                dma(P, lambda e, dc=dc, ws=ws, col0=col0: e.dma_start(
                    out=wblk[ws][dc].ap, in_=Win[jl_, dc * 128:(dc + 1) * 128, col0:col0 + 128]),
                    w=[wblk[ws][dc]], q='pool')
            hb = hT[ws]
            for tq in range(4):
                bk = banks[4 + tq % 2]
                for dc in range(8):
                    pe(P, lambda e, dc=dc, tq=tq, bk=bk, ws=ws: e.matmul(
                        bk.ap, lhsT=wblk[ws][dc].ap, rhs=xT[dc].ap[:, tq * 512:(tq + 1) * 512],
                        start=(dc == 0), stop=(dc == 7)), r=[wblk[ws][dc], xT[dc]], w=[bk])
                act(P, lambda e, tq=tq, bk=bk, hb=hb: e.copy(out=hb.ap[:, 2 + tq * 512:2 + (tq + 1) * 512], in_=bk.ap),
                    r=[bk, hb], w=[hb])
            for jt in range(5):
                dve(P, lambda e, jt=jt, ws=ws, blk=blk: e.tensor_scalar(
                    out=dg[ws][jt].ap, in0=C.ident_b.ap, scalar1=cw.ap[:, blk, jt:jt + 1], scalar2=None, op0=ALU.mult),
                    r=[C.ident_b, cw], w=[dg[ws][jt]])
            if kind in ('q', 'k'):
                dst = qT[li_] if kind == 'q' else kT[li_]
                ebias = float(-0.5 * np.log(128.0)) if kind == 'q' else 0.0
                for tq in range(4):
                    bk = banks[6 + tq % 2]
                    r_ = tq % 2
                    for jt in range(5):
                        pe(P, lambda e, jt=jt, tq=tq, bk=bk, ws=ws, hb=hb: e.matmul(
                            bk.ap, lhsT=dg[ws][jt].ap, rhs=hb.ap[:, tq * 512 + jt:tq * 512 + jt + 512],
                            start=(jt == 0), stop=(jt == 4)), r=[dg[ws][jt], hb], w=[bk])
                    act(P, lambda e, bk=bk, r_=r_: e.activation(out=qf[r_].ap, in_=bk.ap, func=AF.Silu), r=[bk], w=[qf[r_]])
                    dve(P, lambda e, r_=r_: e.tensor_tensor(out=sqb[r_].ap, in0=qf[r_].ap, in1=qf[r_].ap, op=ALU.mult),
                        r=[qf[r_]], w=[sqb[r_]])
                    bo = banks[0 + tq % 2]
                    pe(P, lambda e, bo=bo, r_=r_: e.matmul(bo.ap, lhsT=C.OnesF.ap, rhs=sqb[r_].ap, start=True, stop=True),
                       r=[C.OnesF, sqb[r_]], w=[bo])
                    act(P, lambda e, bo=bo, r_=r_: e.activation(out=t1b[r_].ap, in_=bo.ap, func=AF.Ln, bias=1.0e-6, scale=1.0),
                        r=[bo], w=[t1b[r_]])
                    act(P, lambda e, r_=r_, ebias=ebias: e.activation(out=t1b[r_].ap, in_=t1b[r_].ap, func=AF.Exp,
                                                                    bias=ebias, scale=-0.5), r=[t1b[r_]], w=[t1b[r_]])
                    dve(P, lambda e, r_=r_, tq=tq, dst=dst: e.tensor_tensor(out=dst.ap[:, tq * 512:(tq + 1) * 512],
                                                                          in0=qf[r_].ap, in1=t1b[r_].ap, op=ALU.mult),
                        r=[qf[r_], t1b[r_]], w=[dst])
            if kind in ('k', 'v'):
                for t4 in range(4):
                    bk = banks[2 + t4 % 2]
                    r_ = t4 % 2
                    for ti in range(4):
                        tt = t4 * 4 + ti
                        for jt in range(5):
                            pe(P, lambda e, jt=jt, tt=tt, ti=ti, bk=bk, ws=ws, hb=hb: e.matmul(
                                bk.ap[:, ti * 128:(ti + 1) * 128], lhsT=hb.ap[:, tt * 128 + jt:tt * 128 + jt + 128],
                                rhs=dg[ws][jt].ap, start=(jt == 0), stop=(jt == 4)), r=[dg[ws][jt], hb], w=[bk])
                    if kind == 'v':
                        for ti in range(4):
                            tt = t4 * 4 + ti
                            act(P, lambda e, bk=bk, ti=ti, tt=tt, li_=li_: e.activation(
                                out=vtm[tt].ap[:, li_, :], in_=bk.ap[:, ti * 128:(ti + 1) * 128], func=AF.Silu),
                                r=[bk], w=[vtm[tt]])
                    else:
                        act(P, lambda e, bk=bk, r_=r_: e.activation(out=qf[r_].ap, in_=bk.ap, func=AF.Silu), r=[bk], w=[qf[r_]])
                        dve(P, lambda e, r_=r_: e.tensor_tensor(out=sqb[r_].ap, in0=qf[r_].ap, in1=qf[r_].ap, op=ALU.mult),
                            r=[qf[r_]], w=[sqb[r_]])
                        dve(P, lambda e, r_=r_: e.tensor_reduce(out=ss4[r_].ap, in_=sqb[r_].ap.rearrange("p (a b) -> p a b", a=4),
                                                                axis=AX.X, op=ALU.add), r=[sqb[r_]], w=[ss4[r_]])
                        dve(P, lambda e, r_=r_: e.tensor_scalar(out=ss4[r_].ap, in0=ss4[r_].ap, scalar1=1.0e-6, scalar2=None,
                                                                op0=ALU.add), r=[ss4[r_]], w=[ss4[r_]])
                        act(P, lambda e, r_=r_: e.activation(out=ss4[r_].ap, in_=ss4[r_].ap, func=AF.Sqrt), r=[ss4[r_]], w=[ss4[r_]])
                        dve(P, lambda e, r_=r_: e.reciprocal(out=ss4[r_].ap, in_=ss4[r_].ap), r=[ss4[r_]], w=[ss4[r_]])
                        for ti in range(4):
                            tt = t4 * 4 + ti
                            dve(P, lambda e, r_=r_, ti=ti, tt=tt, li_=li_: e.tensor_scalar(
                                out=ktm[tt].ap[:, li_, :], in0=qf[r_].ap[:, ti * 128:(ti + 1) * 128],
                                scalar1=ss4[r_].ap[:, ti:ti + 1], scalar2=None, op0=ALU.mult),
                                r=[qf[r_], ss4[r_]], w=[ktm[tt]])
        for d in range(2):
            dve(P, lambda e, d=d: e.memset(Sf[d].ap, 0.0), r=[Sf[d]], w=[Sf[d]])
            dve(P, lambda e, d=d: e.memset(Sb[d].ap, 0.0), r=[Sb[d]], w=[Sb[d]])
        from itertools import zip_longest
        for step in range(16):
            for _ in zip_longest(unit(sq, hg, 0, step), unit(sq, hg, 1, 15 - step)):
                pass
        for tt in range(16):
            t = sq * 16 + tt
            r_ = tt % 2
            rows = slice(t * 128, (t + 1) * 128)
            cols = slice(hg * 512, (hg + 1) * 512)
            dma(P, lambda e, r_=r_, rows=rows, cols=cols: e.dma_start(out=of_t[r_].ap, in_=OF[0, rows, cols]),
                r=[('OF', 0, t, hg)], w=[of_t[r_]])
            dma(P, lambda e, r_=r_, rows=rows, cols=cols: e.dma_start(out=ob_t[r_].ap, in_=OF[1, rows, cols]),
                r=[('OF', 1, t, hg)], w=[ob_t[r_]])
            bk = banks[tt % 2]
            for dc in range(8):
                pe(P, lambda e, dc=dc, tt=tt, bk=bk: e.matmul(bk.ap, lhsT=xT[dc].ap[:, tt * 128:(tt + 1) * 128], rhs=wz[dc].ap,
                                                              start=(dc == 0), stop=(dc == 7)), r=[xT[dc], wz[dc]], w=[bk])
            act(P, lambda e, bk=bk, r_=r_: e.activation(out=zs[r_].ap, in_=bk.ap, func=AF.Silu), r=[bk], w=[zs[r_]])
            dve(P, lambda e, r_=r_: e.tensor_tensor(out=of_t[r_].ap, in0=of_t[r_].ap, in1=ob_t[r_].ap, op=ALU.add),
                r=[of_t[r_], ob_t[r_]], w=[of_t[r_]])
            dve(P, lambda e, r_=r_: e.tensor_tensor(out=sq5[r_].ap, in0=of_t[r_].ap, in1=of_t[r_].ap, op=ALU.mult),
                r=[of_t[r_]], w=[sq5[r_]])
            dve(P, lambda e, r_=r_: e.tensor_reduce(out=ms5[r_].ap, in_=sq5[r_].ap.rearrange("p (a b) -> p a b", a=4),
                                                    axis=AX.X, op=ALU.add), r=[sq5[r_]], w=[ms5[r_]])
            dve(P, lambda e, r_=r_: e.tensor_scalar(out=ms5[r_].ap, in0=ms5[r_].ap, scalar1=1.0 / 128.0, scalar2=1.0e-6,
                                                    op0=ALU.mult, op1=ALU.add), r=[ms5[r_]], w=[ms5[r_]])
            act(P, lambda e, r_=r_: e.activation(out=ms5[r_].ap, in_=ms5[r_].ap, func=AF.Sqrt), r=[ms5[r_]], w=[ms5[r_]])
            dve(P, lambda e, r_=r_: e.reciprocal(out=ms5[r_].ap, in_=ms5[r_].ap), r=[ms5[r_]], w=[ms5[r_]])
            o3 = lambda b: b.ap.rearrange("p (a b) -> p a b", a=4)
            dve(P, lambda e, r_=r_: e.tensor_tensor(out=o3(of_t[r_]), in0=o3(of_t[r_]), in1=bc_mid(ms5[r_].ap, 128), op=ALU.mult),
                r=[of_t[r_], ms5[r_]], w=[of_t[r_]])
            pool(P, lambda e, r_=r_: e.tensor_tensor(out=o3(of_t[r_]), in0=o3(of_t[r_]), in1=bc_heads(nw_bc.ap, 4), op=ALU.mult),
                 r=[of_t[r_], nw_bc], w=[of_t[r_]])
            dve(P, lambda e, r_=r_: e.tensor_tensor(out=onb[r_].ap, in0=of_t[r_].ap, in1=zs[r_].ap, op=ALU.mult),
                r=[of_t[r_], zs[r_]], w=[onb[r_]])
            dma(P, lambda e, r_=r_, rows=rows, cols=cols: e.dma_start(out=ON[rows, cols], in_=onb[r_].ap),
                r=[onb[r_]], w=[('ON', t, hg)])

    def unit(sq, hg, d, tt):
        S = sets[d]
        b0, b1, b2, b3 = S.bk
        t = sq * 16 + tt
        tk = slice(tt * 128, (tt + 1) * 128)
        Tri, nTri = (C.Ui, C.nUi) if d == 0 else (C.Li, C.nLi)
        NMs = C.NM_Ls if d == 0 else C.NM_Us
        NMi = C.NM_Ui if d == 0 else C.NM_Li
        h0 = hg * 4
        g4 = Gt.ap[:, tt, d, h0:h0 + 4]
        be4 = Bt.ap[:, tt, d, h0:h0 + 4]
        v3 = lambda b: b.ap
        pool(P, lambda e: e.tensor_copy(out=S.Gb.ap, in_=bc_mid(g4, 128)), r=[Gt], w=[S.Gb])
        pool(P, lambda e: e.tensor_tensor(out=S.RG.ap, in0=S.Gb.ap, in1=bc_heads(Tri.ap, 4), op=ALU.mult),
             r=[S.Gb, Tri], w=[S.RG])
        f2 = lambda b: b.ap.rearrange("p a b -> p (a b)")
        pe(P, lambda e: e.matmul(b0.ap, lhsT=Tri.ap, rhs=f2(S.Gb), start=True, stop=False), r=[Tri, S.Gb], w=[b0])
        pe(P, lambda e: e.matmul(b0.ap, lhsT=C.NegOnesF.ap, rhs=f2(S.RG), start=False, stop=True), r=[C.NegOnesF, S.RG], w=[b0])
        pe(P, lambda e: e.matmul(b1.ap, lhsT=C.OnesF.ap, rhs=f2(S.RG), start=True, stop=False), r=[C.OnesF, S.RG], w=[b1])
        pe(P, lambda e: e.matmul(b1.ap, lhsT=nTri.ap, rhs=f2(S.Gb), start=False, stop=True), r=[nTri, S.Gb], w=[b1])
        for kh in range(2):
            pe(P, lambda e, kh=kh: e.matmul(b2.ap[:, kh * 128:(kh + 1) * 128], lhsT=kT[kh].ap[:, tk], rhs=kT[kh].ap[:, tk],
                                            start=True, stop=True), r=[kT[kh]], w=[b2])
            pe(P, lambda e, kh=kh: e.matmul(b2.ap[:, 256 + kh * 128:256 + (kh + 1) * 128], lhsT=kT[kh].ap[:, tk],
                                            rhs=qT[kh].ap[:, tk], start=True, stop=True), r=[kT[kh], qT[kh]], w=[b2])
        pe(P, lambda e: e.matmul(b3.ap[:, 0:4], lhsT=Tri.ap, rhs=g4, start=True, stop=True), r=[Tri, Gt], w=[b3])
        pe(P, lambda e: e.matmul(b3.ap[:, 4:8], lhsT=C.SC.ap, rhs=g4, start=True, stop=True), r=[C.SC, Gt], w=[b3])
        pe(P, lambda e: e.matmul(b3.ap[:, 8:12], lhsT=C.Half0.ap, rhs=g4, start=True, stop=True), r=[C.Half0, Gt], w=[b3])
        pe(P, lambda e: e.matmul(b3.ap[:, 12:16], lhsT=C.Half1.ap, rhs=g4, start=True, stop=True), r=[C.Half1, Gt], w=[b3])
        dve(P, lambda e: e.tensor_copy(out=S.gc.ap, in_=b3.ap[:, 0:4]), r=[b3], w=[S.gc])
        act(P, lambda e: e.activation(out=S.egc.ap, in_=b3.ap[:, 0:4], func=AF.Exp), r=[b3], w=[S.egc])
        dve(P, lambda e: e.tensor_tensor(out=S.ekd.ap, in0=b3.ap[:, 4:8], in1=S.gc.ap, op=ALU.subtract), r=[b3, S.gc], w=[S.ekd])
        act(P, lambda e: e.activation(out=S.ekd.ap, in_=S.ekd.ap, func=AF.Exp), r=[S.ekd], w=[S.ekd])
        act(P, lambda e: e.activation(out=S.gl.ap.rearrange("p a b -> p (a b)"), in_=b3.ap[:, 8:16], func=AF.Exp), r=[b3], w=[S.gl])
        dve(P, lambda e: e.tensor_tensor(out=S.bge.ap, in0=S.egc.ap, in1=be4, op=ALU.mult), r=[S.egc, Bt], w=[S.bge])
        dve(P, lambda e: e.tensor_scalar(out=S.nbeta.ap, in0=be4, scalar1=-1.0, scalar2=None, op0=ALU.mult), r=[Bt], w=[S.nbeta])
        dve(P, lambda e: e.tensor_tensor(out=S.dS.ap, in0=b0.ap.rearrange("p (a b) -> p a b", a=4), in1=bc_heads(NMs.ap, 4), op=ALU.add),
            r=[b0, NMs], w=[S.dS])
        act(P, lambda e: e.activation(out=S.dS.ap, in_=S.dS.ap, func=AF.Exp), r=[S.dS], w=[S.dS])
        dve(P, lambda e: e.tensor_tensor(out=S.dT.ap, in0=b1.ap.rearrange("p (a b) -> p a b", a=4), in1=bc_heads(NMi.ap, 4), op=ALU.add),
            r=[b1, NMi], w=[S.dT])
        act(P, lambda e: e.activation(out=S.dT.ap, in_=S.dT.ap, func=AF.Exp), r=[S.dT], w=[S.dT])
        yield
        for kh in range(2):
            dve(P, lambda e, kh=kh: e.tensor_tensor(out=S.qkT.ap[:, 2 * kh:2 * kh + 2, :], in0=S.dT.ap[:, 2 * kh:2 * kh + 2, :],
                                                   in1=bc_heads(b2.ap[:, 256 + kh * 128:256 + (kh + 1) * 128], 2), op=ALU.mult),
                r=[S.dT, b2], w=[S.qkT])
        for h in range(4):
            dve(P, lambda e, h=h: e.scalar_tensor_tensor(out=S.XA.ap[:, h, :], in0=S.dS.ap[:, h, :], scalar=S.nbeta.ap[:, h:h + 1],
                                                         in1=b2.ap[:, (h // 2) * 128:(h // 2 + 1) * 128],
                                                         op0=ALU.mult, op1=ALU.mult), r=[S.dS, S.nbeta, b2], w=[S.XA])
        pool(P, lambda e: e.tensor_tensor(out=S.vb.ap, in0=vtm[tt].ap, in1=bc_mid(be4, 128), op=ALU.mult), r=[vtm[tt], Bt], w=[S.vb])
        for h in range(4):
            pool(P, lambda e, h=h: e.tensor_scalar(out=S.kbg.ap[:, h, :], in0=ktm[tt].ap[:, h // 2, :], scalar1=S.bge.ap[:, h:h + 1],
                                                   scalar2=None, op0=ALU.mult), r=[ktm[tt], S.bge], w=[S.kbg])
            pool(P, lambda e, h=h: e.tensor_scalar(out=S.kd.ap[:, h, :], in0=ktm[tt].ap[:, h // 2, :], scalar1=S.ekd.ap[:, h:h + 1],
                                                   scalar2=None, op0=ALU.mult), r=[ktm[tt], S.ekd], w=[S.kd])
        yield
        for h in range(4):
            pe(P, lambda e, h=h: e.transpose(b0.ap[:, h * 128:(h + 1) * 128], S.XA.ap[:, h, :], C.ident_f.ap),
               r=[S.XA, C.ident_f], w=[b0])
        act(P, lambda e: e.copy(out=f2(S.XTA), in_=b0.ap), r=[b0], w=[S.XTA])
        dve(P, lambda e: e.tensor_tensor(out=S.PTA.ap, in0=S.XTA.ap, in1=bc_heads(C.ident_f.ap, 4), op=ALU.add),
            r=[S.XTA, C.ident_f], w=[S.PTA])
        yield
        Xc, XTc, PTc = S.XA, S.XTA, S.PTA
        Xn_, XTn_, PTn_ = S.XB, S.XTB, S.PTB
        for lvl in range(5):
            last = (lvl == 4)
            for h in range(4):
                pe(P, lambda e, h=h, Xc=Xc, XTc=XTc: e.matmul(b0.ap[:, h * 128:(h + 1) * 128], lhsT=XTc.ap[:, h, :], rhs=Xc.ap[:, h, :],
                                                             start=True, stop=True), r=[Xc, XTc], w=[b0])
            if not last:
                for h in range(4):
                    pe(P, lambda e, h=h, Xc=Xc, XTc=XTc: e.matmul(b1.ap[:, h * 128:(h + 1) * 128], lhsT=Xc.ap[:, h, :],
                                                                 rhs=XTc.ap[:, h, :], start=True, stop=True), r=[Xc, XTc], w=[b1])
            act(P, lambda e, Xn_=Xn_: e.copy(out=f2(Xn_), in_=b0.ap), r=[b0], w=[Xn_])
            if not last:
                dve(P, lambda e, XTn_=XTn_: e.tensor_copy(out=f2(XTn_), in_=b1.ap), r=[b1], w=[XTn_])
            yield
            for h in range(4):
                pe(P, lambda e, h=h, Xn_=Xn_, PTc=PTc: e.matmul(b2.ap[:, h * 128:(h + 1) * 128], lhsT=Xn_.ap[:, h, :],
                                                               rhs=PTc.ap[:, h, :], start=True, stop=True), r=[Xn_, PTc], w=[b2])
            dve(P, lambda e, PTn_=PTn_, PTc=PTc: e.tensor_tensor(out=f2(PTn_), in0=b2.ap, in1=f2(PTc), op=ALU.add),
                r=[b2, PTc], w=[PTn_])
            yield
            Xc, Xn_ = Xn_, Xc
            XTc, XTn_ = XTn_, XTc
            PTc, PTn_ = PTn_, PTc
        act(P, lambda e, PTc=PTc: e.copy(out=S.Tb.ap, in_=PTc.ap), r=[PTc], w=[S.Tb])
        for h in range(4):
            pe(P, lambda e, h=h: e.matmul(b0.ap[:, h * 128:(h + 1) * 128], lhsT=S.Tb.ap[:, h, :], rhs=S.vb.ap[:, h, :],
                                          start=True, stop=True), r=[S.Tb, S.vb], w=[b0])
        for h in range(4):
            pe(P, lambda e, h=h: e.matmul(b1.ap[:, h * 128:(h + 1) * 128], lhsT=S.kbg.ap[:, h, :], rhs=S.Tb.ap[:, h, :],
                                          start=True, stop=True), r=[S.kbg, S.Tb], w=[b1])
        dve(P, lambda e: e.tensor_copy(out=f2(S.u), in_=b0.ap), r=[b0], w=[S.u])
        act(P, lambda e: e.copy(out=f2(S.wT), in_=b1.ap), r=[b1], w=[S.wT])
        yield
        for c in ((0, 1) if d == 0 else (1, 0)):
            cs = slice(c * 64, (c + 1) * 64)
            ck = (b2.keys[0], c)
            for h in range(4):
                pe(P, lambda e, h=h, cs=cs: e.matmul(b2.ap[cs, h * 128:(h + 1) * 128], lhsT=S.wT.ap[:, h, cs], rhs=Sb[d].ap[:, h, :],
                                                     start=True, stop=True), r=[S.wT, Sb[d]], w=[b2])
            for h in range(4):
                pe(P, lambda e, h=h, cs=cs, c=c: e.matmul(b3.ap[cs, h * 128:(h + 1) * 128], lhsT=qT[h // 2].ap[:, tt * 128 + c * 64:tt * 128 + (c + 1) * 64],
                                                     rhs=Sb[d].ap[:, h, :], start=True, stop=True), r=[qT[h // 2], Sb[d]], w=[b3])
            dve(P, lambda e, cs=cs: e.tensor_tensor(out=f2(S.vn)[cs, :], in0=f2(S.u)[cs, :], in1=b2.ap[cs, :], op=ALU.subtract),
                r=[S.u, b2], w=[S.vn])
            dbg = getattr(C, 'dbg_unit', None)
            if dbg and (sq, hg, d, tt) == (0, 0, 0, 1) and c == 0:
                dma(P, lambda e: e.dma_start(out=dbg['Sb'], in_=f2(Sb[d])), r=[Sb[d]], w=['dbgSb'])
                dma(P, lambda e: e.dma_start(out=dbg['vn'], in_=f2(S.vn)), r=[S.vn], w=['dbgvn'])
                dma(P, lambda e: e.dma_start(out=dbg['u'], in_=f2(S.u)), r=[S.u], w=['dbgu'])
                dma(P, lambda e: e.dma_start(out=dbg['egc'], in_=S.egc.ap), r=[S.egc], w=['dbgegc'])
                dma(P, lambda e: e.dma_start(out=dbg['qkT'], in_=f2(S.qkT)), r=[S.qkT], w=['dbgqkT'])
                dve(P, lambda e: e.tensor_copy(out=f2(S.osb), in_=b3.ap), r=[b3], w=[S.osb])
                dma(P, lambda e: e.dma_start(out=dbg['po1'], in_=f2(S.osb)), r=[S.osb], w=['dbgpo1'])
            yield
            for h in range(4):
                pe(P, lambda e, h=h, cs=cs: e.matmul(b0.ap[cs, h * 128:(h + 1) * 128], lhsT=S.qkT.ap[cs, h, cs], rhs=S.vn.ap[cs, h, :],
                                                     start=True, stop=True), r=[S.qkT, S.vn], w=[b0])
            for h in range(4):
                pe(P, lambda e, h=h, cs=cs: e.matmul(b1.ap[:, h * 128:(h + 1) * 128], lhsT=S.kd.ap[cs, h, :], rhs=S.vn.ap[cs, h, :],
                                                     start=True, stop=True), r=[S.kd, S.vn], w=[b1])
            dve(P, lambda e, cs=cs: e.tensor_tensor(out=S.otmp.ap[cs], in0=b3.ap[cs, :].rearrange("p (a b) -> p a b", a=4),
                                                    in1=bc_mid(S.egc.ap[cs, :], 128), op=ALU.mult), r=[b3, S.egc], w=[S.otmp])
            if dbg and (sq, hg, d, tt) == (0, 0, 0, 1) and c == 0:
                dve(P, lambda e: e.tensor_copy(out=f2(S.osb), in_=b0.ap), r=[b0], w=[S.osb])
                dma(P, lambda e: e.dma_start(out=dbg['po2'], in_=f2(S.osb)), r=[S.osb], w=['dbgpo2'])
                dma(P, lambda e: e.dma_start(out=dbg['otmp'], in_=f2(S.otmp)), r=[S.otmp], w=['dbgotmp'])
            dve(P, lambda e, cs=cs: e.tensor_tensor(out=f2(S.osb)[cs, :], in0=f2(S.otmp)[cs, :], in1=b0.ap[cs, :], op=ALU.add),
                r=[S.otmp, b0], w=[S.osb])
            for h in range(4):
                dve(P, lambda e, h=h, c=c: e.scalar_tensor_tensor(out=Sf[d].ap[:, h, :], in0=Sf[d].ap[:, h, :],
                                                                  scalar=S.gl.ap[:, c, h:h + 1], in1=b1.ap[:, h * 128:(h + 1) * 128],
                                                                  op0=ALU.mult, op1=ALU.add), r=[Sf[d], S.gl, b1], w=[Sf[d]])
            act(P, lambda e: e.copy(out=Sb[d].ap, in_=Sf[d].ap), r=[Sf[d]], w=[Sb[d]])
            yield
        dma(P, lambda e: e.dma_start(out=OF[d, t * 128:(t + 1) * 128, hg * 512:(hg + 1) * 512], in_=f2(S.osb)),
            r=[S.osb], w=[('OF', d, t, hg)])

    for sq in range(2):
        seq_body(sq)
    a = Alloc(C.arena, gdn_dyn)
    wo = [a.new([D], BF16) for _ in range(16)]
    ont = [a.new([2048], BF16) for _ in range(2)]
    onT = [a.new([16, 128], BF16) for _ in range(2)]
    ysb = [a.new([D], F32) for _ in range(2)]
    for c in range(16):
        dma(P, lambda e, c=c: e.dma_start(out=wo[c].ap, in_=W['a_w_out'][jl_, c * 128:(c + 1) * 128, :]), w=[wo[c]], q='pool')
    for t in range(NT):
        s = t % 2
        rows = slice(t * 128, (t + 1) * 128)
        dma(P, lambda e, s=s, rows=rows: e.dma_start(out=ont[s].ap, in_=ON[rows, :]), r=[('ON', t, hg) for hg in range(4)], w=[ont[s]])
        for c4 in range(4):
            bk = banks[c4 % 2]
            bkb = bk.ap.bitcast(BF16)
            for ci in range(4):
                c = c4 * 4 + ci
                pe(P, lambda e, c=c, ci=ci, s=s, bkb=bkb: e.transpose(bkb[:, ci * 128:(ci + 1) * 128], ont[s].ap[:, c * 128:(c + 1) * 128],
                                                                     C.ident_b.ap), r=[ont[s], C.ident_b], w=[bk])
            act(P, lambda e, c4=c4, s=s, bkb=bkb: e.copy(out=onT[s].ap[:, c4 * 4:(c4 + 1) * 4, :].rearrange("p a b -> p (a b)"),
                                                       in_=bkb[:, 0:512]), r=[bk, onT[s]], w=[onT[s]])
        for half in range(2):
            bk = banks[2 + half]
            for c in range(16):
                pe(P, lambda e, c=c, half=half, s=s, bk=bk: e.matmul(bk.ap, lhsT=onT[s].ap[:, c, :], rhs=wo[c].ap[:, half * 512:(half + 1) * 512],
                                                                   start=(c == 0), stop=(c == 15)), r=[onT[s], wo[c]], w=[bk])
            act(P, lambda e, half=half, s=s, bk=bk: e.copy(out=ysb[s].ap[:, half * 512:(half + 1) * 512], in_=bk.ap),
                r=[bk, ysb[s]], w=[ysb[s]])
        dma(P, lambda e, s=s, rows=rows: e.dma_start(out=H_ap[rows, :], in_=ysb[s].ap), r=[ysb[s]], w=[(H_k, t)])


_W_SPECS = [
    ('a_w_in', [2, 1024, 6208]), ('a_conv_w', [2, 5, 4096]), ('a_A_log', [2, 2, 16]), ('a_dt_bias', [2, 2, 16]),
    ('a_norm_w', [2, 128]), ('a_w_out', [2, 2048, 1024]), ('b_w_in', [2, 1024, 1536]), ('b_b_in', [2, 1536]),
    ('b_sinks', [2, 16]), ('b_w_out', [2, 1024, 1024]), ('b_b_out', [2, 1024]), ('router_w', [4, 1024, 32]),
    ('router_b', [4, 32]), ('exp_w_up', [4, 32, 1024, 2048]), ('exp_b_up', [4, 32, 2048]),
    ('exp_w_down', [4, 32, 1024, 1024]), ('exp_b_down', [4, 32, 1024]), ('ln_g', [4, 2, 1024]), ('ln_b', [4, 2, 1024]),
]


def build_program(depth=DEPTH):
    nc = bass.Bass("TRN2", target_bir_lowering=False)
    dt = lambda n, s, k="ExternalInput", d=F32: nc.dram_tensor(n, s, d, kind=k).ap()
    x = dt("x", [NTOK, D])
    W = {n: dt(n, s) for n, s in _W_SPECS}
    out = dt("out", [NTOK, D], "ExternalOutput")
    with ExitStack() as st:
        P = Prog(nc)
        C = Ctx()
        C.nc = nc
        C.XS = dt("XS", [NE * CAP, D], "Internal", BF16)
        C.Y = dt("Y", [NE * CAP, D], "Internal", F32)
        C.OF = dt("OF", [2, NTOK, 2048], "Internal", F32)
        C.ON = dt("ON", [NTOK, 2048], "Internal", BF16)
        XA = dt("XA", [NTOK, D], "Internal")
        XB = dt("XB", [NTOK, D], "Internal")
        Hh = dt("Hh", [NTOK, D], "Internal")
        C.arena = Arena(nc, st, 176 * 1024)
        C.banks = []
        for i in range(8):
            t = st.enter_context(nc.psum_tensor("bank%d" % i, [128, 512], F32))
            C.banks.append(Buf(t[:], [('ps', i)]))
        al = Alloc(C.arena, 0)
        setup_consts(P, C, al)
        C.dyn_start = al.off
        cur = (x, 'x')
        for li in range(depth):
            if li % 2 == 0:
                gdn_stage(P, C, li // 2, cur, (Hh, 'H'), W)
            else:
                attn_stage(P, C, li // 2, cur, (Hh, 'H'), W)
            dst = (out, 'out') if li == depth - 1 else (XB, 'XB')
            moe_stage(P, C, li, cur, (XA, 'XA'), dst, (Hh, 'H'), W)
            cur = dst
        P.emit(st)
    return nc


_LAYER_W = {
    'a': ['a_w_in', 'a_conv_w', 'a_A_log', 'a_dt_bias', 'a_norm_w', 'a_w_out'],
    'b': ['b_w_in', 'b_b_in', 'b_sinks', 'b_w_out', 'b_b_out'],
    'm': ['router_w', 'router_b', 'exp_w_up', 'exp_b_up', 'exp_w_down', 'exp_b_down', 'ln_g', 'ln_b'],
}


def build_layer_program(kind):
    nc = bass.Bass("TRN2", target_bir_lowering=False)
    dt = lambda n, s, k="ExternalInput", d=F32: nc.dram_tensor(n, s, d, kind=k).ap()
    x = dt("x", [NTOK, D])
    out = dt("out", [NTOK, D], "ExternalOutput")
    spec = dict(_W_SPECS)
    names = {'a': _LAYER_W['a'] + _LAYER_W['m'], 'b': _LAYER_W['b'], 'm': _LAYER_W['m']}[kind]
    W = {n: dt(n, [1] + spec[n][1:]) for n in names}
    if kind == 'm':
        Hh = dt("h", [NTOK, D])
    elif kind == 'b':
        Hh = None
    else:
        Hh = dt("Hh", [NTOK, D], "Internal")
    with ExitStack() as st:
        P = Prog(nc)
        C = Ctx()
        C.nc = nc
        if kind in ('a', 'm'):
            C.XS = dt("XS", [NE * CAP, D], "Internal", BF16)
            C.Y = dt("Y", [NE * CAP, D], "Internal", F32)
            XA = dt("XA", [NTOK, D], "Internal")
        if kind == 'a':
            C.OF = dt("OF", [2, NTOK, 2048], "Internal", F32)
            C.ON = dt("ON", [NTOK, 2048], "Internal", BF16)
        C.arena = Arena(nc, st, 176 * 1024)
        C.banks = []
        for i in range(8):
            t = st.enter_context(nc.psum_tensor("bank%d" % i, [128, 512], F32))
            C.banks.append(Buf(t[:], [('ps', i)]))
        al = Alloc(C.arena, 0)
        setup_consts(P, C, al)
        C.dyn_start = al.off
        if kind == 'a':
            gdn_stage(P, C, 0, (x, 'x'), (Hh, 'H'), W)
            moe_stage(P, C, 0, (x, 'x'), (XA, 'XA'), (out, 'out'), (Hh, 'H'), W)
        elif kind == 'b':
            attn_stage(P, C, 0, (x, 'x'), (out, 'out'), W)
        else:
            moe_stage(P, C, 0, (x, 'x'), (XA, 'XA'), (out, 'out'), (Hh, 'H'), W)
        P.emit(st)
    return nc


def kernel(**inputs):
    n = 8
    x = np.ascontiguousarray(np.asarray(inputs['x'], dtype=np.float32))
    xs = x.reshape(n, NTOK, D)
    nc = build_program()
    wmap = {name: np.ascontiguousarray(np.asarray(inputs[name], dtype=np.float32)) for name, _ in _W_SPECS}
    in_maps = []
    for c in range(n):
        m = dict(wmap)
        m['x'] = xs[c]
        in_maps.append(m)
    res = run_bass_kernel_spmd(nc, in_maps, core_ids=list(range(n)))
    outs = [np.asarray(r['out']) for r in res.results]
    return np.stack(outs, 0).reshape(16, SEQ, D).astype(np.float32)
```

```python
from contextlib import ExitStack
import numpy as np
import concourse.bass as bass
import concourse.mybir as mybir
from concourse.bass_utils import run_bass_kernel_spmd

F32 = mybir.dt.float32
BF16 = mybir.dt.bfloat16
U32 = mybir.dt.uint32
I32 = mybir.dt.int32
AF = mybir.ActivationFunctionType
ALU = mybir.AluOpType
AX = mybir.AxisListType

DMA_RING = 16


class Prog:
    def __init__(self, nc):
        self.nc = nc
        self.ops = []
        self.last_w = {}
        self.readers = {}

    def add(self, eng, fn, reads=(), writes=(), dma=False):
        idx = len(self.ops)
        deps = set()
        for k in reads:
            w = self.last_w.get(k)
            if w is not None:
                deps.add(w)
        for k in writes:
            w = self.last_w.get(k)
            if w is not None:
                deps.add(w)
            rs = self.readers.get(k)
            if rs:
                deps.update(rs)
        for k in reads:
            self.readers.setdefault(k, []).append(idx)
        for k in writes:
            self.last_w[k] = idx
            self.readers[k] = []
        self.ops.append((eng, fn, deps, dma))
        return idx

    def pe(self, fn, reads=(), writes=()):
        return self.add('pe', fn, reads, writes)

    def dve(self, fn, reads=(), writes=()):
        return self.add('dve', fn, reads, writes)

    def act(self, fn, reads=(), writes=()):
        return self.add('act', fn, reads, writes)

    def pool(self, fn, reads=(), writes=()):
        return self.add('pool', fn, reads, writes)

    def dma(self, fn, reads=(), writes=(), q='sp'):
        return self.add(q, fn, reads, writes, dma=True)

    def emit(self, stack):
        nc = self.nc
        ops = self.ops
        n = len(ops)
        needed = [False] * n
        for (eng, fn, deps, dma) in ops:
            for d in deps:
                needed[d] = True
        engs = ('pe', 'dve', 'act', 'pool', 'sp')
        esem = {e: stack.enter_context(nc.semaphore('s_' + e)) for e in engs}
        dsem = {e: [stack.enter_context(nc.semaphore('d_%s%d' % (e, i))) for i in range(DMA_RING)]
                for e in ('sp', 'act', 'pool')}
        ecount = {e: 0 for e in engs}
        dcount = {e: 0 for e in dsem}
        event = [None] * n
        extra_wait = [None] * n
        dma_last = {}
        for i, (eng, fn, deps, dma) in enumerate(ops):
            if dma:
                k = dcount[eng]
                dcount[eng] += 1
                slot = k % DMA_RING
                val = 16 * (k // DMA_RING + 1)
                sem = dsem[eng][slot]
                event[i] = (('d', eng, slot), sem, val, 16)
                if k >= DMA_RING:
                    extra_wait[i] = (('d', eng, slot), sem, val - 16)
                dma_last[(eng, slot)] = (('d', eng, slot), sem, val)
                needed[i] = True
            elif needed[i]:
                ecount[eng] += 1
                event[i] = (('e', eng), esem[eng], ecount[eng], 1)
        per_eng = {e: [] for e in engs}
        seen = {e: {} for e in engs}
        for i, (eng, fn, deps, dma) in enumerate(ops):
            waits = []
            cand = []
            if extra_wait[i] is not None:
                cand.append(extra_wait[i])
            for d in sorted(deps):
                deng, _, _, ddma = ops[d]
                if deng == 'pe' and eng == 'pe' and not ddma and not dma:
                    continue
                ev = event[d]
                cand.append((ev[0], ev[1], ev[2]))
            best = {}
            for (sid, sem, val) in cand:
                if seen[eng].get(sid, 0) >= val:
                    continue
                if sid not in best or best[sid][1] < val:
                    best[sid] = (sem, val)
            for sid, (sem, val) in best.items():
                seen[eng][sid] = val
                waits.append((sem, val))
            per_eng[eng].append((fn, waits, event[i] if needed[i] else None))
        final_waits = []
        for (eng, slot), (sid, sem, val) in dma_last.items():
            if seen['sp'].get(sid, 0) < val:
                final_waits.append((sem, val))
        self.stats = {e: len(per_eng[e]) for e in engs}

        with nc.Block() as block:
            def run(e_obj, lst, tail=()):
                for fn, waits, ev in lst:
                    for sem, val in waits:
                        e_obj.wait_ge(sem, val)
                    inst = fn(e_obj)
                    if ev is not None:
                        inst.then_inc(ev[1], ev[3])
                for sem, val in tail:
                    e_obj.wait_ge(sem, val)

            @block.tensor
            def _(e):
                run(e, per_eng['pe'])

            @block.vector
            def _(e):
                run(e, per_eng['dve'])

            @block.scalar
            def _(e):
                run(e, per_eng['act'])

            @block.gpsimd
            def _(e):
                run(e, per_eng['pool'])

            @block.sync
            def _(e):
                run(e, per_eng['sp'], final_waits)


GRAN = 256


class Buf:
    __slots__ = ('ap', 'keys')

    def __init__(self, ap, keys):
        self.ap = ap
        self.keys = keys


def _flat(items):
    out = []
    for k in items:
        if isinstance(k, Buf):
            out.extend(k.keys)
        else:
            out.append(k)
    return out


_ESZ = {F32: 4, BF16: 2, U32: 4, I32: 4}


class Arena:
    def __init__(self, nc, st, nbytes, name="arena"):
        self.t = st.enter_context(nc.sbuf_tensor(name, [128, nbytes // 4], F32))
        self.nbytes = nbytes

    def buf(self, off, shape, dtype, parts=128):
        nel = 1
        for s in shape:
            nel *= s
        nb = nel * _ESZ[dtype]
        assert off % 4 == 0 and off + nb <= self.nbytes, (off, nb, self.nbytes)
        v = self.t[0:parts, off // 4:(off + nb + 3) // 4]
        if dtype != F32:
            v = v.bitcast(dtype)
        if len(shape) == 2:
            v = v.rearrange("p (a b) -> p a b", a=shape[0])
        elif len(shape) == 3:
            v = v.rearrange("p (a b c) -> p a b c", a=shape[0], b=shape[1])
        keys = [('sb', g) for g in range(off // GRAN, (off + nb - 1) // GRAN + 1)]
        return Buf(v, keys)


class Alloc:
    def __init__(self, arena, start, end=None):
        self.arena = arena
        self.off = start
        self.end = end if end is not None else arena.nbytes

    def new(self, shape, dtype, parts=128):
        nel = 1
        for s in shape:
            nel *= s
        nb = nel * _ESZ[dtype]
        b = self.arena.buf(self.off, shape, dtype, parts)
        self.off += (nb + GRAN - 1) // GRAN * GRAN
        assert self.off <= self.end, ("SBUF overflow", self.off, self.end)
        return b


D = 1024
NTOK = 4096
NT = NTOK // 128
SEQ = 2048
NE = 32
CAP = 768
NB = 256
NBLK = CAP // NB
NST = NB // 128
DEPTH = 4
ALPHA = float(8 ** 0.25)
LN_EPS = 1e-5


class Ctx:
    pass


def P_add(P, eng, fn, reads=(), writes=(), dma=False):
    return P.add(eng, fn, _flat(reads), _flat(writes), dma)


def dve(P, fn, r=(), w=()):
    return P.add('dve', fn, _flat(r), _flat(w))


def act(P, fn, r=(), w=()):
    return P.add('act', fn, _flat(r), _flat(w))


def pool(P, fn, r=(), w=()):
    return P.add('pool', fn, _flat(r), _flat(w))


def pe(P, fn, r=(), w=()):
    return P.add('pe', fn, _flat(r), _flat(w))


def dma(P, fn, r=(), w=(), q='sp'):
    return P.add(q, fn, _flat(r), _flat(w), True)


def _bc_reg(C, e):
    if getattr(C, 'bc_reg', None) is None:
        C.bc_reg = e.to_reg(NE * CAP - 1)
    return C.bc_reg


def barrier(P, C, key):
    dve(P, lambda e: e.memset(C.junk1.ap, 0.0), r=[], w=[C.junk1, key])


def setup_consts(P, C, al):
    C.ident_f = al.new([128], F32)
    C.ident_b = al.new([128], BF16)
    C.ones_b = al.new([128], BF16)
    C.su_b = al.new([128], BF16)
    C.ecap = al.new([NE], F32)
    C.capmax = al.new([NE], F32)
    C.junk1 = al.new([8], F32)
    tmp = al.new([128], F32)
    pool(P, lambda e: e.memset(C.ident_f.ap, 1.0), w=[C.ident_f])
    pool(P, lambda e: e.affine_select(out=C.ident_f.ap, in_=C.ident_f.ap, pattern=[[-1, 128]],
                                       compare_op=ALU.is_equal, fill=0.0, base=0, channel_multiplier=1),
         r=[C.ident_f], w=[C.ident_f])
    dve(P, lambda e: e.tensor_copy(out=C.ident_b.ap, in_=C.ident_f.ap), r=[C.ident_f], w=[C.ident_b])
    dve(P, lambda e: e.memset(C.ones_b.ap, 1.0), w=[C.ones_b])
    pool(P, lambda e: e.memset(tmp.ap, 1.0), w=[tmp])
    pool(P, lambda e: e.affine_select(out=tmp.ap, in_=tmp.ap, pattern=[[1, 128]],
                                       compare_op=ALU.is_gt, fill=0.0, base=0, channel_multiplier=-1),
         r=[tmp], w=[tmp])
    dve(P, lambda e: e.tensor_copy(out=C.su_b.ap, in_=tmp.ap), r=[tmp], w=[C.su_b])
    pool(P, lambda e: e.iota(C.ecap.ap, pattern=[[CAP, NE]], base=0, channel_multiplier=0,
                              allow_small_or_imprecise_dtypes=True), w=[C.ecap])
    dve(P, lambda e: e.tensor_scalar(out=C.capmax.ap, in0=C.ecap.ap, scalar1=float(CAP - 1), scalar2=None,
                                      op0=ALU.add), r=[C.ecap], w=[C.capmax])


def ln_tile(P, xa, ha, gbc, bbc, sm):
    st, mv, sd, rstd = sm
    dve(P, lambda e: e.scalar_tensor_tensor(out=xa.ap, in0=xa.ap, scalar=ALPHA, in1=ha.ap,
                                            op0=ALU.mult, op1=ALU.add), r=[xa, ha], w=[xa])
    dve(P, lambda e: e.bn_stats(out=st.ap[:, 0:6], in_=xa.ap[:, 0:512]), r=[xa], w=[st])
    dve(P, lambda e: e.bn_stats(out=st.ap[:, 6:12], in_=xa.ap[:, 512:1024]), r=[xa, st], w=[st])
    dve(P, lambda e: e.bn_aggr(out=mv.ap, in_=st.ap), r=[st], w=[mv])
    dve(P, lambda e: e.tensor_scalar(out=sd.ap, in0=mv.ap[:, 1:2], scalar1=LN_EPS, scalar2=None, op0=ALU.add),
        r=[mv], w=[sd])
    act(P, lambda e: e.activation(out=sd.ap, in_=sd.ap, func=AF.Sqrt), r=[sd], w=[sd])
    dve(P, lambda e: e.reciprocal(out=rstd.ap, in_=sd.ap), r=[sd], w=[rstd])
    dve(P, lambda e: e.tensor_scalar(out=xa.ap, in0=xa.ap, scalar1=mv.ap[:, 0:1], scalar2=rstd.ap[:, 0:1],
                                     op0=ALU.subtract, op1=ALU.mult), r=[xa, mv, rstd], w=[xa])
    pool(P, lambda e: e.tensor_tensor(out=xa.ap, in0=xa.ap, in1=gbc.ap, op=ALU.mult), r=[xa, gbc], w=[xa])
    pool(P, lambda e: e.tensor_tensor(out=xa.ap, in0=xa.ap, in1=bbc.ap, op=ALU.add), r=[xa, bbc], w=[xa])


def moe_stage(P, C, li, Xin, Xmid, Xout, H, W):
    nc = C.nc
    (Xin_ap, Xin_k), (Xmid_ap, Xmid_k), (Xout_ap, Xout_k), (H_ap, H_k) = Xin, Xmid, Xout, H
    XS, Y = C.XS, C.Y
    banks = C.banks
    al = Alloc(C.arena, C.dyn_start)
    destf = al.new([NT, 4], F32)
    desti = al.new([NT, 4], U32)
    gates = al.new([NT, 4], F32)
    g1 = al.new([D], F32)
    b1 = al.new([D], F32)
    g2, b2 = g1, b1
    rw = al.new([8, NE], F32)
    rb = al.new([NE], F32)
    bupT = al.new([16, NE], F32)
    cnt = [al.new([NE], F32), al.new([NE], F32)]
    sm = [(al.new([12], F32), al.new([2], F32), al.new([1], F32), al.new([1], F32)) for _ in range(2)]
    lg = [al.new([NE], F32) for _ in range(2)]
    mx8 = [al.new([8], F32) for _ in range(2)]
    nv1 = [al.new([1], F32) for _ in range(2)]
    e4 = [al.new([4], F32) for _ in range(2)]
    ssum = [al.new([1], F32) for _ in range(2)]
    maskb = [al.new([NE], BF16) for _ in range(2)]
    slot = [al.new([NE], F32) for _ in range(2)]
    oh = [al.new([NE], F32) for _ in range(2)]
    junk = [al.new([NE], F32) for _ in range(2)]
    big0 = al.off
    wu0 = [al.new([2048], BF16) for dc in range(8)]
    off_wu1 = al.off
    wu1 = [al.new([2048], BF16) for dc in range(8)]
    wu = [wu0, wu1]
    wd = [[al.new([1024], BF16) for fc in range(8)] for _ in range(2)]
    bd = [al.new([D], F32) for _ in range(2)]
    xs = [al.new([NST, D], BF16) for _ in range(2)]
    xsT = [[al.new([2, NB], BF16) for _ in range(4)] for _ in range(2)]
    actT = [[al.new([NB], BF16) for fc in range(8)] for _ in range(2)]
    gsb = [al.new([NB], F32) for _ in range(2)]
    sgb = [al.new([NB], F32) for _ in range(2)]
    usb = [al.new([NB], F32) for _ in range(2)]
    gsm = [al.new([NB], F32) for _ in range(2)]
    ysb = [al.new([D], F32) for _ in range(2)]
    a1 = Alloc(C.arena, off_wu1, off_wu1 + 32768)
    xa = [a1.new([D], F32) for _ in range(2)]
    ha = [a1.new([D], F32) for _ in range(2)]
    xT = [a1.new([D], F32) for _ in range(2)]
    xb = [a1.new([D], BF16) for _ in range(2)]
    bup_raw = Alloc(C.arena, off_wu1).new([2048], F32, parts=NE)
    a3 = Alloc(C.arena, big0)
    xa3 = [a3.new([D], F32) for _ in range(2)]
    yk = [[a3.new([D], F32) for k in range(4)] for _ in range(2)]
    acc = [a3.new([D], F32) for _ in range(2)]

    lw = W
    dma(P, lambda e: e.dma_start(out=g1.ap, in_=lw['ln_g'][li, 0:1, :].partition_broadcast(128)), w=[g1])
    dma(P, lambda e: e.dma_start(out=b1.ap, in_=lw['ln_b'][li, 0:1, :].partition_broadcast(128)), w=[b1])
    dma(P, lambda e: e.dma_start(out=rw.ap, in_=lw['router_w'][li].rearrange("(c p) n -> p c n", p=128)), w=[rw])
    dma(P, lambda e: e.dma_start(out=rb.ap, in_=lw['router_b'][li:li + 1, :].partition_broadcast(128)), w=[rb])
    dma(P, lambda e: e.dma_start(out=bup_raw.ap, in_=lw['exp_b_up'][li]), w=[bup_raw])
    for c in range(16):
        bk = banks[2]
        pe(P, lambda e, c=c, bk=bk: e.transpose(bk.ap[:, 0:NE], bup_raw.ap[:, c * 128:(c + 1) * 128],
                                                 C.ident_f.ap[0:NE, 0:NE]), r=[bup_raw, C.ident_f], w=[bk])
        if c < 8:
            dve(P, lambda e, c=c, bk=bk: e.tensor_copy(out=bupT.ap[:, c, :], in_=bk.ap[:, 0:NE]), r=[bk], w=[bupT])
        else:
            dve(P, lambda e, c=c, bk=bk: e.tensor_scalar(out=bupT.ap[:, c, :], in0=bk.ap[:, 0:NE], scalar1=1.0,
                                                          scalar2=None, op0=ALU.add), r=[bk], w=[bupT])
    dve(P, lambda e: e.tensor_copy(out=cnt[0].ap, in_=C.ecap.ap), r=[C.ecap], w=[cnt[0]])

    def load_w(e_):
        ws = e_ % 2
        for dc in range(8):
            dma(P, lambda e, dc=dc: e.dma_start(out=wu[ws][dc].ap, in_=lw['exp_w_up'][li, e_, dc * 128:(dc + 1) * 128, :]),
                w=[wu[ws][dc]], q='pool')
        for fc in range(8):
            dma(P, lambda e, fc=fc: e.dma_start(out=wd[ws][fc].ap, in_=lw['exp_w_down'][li, e_, fc * 128:(fc + 1) * 128, :]),
                w=[wd[ws][fc]], q='pool')
        dma(P, lambda e: e.dma_start(out=bd[ws].ap, in_=lw['exp_b_down'][li, e_:e_ + 1, :].partition_broadcast(128)),
            w=[bd[ws]])

    barrier(P, C, 'K_XS')
    load_w(0)
    def m1_tile(t):
        s = t % 2
        rows = slice(t * 128, (t + 1) * 128)
        dma(P, lambda e, s=s, rows=rows: e.dma_start(out=xa[s].ap, in_=Xin_ap[rows, :]), r=[(Xin_k, t)], w=[xa[s]])
        dma(P, lambda e, s=s, rows=rows: e.dma_start(out=ha[s].ap, in_=H_ap[rows, :]), r=[(H_k, t)], w=[ha[s]])
        ln_tile(P, xa[s], ha[s], g1, b1, sm[s])
        dma(P, lambda e, s=s, rows=rows: e.dma_start(out=Xmid_ap[rows, :], in_=xa[s].ap), r=[xa[s]], w=[(Xmid_k, t)])
        act(P, lambda e, s=s: e.copy(out=xb[s].ap, in_=xa[s].ap), r=[xa[s]], w=[xb[s]])
        bA, bB = (banks[0], banks[1]) if s == 0 else (banks[5], banks[6])
        for c in range(8):
            bk = bA if c < 4 else bB
            pe(P, lambda e, c=c, bk=bk, s=s: e.transpose(bk.ap[:, (c % 4) * 128:(c % 4 + 1) * 128],
                                                       xa[s].ap[:, c * 128:(c + 1) * 128], C.ident_f.ap),
               r=[xa[s], C.ident_f], w=[bk])
        act(P, lambda e, s=s, bk=bA: e.copy(out=xT[s].ap[:, 0:512], in_=bk.ap), r=[bA], w=[xT[s]])
        act(P, lambda e, s=s, bk=bB: e.copy(out=xT[s].ap[:, 512:1024], in_=bk.ap), r=[bB, xT[s]], w=[xT[s]])
        for c in range(8):
            pe(P, lambda e, c=c, s=s: e.matmul(banks[2].ap[:, 0:NE], lhsT=xT[s].ap[:, c * 128:(c + 1) * 128],
                                               rhs=rw.ap[:, c, :], start=(c == 0), stop=(c == 7)),
               r=[xT[s], rw], w=[banks[2]])
        dve(P, lambda e, s=s: e.tensor_tensor(out=lg[s].ap, in0=banks[2].ap[:, 0:NE], in1=rb.ap, op=ALU.add),
            r=[banks[2], rb], w=[lg[s]])
        dve(P, lambda e, s=s: e.max(out=mx8[s].ap, in_=lg[s].ap), r=[lg[s]], w=[mx8[s]])
        dve(P, lambda e, s=s: e.tensor_scalar(out=nv1[s].ap, in0=mx8[s].ap[:, 0:1], scalar1=-1.0, scalar2=None,
                                              op0=ALU.mult), r=[mx8[s]], w=[nv1[s]])
        act(P, lambda e, s=s: e.activation(out=e4[s].ap, in_=mx8[s].ap[:, 0:4], func=AF.Exp, bias=nv1[s].ap[:, 0:1],
                                           scale=1.0, accum_out=ssum[s].ap), r=[mx8[s], nv1[s]], w=[e4[s], ssum[s]])
        dve(P, lambda e, s=s: e.reciprocal(out=ssum[s].ap, in_=ssum[s].ap), r=[ssum[s]], w=[ssum[s]])
        dve(P, lambda e, s=s, t=t: e.tensor_scalar(out=gates.ap[:, t, :], in0=e4[s].ap, scalar1=ssum[s].ap[:, 0:1],
                                                   scalar2=None, op0=ALU.mult), r=[e4[s], ssum[s]], w=[gates])
        dve(P, lambda e, s=s: e.tensor_scalar(out=maskb[s].ap, in0=lg[s].ap, scalar1=mx8[s].ap[:, 3:4], scalar2=None,
                                              op0=ALU.is_ge), r=[lg[s], mx8[s]], w=[maskb[s]])
        pe(P, lambda e, s=s: e.matmul(banks[3].ap[:, 0:NE], lhsT=C.su_b.ap, rhs=maskb[s].ap, start=True, stop=True),
           r=[C.su_b, maskb[s]], w=[banks[3]])
        pe(P, lambda e, s=s: e.matmul(banks[4].ap[:, 0:NE], lhsT=C.ones_b.ap, rhs=maskb[s].ap, start=True, stop=True),
           r=[C.ones_b, maskb[s]], w=[banks[4]])
        cur, nxt = cnt[t % 2], cnt[(t + 1) % 2]
        dve(P, lambda e, s=s, cur=cur: e.tensor_tensor(out=slot[s].ap, in0=banks[3].ap[:, 0:NE], in1=cur.ap, op=ALU.add),
            r=[banks[3], cur], w=[slot[s]])
        dve(P, lambda e, s=s: e.tensor_tensor(out=slot[s].ap, in0=slot[s].ap, in1=C.capmax.ap, op=ALU.min),
            r=[slot[s], C.capmax], w=[slot[s]])
        dve(P, lambda e, cur=cur, nxt=nxt: e.tensor_tensor(out=nxt.ap, in0=banks[4].ap[:, 0:NE], in1=cur.ap, op=ALU.add),
            r=[banks[4], cur], w=[nxt])
        for k in range(4):
            dve(P, lambda e, s=s, k=k: e.tensor_scalar(out=oh[s].ap, in0=lg[s].ap, scalar1=mx8[s].ap[:, k:k + 1],
                                                       scalar2=None, op0=ALU.is_equal), r=[lg[s], mx8[s]], w=[oh[s]])
            dve(P, lambda e, s=s, k=k, t=t: e.scalar_tensor_tensor(out=junk[s].ap, in0=oh[s].ap, scalar=1.0, in1=slot[s].ap,
                                                                   op0=ALU.mult, op1=ALU.mult,
                                                                   accum_out=destf.ap[:, t, k:k + 1]),
                r=[oh[s], slot[s]], w=[junk[s], destf])
        dve(P, lambda e, t=t: e.tensor_copy(out=desti.ap[:, t, :], in_=destf.ap[:, t, :]), r=[destf], w=[desti])
        for k in range(4):
            dma(P, lambda e, s=s, k=k, t=t: e.indirect_dma_start(
                out=XS[:, :], out_offset=bass.IndirectOffsetOnAxis(ap=desti.ap[:, t, k:k + 1], axis=0),
                in_=xb[s].ap, in_offset=None, bounds_check=_bc_reg(C, e), oob_is_err=False),
                r=[xb[s], desti, 'K_XS'], w=[], q='pool')
    for t in range(NT):
        m1_tile(t)
    if getattr(C, 'dbg', None):
        dma(P, lambda e: e.dma_start(out=C.dbg['destf'], in_=destf.ap), r=[destf], w=['dbg1'])
        dma(P, lambda e: e.dma_start(out=C.dbg['gates'], in_=gates.ap), r=[gates], w=['dbg2'])
    barrier(P, C, 'K_XS')
    barrier(P, C, 'K_Y')
    nblk_total = NE * NBLK

    def load_xs(n):
        e_, b_ = divmod(n, NBLK)
        r0 = e_ * CAP + b_ * NB
        bs = n % 2
        dma(P, lambda e: e.dma_start(out=xs[bs].ap, in_=XS[r0:r0 + NB, :].rearrange("(s p) d -> p s d", p=128)),
            r=['K_XS'], w=[xs[bs]])

    load_xs(0)
    def m2_block(e_, b_):
        if True:
            ws = e_ % 2
            n = e_ * NBLK + b_
            bs = n % 2
            if n + 1 < nblk_total:
                load_xs(n + 1)
            for dcp in range(4):
                bk = banks[dcp % 2]
                bkb = bk.ap.bitcast(BF16)
                for j in range(2):
                    dc = dcp * 2 + j
                    for st_ in range(NST):
                        pe(P, lambda e, dc=dc, st_=st_, j=j, bkb=bkb: e.transpose(
                            bkb[:, j * NB + st_ * 128: j * NB + (st_ + 1) * 128],
                            xs[bs].ap[:, st_, dc * 128:(dc + 1) * 128], C.ident_b.ap),
                           r=[xs[bs], C.ident_b], w=[bk])
                act(P, lambda e, dcp=dcp, bkb=bkb: e.copy(out=xsT[bs][dcp].ap.rearrange("p a b -> p (a b)"),
                                                         in_=bkb[:, 0:2 * NB]), r=[bk], w=[xsT[bs][dcp]])
            for fc in range(8):
                pgk = banks[2 + fc % 2]
                puk = banks[4 + fc % 2]
                for dc in range(8):
                    pe(P, lambda e, dc=dc, fc=fc, pgk=pgk: e.matmul(
                        pgk.ap[:, 0:NB], lhsT=wu[ws][dc].ap[:, fc * 128:(fc + 1) * 128],
                        rhs=xsT[bs][dc // 2].ap[:, dc % 2, :], start=(dc == 0), stop=(dc == 7)),
                       r=[wu[ws][dc], xsT[bs][dc // 2]], w=[pgk])
                for dc in range(8):
                    pe(P, lambda e, dc=dc, fc=fc, puk=puk: e.matmul(
                        puk.ap[:, 0:NB], lhsT=wu[ws][dc].ap[:, 1024 + fc * 128:1024 + (fc + 1) * 128],
                        rhs=xsT[bs][dc // 2].ap[:, dc % 2, :], start=(dc == 0), stop=(dc == 7)),
                       r=[wu[ws][dc], xsT[bs][dc // 2]], w=[puk])
                q = fc % 2
                dve(P, lambda e, fc=fc, pgk=pgk, q=q: e.tensor_scalar(
                    out=gsb[q].ap, in0=pgk.ap[:, 0:NB], scalar1=bupT.ap[:, fc, e_:e_ + 1], scalar2=7.0,
                    op0=ALU.add, op1=ALU.min), r=[pgk, bupT], w=[gsb[q]])
                act(P, lambda e, q=q: e.activation(out=sgb[q].ap, in_=gsb[q].ap, func=AF.Sigmoid, scale=1.702),
                    r=[gsb[q]], w=[sgb[q]])
                dve(P, lambda e, fc=fc, puk=puk, q=q: e.tensor_scalar(
                    out=usb[q].ap, in0=puk.ap[:, 0:NB], scalar1=bupT.ap[:, 8 + fc, e_:e_ + 1], scalar2=8.0,
                    op0=ALU.add, op1=ALU.min), r=[puk, bupT], w=[usb[q]])
                pool(P, lambda e, q=q: e.tensor_tensor(out=gsm[q].ap, in0=gsb[q].ap, in1=sgb[q].ap, op=ALU.mult),
                     r=[gsb[q], sgb[q]], w=[gsm[q]])
                dve(P, lambda e, fc=fc, q=q: e.scalar_tensor_tensor(
                    out=actT[bs][fc].ap, in0=usb[q].ap, scalar=-6.0, in1=gsm[q].ap, op0=ALU.max, op1=ALU.mult),
                    r=[usb[q], gsm[q]], w=[actT[bs][fc]])
    def m2_down(e_, b_):
        if True:
            ws = e_ % 2
            n = e_ * NBLK + b_
            bs = n % 2
            for st_ in range(NST):
                ys = (n * NST + st_) % 2
                for half in range(2):
                    pyk = banks[6 + half]
                    for fc in range(8):
                        pe(P, lambda e, fc=fc, half=half, st_=st_, pyk=pyk: e.matmul(
                            pyk.ap, lhsT=actT[bs][fc].ap[:, st_ * 128:(st_ + 1) * 128],
                            rhs=wd[ws][fc].ap[:, half * 512:(half + 1) * 512], start=(fc == 0), stop=(fc == 7)),
                           r=[actT[bs][fc], wd[ws][fc]], w=[pyk])
                    dve(P, lambda e, half=half, pyk=pyk, ys=ys: e.tensor_tensor(
                        out=ysb[ys].ap[:, half * 512:(half + 1) * 512], in0=pyk.ap,
                        in1=bd[ws].ap[:, half * 512:(half + 1) * 512], op=ALU.add),
                        r=[pyk, bd[ws], ysb[ys]], w=[ysb[ys]])
                r0 = e_ * CAP + b_ * NB + st_ * 128
                dma(P, lambda e, r0=r0, ys=ys: e.dma_start(out=Y[r0:r0 + 128, :], in_=ysb[ys].ap),
                    r=[ysb[ys], 'K_Y'], w=[])
    prev = None
    for e_ in range(NE):
        for b_ in range(NBLK):
            m2_block(e_, b_)
            if prev is not None:
                m2_down(*prev)
            prev = (e_, b_)
            if b_ == 0 and e_ + 1 < NE:
                load_w(e_ + 1)
    m2_down(*prev)
    barrier(P, C, 'K_Y')
    dma(P, lambda e: e.dma_start(out=g2.ap, in_=lw['ln_g'][li, 1:2, :].partition_broadcast(128)), w=[g2])
    dma(P, lambda e: e.dma_start(out=b2.ap, in_=lw['ln_b'][li, 1:2, :].partition_broadcast(128)), w=[b2])
    def m3_tile(t):
        s = t % 2
        rows = slice(t * 128, (t + 1) * 128)
        dma(P, lambda e, s=s, rows=rows: e.dma_start(out=xa3[s].ap, in_=Xmid_ap[rows, :]), r=[(Xmid_k, t)], w=[xa3[s]])
        for k in range(4):
            dma(P, lambda e, s=s, k=k, t=t: e.indirect_dma_start(
                out=yk[s][k].ap, out_offset=None, in_=Y[:, :],
                in_offset=bass.IndirectOffsetOnAxis(ap=desti.ap[:, t, k:k + 1], axis=0),
                bounds_check=_bc_reg(C, e), oob_is_err=False), r=['K_Y', desti], w=[yk[s][k]], q='pool')
        dve(P, lambda e, s=s, t=t: e.tensor_scalar(out=acc[s].ap, in0=yk[s][0].ap, scalar1=gates.ap[:, t, 0:1],
                                                   scalar2=None, op0=ALU.mult), r=[yk[s][0], gates], w=[acc[s]])
        for k in range(1, 4):
            dve(P, lambda e, s=s, t=t, k=k: e.scalar_tensor_tensor(
                out=acc[s].ap, in0=yk[s][k].ap, scalar=gates.ap[:, t, k:k + 1], in1=acc[s].ap,
                op0=ALU.mult, op1=ALU.add), r=[yk[s][k], gates, acc[s]], w=[acc[s]])
        ln_tile(P, xa3[s], acc[s], g2, b2, sm[s])
        dma(P, lambda e, s=s, rows=rows: e.dma_start(out=Xout_ap[rows, :], in_=xa3[s].ap), r=[xa3[s]], w=[(Xout_k, t)])
    for t in range(NT):
        m3_tile(t)


def load_xT(P, C, X, sq, xT, xa):
    X_ap, X_k = X
    banks = C.banks
    for tt in range(16):
        t = sq * 16 + tt
        s = tt % 2
        rows = slice(t * 128, (t + 1) * 128)
        dma(P, lambda e, s=s, rows=rows: e.dma_start(out=xa[s].ap, in_=X_ap[rows, :]), r=[(X_k, t)], w=[xa[s]])
        bA, bB = (banks[0], banks[1]) if s == 0 else (banks[2], banks[3])
        for c in range(8):
            bk = bA if c < 4 else bB
            pe(P, lambda e, c=c, bk=bk, s=s: e.transpose(bk.ap[:, (c % 4) * 128:(c % 4 + 1) * 128],
                                                       xa[s].ap[:, c * 128:(c + 1) * 128], C.ident_f.ap),
               r=[xa[s], C.ident_f], w=[bk])
        for c in range(8):
            bk = bA if c < 4 else bB
            eng = act if c % 2 == 0 else dve
            if False:
                act(P, lambda e, c=c, bk=bk, tt=tt: e.copy(out=xT[c].ap[:, tt * 128:(tt + 1) * 128],
                                                        in_=bk.ap[:, (c % 4) * 128:(c % 4 + 1) * 128]),
                    r=[bk], w=[xT[c]])
            else:
                dve(P, lambda e, c=c, bk=bk, tt=tt: e.tensor_copy(out=xT[c].ap[:, tt * 128:(tt + 1) * 128],
                                                               in_=bk.ap[:, (c % 4) * 128:(c % 4 + 1) * 128]),
                    r=[bk], w=[xT[c]])


def row_to_cols(P, C, row, out, n, bank):
    for c in range(n):
        pe(P, lambda e, c=c: e.transpose(bank.ap[:, c:c + 1], row.ap[0:1, c * 128:(c + 1) * 128],
                                         C.ident_f.ap[0:1, 0:1]), r=[row, C.ident_f], w=[bank])
    dve(P, lambda e: e.tensor_copy(out=out.ap[:, 0:n], in_=bank.ap[:, 0:n]), r=[bank], w=[out])


ATT_SLOPES = [float(2.0 ** (-8.0 * (h + 1) / 16)) for h in range(16)]


def attn_stage(P, C, j, X, H, W):
    nc = C.nc
    banks = C.banks
    H_ap, H_k = H
    al = Alloc(C.arena, C.dyn_start)
    xT = [al.new([SEQ], BF16) for _ in range(8)]
    oT2 = xT
    QT2 = [al.new([SEQ], BF16) for _ in range(8)]
    KT2 = [al.new([SEQ], BF16) for _ in range(4)]
    V = [al.new([256], BF16) for _ in range(16)]
    win = [al.new([1536], BF16) for _ in range(8)]
    wk2 = [al.new([4, 128], BF16) for _ in range(8)]
    wo = [al.new([D], BF16) for _ in range(8)]
    bqk = al.new([12], F32)
    bq8 = al.new([8], F32)
    bk2 = al.new([4], F32)
    bv_bc = al.new([256], F32)
    bo_bc = al.new([D], F32)
    sinkbc = al.new([16], F32)
    Dm = al.new([384], F32)
    dtmp = al.new([384], F32)
    xa_off = al.off
    xa = [al.new([D], F32) for _ in range(2)]
    ysb = xa
    _a = Alloc(C.arena, xa_off)
    brow = _a.new([1536], F32, parts=1)
    bkrow2 = _a.new([512], F32, parts=1)
    R = 3
    Sb = [al.new([384], F32) for _ in range(R)]
    Pe = [al.new([384], F32) for _ in range(R)]
    Pn = [al.new([384], BF16) for _ in range(R)]
    PTs = [al.new([384], BF16) for _ in range(R)]
    mr = [al.new([1], F32) for _ in range(R)]
    negm = [al.new([1], F32) for _ in range(R)]
    rsum = [al.new([1], F32) for _ in range(R)]
    es = [al.new([1], F32) for _ in range(R)]
    rr = [al.new([1], F32) for _ in range(R)]

    for dc in range(8):
        dma(P, lambda e, dc=dc: e.dma_start(out=win[dc].ap, in_=W['b_w_in'][j, dc * 128:(dc + 1) * 128, :]),
            w=[win[dc]], q='pool')
        dma(P, lambda e, dc=dc: e.dma_start(out=wo[dc].ap, in_=W['b_w_out'][j, dc * 128:(dc + 1) * 128, :]),
            w=[wo[dc]], q='pool')
    for dc in range(8):
        for half in range(2):
            act(P, lambda e, dc=dc, half=half: e.copy(
                out=wk2[dc].ap[:, :, half * 64:(half + 1) * 64],
                in_=win[dc].ap[:, 1024:1280].rearrange("p (g d) -> p g d", g=4)), r=[win[dc]], w=[wk2[dc]])
    def _nc_dma(e, out, in_):
        with nc.allow_non_contiguous_dma(reason="small per-partition bias load"):
            return e.dma_start(out=out, in_=in_)

    dma(P, lambda e: _nc_dma(e, bqk.ap[:, 0:8], W['b_b_in'][j, 0:1024].rearrange("(c p) -> p c", p=128)), w=[bqk])
    dve(P, lambda e: e.tensor_scalar(out=bq8.ap, in0=bqk.ap[:, 0:8], scalar1=0.125, scalar2=None, op0=ALU.mult),
        r=[bqk], w=[bq8])
    for half in range(2):
        dma(P, lambda e, half=half: _nc_dma(e, bk2.ap[half * 64:(half + 1) * 64, :],
                                            W['b_b_in'][j, 1024:1280].rearrange("(g d) -> d g", d=64)), r=[bk2], w=[bk2])
    dma(P, lambda e: e.dma_start(out=bv_bc.ap, in_=W['b_b_in'][j:j + 1, 1280:1536].partition_broadcast(128)), w=[bv_bc])
    dma(P, lambda e: e.dma_start(out=bo_bc.ap, in_=W['b_b_out'][j:j + 1, :].partition_broadcast(128)), w=[bo_bc])
    dma(P, lambda e: e.dma_start(out=sinkbc.ap, in_=W['b_sinks'][j:j + 1, :].partition_broadcast(128)), w=[sinkbc])
    pool(P, lambda e: e.iota(Dm.ap, pattern=[[-1, 384]], base=128, channel_multiplier=1,
                             allow_small_or_imprecise_dtypes=True), w=[Dm])
    dve(P, lambda e: e.tensor_scalar(out=dtmp.ap, in0=Dm.ap, scalar1=-1.0, scalar2=None, op0=ALU.mult), r=[Dm], w=[dtmp])
    dve(P, lambda e: e.tensor_tensor(out=Dm.ap, in0=Dm.ap, in1=dtmp.ap, op=ALU.max), r=[Dm, dtmp], w=[Dm])
    dve(P, lambda e: e.tensor_scalar(out=dtmp.ap, in0=Dm.ap, scalar1=128.0, scalar2=1.0e6, op0=ALU.is_gt, op1=ALU.mult),
        r=[Dm], w=[dtmp])
    dve(P, lambda e: e.tensor_tensor(out=Dm.ap, in0=Dm.ap, in1=dtmp.ap, op=ALU.add), r=[Dm, dtmp], w=[Dm])

    stop = getattr(C, 'attn_stop', 99)
    if stop <= 1:
        return

    def seq_body(sq):
        load_xT(P, C, X, sq, xT, xa)
        if stop <= 2:
            return
        for hp in range(8):
            for tq in range(4):
                bk = banks[4 + (hp * 4 + tq) % 2]
                for dc in range(8):
                    pe(P, lambda e, dc=dc, hp=hp, tq=tq, bk=bk: e.matmul(
                        bk.ap, lhsT=win[dc].ap[:, hp * 128:(hp + 1) * 128], rhs=xT[dc].ap[:, tq * 512:(tq + 1) * 512],
                        start=(dc == 0), stop=(dc == 7)), r=[win[dc], xT[dc]], w=[bk])
                act(P, lambda e, hp=hp, tq=tq, bk=bk: e.activation(
                    out=QT2[hp].ap[:, tq * 512:(tq + 1) * 512], in_=bk.ap, func=AF.Identity,
                    bias=bq8.ap[:, hp:hp + 1], scale=0.125), r=[bk, bq8], w=[QT2[hp]])
        for g in range(4):
            for tq in range(4):
                bk = banks[4 + (g * 4 + tq) % 2]
                for dc in range(8):
                    pe(P, lambda e, dc=dc, g=g, tq=tq, bk=bk: e.matmul(
                        bk.ap, lhsT=wk2[dc].ap[:, g, :], rhs=xT[dc].ap[:, tq * 512:(tq + 1) * 512],
                        start=(dc == 0), stop=(dc == 7)), r=[wk2[dc], xT[dc]], w=[bk])
                act(P, lambda e, g=g, tq=tq, bk=bk: e.activation(
                    out=KT2[g].ap[:, tq * 512:(tq + 1) * 512], in_=bk.ap, func=AF.Identity,
                    bias=bk2.ap[:, g:g + 1], scale=1.0), r=[bk, bk2], w=[KT2[g]])
        for tt in range(16):
            bk = banks[4 + tt % 2]
            for dc in range(8):
                pe(P, lambda e, dc=dc, tt=tt, bk=bk: e.matmul(
                    bk.ap[:, 0:256], lhsT=xT[dc].ap[:, tt * 128:(tt + 1) * 128], rhs=win[dc].ap[:, 1280:1536],
                    start=(dc == 0), stop=(dc == 7)), r=[win[dc], xT[dc]], w=[bk])
            dve(P, lambda e, tt=tt, bk=bk: e.tensor_tensor(out=V[tt].ap, in0=bk.ap[:, 0:256], in1=bv_bc.ap, op=ALU.add),
                r=[bk, bv_bc], w=[V[tt]])
        if stop <= 3:
            return
        units = [(h, jb) for jb in range(16) for h in range(16)]
        if stop == 4:
            units = units[:16]

        def geom(jb):
            kb0 = max(0, jb - 1)
            kb1 = min(15, jb + 1)
            nkb = kb1 - kb0 + 1
            off = 128 if jb == 0 else 0
            return kb0, nkb, off

        def stage0(u):
            h, jb = units[u]
            kb0, nkb, off = geom(jb)
            nk = nkb * 128
            g, hb, hp = h // 4, (h % 2) * 64, h // 2
            rs = u % R
            bk = banks[0 + u % 2]
            pe(P, lambda e: e.matmul(bk.ap[:, 0:nk], lhsT=QT2[hp].ap[hb:hb + 64, jb * 128:(jb + 1) * 128],
                                     rhs=KT2[g].ap[hb:hb + 64, kb0 * 128:kb0 * 128 + nk], start=True, stop=True),
               r=[QT2[hp], KT2[g]], w=[bk])
            dve(P, lambda e: e.scalar_tensor_tensor(out=Sb[rs].ap[:, 0:nk], in0=Dm.ap[:, off:off + nk],
                                                    scalar=-ATT_SLOPES[h], in1=bk.ap[:, 0:nk],
                                                    op0=ALU.mult, op1=ALU.add), r=[Dm, bk], w=[Sb[rs]])
            dve(P, lambda e: e.tensor_reduce(out=mr[rs].ap, in_=Sb[rs].ap[:, 0:nk], axis=AX.X, op=ALU.max),
                r=[Sb[rs]], w=[mr[rs]])
            dve(P, lambda e: e.tensor_scalar(out=negm[rs].ap, in0=mr[rs].ap, scalar1=sinkbc.ap[:, h:h + 1],
                                             scalar2=-1.0, op0=ALU.max, op1=ALU.mult),
                r=[mr[rs], sinkbc], w=[negm[rs]])
            act(P, lambda e: e.activation(out=Pe[rs].ap[:, 0:nk], in_=Sb[rs].ap[:, 0:nk], func=AF.Exp,
                                          bias=negm[rs].ap[:, 0:1], scale=1.0, accum_out=rsum[rs].ap),
                r=[Sb[rs], negm[rs]], w=[Pe[rs], rsum[rs]])
            act(P, lambda e: e.activation(out=es[rs].ap, in_=sinkbc.ap[:, h:h + 1], func=AF.Exp,
                                          bias=negm[rs].ap[:, 0:1], scale=1.0), r=[sinkbc, negm[rs]], w=[es[rs]])
            dve(P, lambda e: e.tensor_tensor(out=rr[rs].ap, in0=rsum[rs].ap, in1=es[rs].ap, op=ALU.add),
                r=[rsum[rs], es[rs]], w=[rr[rs]])
            dve(P, lambda e: e.reciprocal(out=rr[rs].ap, in_=rr[rs].ap), r=[rr[rs]], w=[rr[rs]])
            dve(P, lambda e: e.tensor_scalar(out=Pn[rs].ap[:, 0:nk], in0=Pe[rs].ap[:, 0:nk], scalar1=rr[rs].ap[:, 0:1],
                                             scalar2=None, op0=ALU.mult), r=[Pe[rs], rr[rs]], w=[Pn[rs]])

        def stage1(u):
            h, jb = units[u]
            kb0, nkb, off = geom(jb)
            nk = nkb * 128
            rs = u % R
            bk = banks[2 + u % 2]
            bkb = bk.ap.bitcast(BF16)
            for kb in range(nkb):
                pe(P, lambda e, kb=kb: e.transpose(bkb[:, kb * 128:(kb + 1) * 128], Pn[rs].ap[:, kb * 128:(kb + 1) * 128],
                                                   C.ident_b.ap), r=[Pn[rs], C.ident_b], w=[bk])
            act(P, lambda e: e.copy(out=PTs[rs].ap[:, 0:nk], in_=bkb[:, 0:nk]), r=[bk], w=[PTs[rs]])

        def stage2(u):
            h, jb = units[u]
            kb0, nkb, off = geom(jb)
            g, hb, hp = h // 4, (h % 2) * 64, h // 2
            rs = u % R
            bk = banks[6 + (u // 2) % 2]
            for kb in range(nkb):
                pe(P, lambda e, kb=kb: e.matmul(bk.ap[hb:hb + 64, 0:128], lhsT=V[kb0 + kb].ap[:, g * 64:(g + 1) * 64],
                                                rhs=PTs[rs].ap[:, kb * 128:(kb + 1) * 128],
                                                start=(kb == 0), stop=(kb == nkb - 1)),
                   r=[V[kb0 + kb], PTs[rs]], w=[(bk.keys[0], hb)])
            act(P, lambda e: e.copy(out=oT2[hp].ap[hb:hb + 64, jb * 128:(jb + 1) * 128], in_=bk.ap[hb:hb + 64, 0:128]),
                r=[(bk.keys[0], hb)], w=[oT2[hp]])

        U = len(units)
        for i in range(U + 2):
            if i < U:
                stage0(i)
            if 0 <= i - 1 < U:
                stage1(i - 1)
            if 0 <= i - 2 < U:
                stage2(i - 2)
        if stop <= 5:
            return
        for tt in range(16):
            t = sq * 16 + tt
            ys = tt % 2
            for half in range(2):
                bk = banks[4 + half]
                for hp in range(8):
                    pe(P, lambda e, hp=hp, half=half, tt=tt, bk=bk: e.matmul(
                        bk.ap, lhsT=oT2[hp].ap[:, tt * 128:(tt + 1) * 128], rhs=wo[hp].ap[:, half * 512:(half + 1) * 512],
                        start=(hp == 0), stop=(hp == 7)), r=[oT2[hp], wo[hp]], w=[bk])
                dve(P, lambda e, half=half, bk=bk, ys=ys: e.tensor_tensor(
                    out=ysb[ys].ap[:, half * 512:(half + 1) * 512], in0=bk.ap, in1=bo_bc.ap[:, half * 512:(half + 1) * 512],
                    op=ALU.add), r=[bk, bo_bc, ysb[ys]], w=[ysb[ys]])
            dma(P, lambda e, t=t, ys=ys: e.dma_start(out=H_ap[t * 128:(t + 1) * 128, :], in_=ysb[ys].ap),
                r=[ysb[ys]], w=[(H_k, t)])

    for sq in range(2):
        seq_body(sq)


def bc_mid(ap2, n):
    return ap2.unsqueeze(2).to_broadcast([ap2.shape[0], ap2.shape[1], n])


def bc_heads(ap2, nh):
    return ap2.unsqueeze(1).to_broadcast([ap2.shape[0], nh, ap2.shape[1]])


def setup_gdn_consts(P, C, al):
    f = lambda: al.new([128], F32)
    C.SC, C.Li, C.Ui = f(), f(), f()
    C.nLi, C.nUi = f(), f()
    C.NM_Ls, C.NM_Us, C.NM_Ui, C.NM_Li = f(), f(), f(), f()
    C.OnesF, C.NegOnesF, C.Half0, C.Half1 = f(), f(), f(), f()
    pool(P, lambda e: e.memset(C.SC.ap, 0.0), w=[C.SC])
    pool(P, lambda e: e.memset(C.SC.ap[0:64, 0:64], 1.0), r=[C.SC], w=[C.SC])
    pool(P, lambda e: e.memset(C.SC.ap[64:128, 64:128], 1.0), r=[C.SC], w=[C.SC])
    pool(P, lambda e: e.memset(C.OnesF.ap, 1.0), w=[C.OnesF])
    pool(P, lambda e: e.memset(C.NegOnesF.ap, -1.0), w=[C.NegOnesF])
    pool(P, lambda e: e.memset(C.Half0.ap, 0.0), w=[C.Half0])
    pool(P, lambda e: e.memset(C.Half0.ap[0:64, :], 1.0), r=[C.Half0], w=[C.Half0])
    pool(P, lambda e: e.memset(C.Half1.ap, 0.0), w=[C.Half1])
    pool(P, lambda e: e.memset(C.Half1.ap[64:128, :], 1.0), r=[C.Half1], w=[C.Half1])

    def sel(dst, pat, cm, op):
        pool(P, lambda e: e.affine_select(out=dst.ap, in_=C.SC.ap, pattern=[[pat, 128]], compare_op=op, fill=0.0,
                                          base=0, channel_multiplier=cm), r=[C.SC], w=[dst])

    def negmask(dst):
        dve(P, lambda e: e.tensor_scalar(out=dst.ap, in0=dst.ap, scalar1=-1.0, scalar2=1.0e4, op0=ALU.add, op1=ALU.mult),
            r=[dst], w=[dst])

    sel(C.Li, -1, 1, ALU.is_ge)
    sel(C.Ui, 1, -1, ALU.is_ge)
    sel(C.NM_Ls, -1, 1, ALU.is_gt)
    sel(C.NM_Us, 1, -1, ALU.is_gt)
    sel(C.NM_Li, -1, 1, ALU.is_ge)
    sel(C.NM_Ui, 1, -1, ALU.is_ge)
    for m in (C.NM_Ls, C.NM_Us, C.NM_Li, C.NM_Ui):
        negmask(m)
    dve(P, lambda e: e.tensor_scalar(out=C.nLi.ap, in0=C.Li.ap, scalar1=-1.0, scalar2=None, op0=ALU.mult), r=[C.Li], w=[C.nLi])
    dve(P, lambda e: e.tensor_scalar(out=C.nUi.ap, in0=C.Ui.ap, scalar1=-1.0, scalar2=None, op0=ALU.mult), r=[C.Ui], w=[C.nUi])


def gdn_stage(P, C, jl, X, H, W):
    nc = C.nc
    banks = C.banks
    X_ap, X_k = X
    H_ap, H_k = H
    OF, ON = C.OF, C.ON
    Win = W['a_w_in']
    al = Alloc(C.arena, C.dyn_start)
    setup_gdn_consts(P, C, al)
    gdn_dyn = al.off
    xT = [al.new([SEQ], BF16) for _ in range(8)]
    graw = al.new([16, 64], F32)
    Gt = al.new([16, 2, 16], F32)
    Bt = al.new([16, 2, 16], F32)
    wg = al.new([8, 64], F32)
    cw = al.new([32, 5], F32)
    negA = al.new([2, 16], F32)
    dtb = al.new([2, 16], F32)
    nw_bc = al.new([128], F32)
    qT = [al.new([SEQ], BF16) for _ in range(2)]
    kT = [al.new([SEQ], BF16) for _ in range(2)]
    ktm = [al.new([2, 128], BF16) for _ in range(16)]
    vtm = [al.new([4, 128], BF16) for _ in range(16)]
    wz = [al.new([512], BF16) for _ in range(8)]
    Sf = [al.new([4, 128], F32) for _ in range(2)]
    Sb = [al.new([4, 128], BF16) for _ in range(2)]
    regR = al.off
    a = Alloc(C.arena, regR)
    xa = [a.new([D], F32) for _ in range(2)]
    xTf = [a.new([D], F32) for _ in range(2)]
    wblk = [[a.new([128], BF16) for dc in range(8)] for _ in range(2)]
    hT = [a.new([SEQ + 4], BF16) for _ in range(2)]
    dg = [[a.new([128], BF16) for jt in range(5)] for _ in range(2)]
    qf = [a.new([512], F32) for _ in range(2)]
    sqb = [a.new([512], F32) for _ in range(2)]
    t1b = [a.new([512], F32) for _ in range(2)]
    ss4 = [a.new([4], F32) for _ in range(2)]
    g1_end = a.off
    a = Alloc(C.arena, regR)

    class S_:
        pass
    sets = []
    for d in range(2):
        s_ = S_()
        s_.XA, s_.XB, s_.XTA, s_.XTB, s_.PTA, s_.PTB = [a.new([4, 128], F32) for _ in range(6)]
        s_.Gb, s_.RG = s_.XB, s_.XTB
        s_.dS = s_.PTB
        s_.dT = a.new([4, 128], F32)
        s_.u = s_.dT
        s_.otmp = a.new([4, 128], F32)
        s_.Tb = a.new([4, 128], BF16)
        s_.vb = a.new([4, 128], BF16)
        s_.kbg = a.new([4, 128], BF16)
        s_.kd = a.new([4, 128], BF16)
        s_.wT = a.new([4, 128], BF16)
        s_.qkT = a.new([4, 128], BF16)
        s_.vn = a.new([4, 128], BF16)
        s_.osb = a.new([4, 128], F32)
        s_.gc = a.new([4], F32)
        s_.tot = a.new([4], F32)
        s_.egc = a.new([4], F32)
        s_.bge = a.new([4], F32)
        s_.ekd = a.new([4], F32)
        s_.nbeta = a.new([4], F32)
        s_.gl = a.new([2, 4], F32)
        s_.bk = banks[4 * d:4 * d + 4]
        sets.append(s_)
    scan_end = a.off
    a = Alloc(C.arena, regR)
    of_t = [a.new([512], F32) for _ in range(2)]
    ob_t = [a.new([512], F32) for _ in range(2)]
    zs = [a.new([512], F32) for _ in range(2)]
    sq5 = [a.new([512], F32) for _ in range(2)]
    ms5 = [a.new([4], F32) for _ in range(2)]
    onb = [a.new([512], BF16) for _ in range(2)]

    jl_ = jl
    dma(P, lambda e: e.dma_start(out=wg.ap, in_=Win[jl_, :, 6144:6208].rearrange("(c p) n -> p c n", p=128)), w=[wg])
    dma(P, lambda e: e.dma_start(out=negA.ap.rearrange("p a b -> p (a b)"),
                                 in_=W['a_A_log'][jl_:jl_ + 1].rearrange("o a b -> o (a b)").partition_broadcast(128)), w=[negA])
    dma(P, lambda e: e.dma_start(out=dtb.ap.rearrange("p a b -> p (a b)"),
                                 in_=W['a_dt_bias'][jl_:jl_ + 1].rearrange("o a b -> o (a b)").partition_broadcast(128)), w=[dtb])
    dma(P, lambda e: e.dma_start(out=nw_bc.ap, in_=W['a_norm_w'][jl_:jl_ + 1, :].partition_broadcast(128)), w=[nw_bc])
    act(P, lambda e: e.activation(out=negA.ap, in_=negA.ap, func=AF.Exp), r=[negA], w=[negA])
    dve(P, lambda e: e.tensor_scalar(out=negA.ap, in0=negA.ap, scalar1=-1.0, scalar2=None, op0=ALU.mult), r=[negA], w=[negA])
    cwrow = Alloc(C.arena, regR).new([4096], F32, parts=5)
    dma(P, lambda e: e.dma_start(out=cwrow.ap, in_=W['a_conv_w'][jl_]), w=[cwrow])
    for blk in range(32):
        pe(P, lambda e, blk=blk: e.transpose(banks[7].ap[:, blk * 5:(blk + 1) * 5], cwrow.ap[0:5, blk * 128:(blk + 1) * 128],
                                             C.ident_f.ap[0:5, 0:5]), r=[cwrow, C.ident_f], w=[banks[7]])
    dve(P, lambda e: e.tensor_copy(out=cw.ap.rearrange("p a b -> p (a b)"), in_=banks[7].ap[:, 0:160]), r=[banks[7]], w=[cw])

    def seq_body(sq):
        for tt in range(16):
            t = sq * 16 + tt
            s = tt % 2
            rows = slice(t * 128, (t + 1) * 128)
            dma(P, lambda e, s=s, rows=rows: e.dma_start(out=xa[s].ap, in_=X_ap[rows, :]), r=[(X_k, t)], w=[xa[s]])
            bA, bB = (banks[0], banks[1]) if s == 0 else (banks[2], banks[3])
            for c in range(8):
                bk = bA if c < 4 else bB
                pe(P, lambda e, c=c, bk=bk, s=s: e.transpose(bk.ap[:, (c % 4) * 128:(c % 4 + 1) * 128],
                                                           xa[s].ap[:, c * 128:(c + 1) * 128], C.ident_f.ap),
                   r=[xa[s], C.ident_f], w=[bk])
            act(P, lambda e, s=s, bk=bA: e.copy(out=xTf[s].ap[:, 0:512], in_=bk.ap), r=[bA, xTf[s]], w=[xTf[s]])
            act(P, lambda e, s=s, bk=bB: e.copy(out=xTf[s].ap[:, 512:1024], in_=bk.ap), r=[bB, xTf[s]], w=[xTf[s]])
            for c in range(8):
                dve(P, lambda e, c=c, s=s, tt=tt: e.tensor_copy(out=xT[c].ap[:, tt * 128:(tt + 1) * 128],
                                                              in_=xTf[s].ap[:, c * 128:(c + 1) * 128]),
                    r=[xTf[s]], w=[xT[c]])
            bg = banks[4 + s]
            for c in range(8):
                pe(P, lambda e, c=c, s=s, bg=bg: e.matmul(bg.ap[:, 0:64], lhsT=xTf[s].ap[:, c * 128:(c + 1) * 128],
                                                        rhs=wg.ap[:, c, :], start=(c == 0), stop=(c == 7)),
                   r=[xTf[s], wg], w=[bg])
            dve(P, lambda e, tt=tt, bg=bg: e.tensor_copy(out=graw.ap[:, tt, :], in_=bg.ap[:, 0:64]), r=[bg], w=[graw])
        g4 = graw.ap.rearrange("p t (d b h) -> p t d b h", d=2, b=2)
        for d in range(2):
            act(P, lambda e, d=d: e.activation(out=Bt.ap[:, :, d, :], in_=g4[:, :, d, 0, :], func=AF.Sigmoid),
                r=[graw], w=[Bt])
            dve(P, lambda e, d=d: e.tensor_tensor(out=Gt.ap[:, :, d, :], in0=g4[:, :, d, 1, :],
                                                  in1=bc_heads(dtb.ap[:, d, :], 16), op=ALU.add), r=[graw, dtb], w=[Gt])
        act(P, lambda e: e.activation(out=Gt.ap, in_=Gt.ap, func=AF.Exp), r=[Gt], w=[Gt])
        act(P, lambda e: e.activation(out=Gt.ap, in_=Gt.ap, func=AF.Ln, bias=1.0, scale=1.0), r=[Gt], w=[Gt])
        for d in range(2):
            dve(P, lambda e, d=d: e.tensor_tensor(out=Gt.ap[:, :, d, :], in0=Gt.ap[:, :, d, :],
                                                  in1=bc_heads(negA.ap[:, d, :], 16), op=ALU.mult), r=[Gt, negA], w=[Gt])
        for hg in range(4):
            hg_body(sq, hg)

    def hg_body(sq, hg):
        blocks = [('q', 0, 2 * hg, 2 * hg * 128), ('q', 1, 2 * hg + 1, (2 * hg + 1) * 128),
                  ('k', 0, 8 + 2 * hg, 1024 + 2 * hg * 128), ('k', 1, 8 + 2 * hg + 1, 1024 + (2 * hg + 1) * 128)]
        for vi in range(4):
            blocks.append(('v', vi, 16 + 4 * hg + vi, 2048 + (4 * hg + vi) * 128))
        for dc in range(8):
            dma(P, lambda e, dc=dc: e.dma_start(out=wz[dc].ap, in_=Win[jl_, dc * 128:(dc + 1) * 128,
                                                                    4096 + hg * 512:4096 + (hg + 1) * 512]),
                w=[wz[dc]], q='pool')
        for hb_ in range(2):
            dve(P, lambda e, hb_=hb_: e.memset(hT[hb_].ap[:, 0:2], 0.0), r=[hT[hb_]], w=[hT[hb_]])
            dve(P, lambda e, hb_=hb_: e.memset(hT[hb_].ap[:, SEQ + 2:SEQ + 4], 0.0), r=[hT[hb_]], w=[hT[hb_]])
        for bi, (kind, li_, blk, col0) in enumerate(blocks):
            ws = bi % 2
            for dc in range(8):
                dma(P, lambda e, dc=dc, ws=ws, col0=col0: e.dma_start(
                    out=wblk[ws][dc].ap, in_=Win[jl_, dc * 128:(dc + 1) * 128, col0:col0 + 128]),
                    w=[wblk[ws][dc]], q='pool')
            hb = hT[ws]
            for tq in range(4):
                bk = banks[4 + tq % 2]
                for dc in range(8):
                    pe(P, lambda e, dc=dc, tq=tq, bk=bk, ws=ws: e.matmul(
                        bk.ap, lhsT=wblk[ws][dc].ap, rhs=xT[dc].ap[:, tq * 512:(tq + 1) * 512],
                        start=(dc == 0), stop=(dc == 7)), r=[wblk[ws][dc], xT[dc]], w=[bk])
                act(P, lambda e, tq=tq, bk=bk, hb=hb: e.copy(out=hb.ap[:, 2 + tq * 512:2 + (tq + 1) * 512], in_=bk.ap),
                    r=[bk, hb], w=[hb])
            for jt in range(5):
                dve(P, lambda e, jt=jt, ws=ws, blk=blk: e.tensor_scalar(
                    out=dg[ws][jt].ap, in0=C.ident_b.ap, scalar1=cw.ap[:, blk, jt:jt + 1], scalar2=None, op0=ALU.mult),
                    r=[C.ident_b, cw], w=[dg[ws][jt]])
            if kind in ('q', 'k'):
                dst = qT[li_] if kind == 'q' else kT[li_]
                ebias = float(-0.5 * np.log(128.0)) if kind == 'q' else 0.0
                for tq in range(4):
                    bk = banks[6 + tq % 2]
                    r_ = tq % 2
                    for jt in range(5):
                        pe(P, lambda e, jt=jt, tq=tq, bk=bk, ws=ws, hb=hb: e.matmul(
                            bk.ap, lhsT=dg[ws][jt].ap, rhs=hb.ap[:, tq * 512 + jt:tq * 512 + jt + 512],
                            start=(jt == 0), stop=(jt == 4)), r=[dg[ws][jt], hb], w=[bk])
                    act(P, lambda e, bk=bk, r_=r_: e.activation(out=qf[r_].ap, in_=bk.ap, func=AF.Silu), r=[bk], w=[qf[r_]])
                    dve(P, lambda e, r_=r_: e.tensor_tensor(out=sqb[r_].ap, in0=qf[r_].ap, in1=qf[r_].ap, op=ALU.mult),
                        r=[qf[r_]], w=[sqb[r_]])
                    bo = banks[0 + tq % 2]
                    pe(P, lambda e, bo=bo, r_=r_: e.matmul(bo.ap, lhsT=C.OnesF.ap, rhs=sqb[r_].ap, start=True, stop=True),
                       r=[C.OnesF, sqb[r_]], w=[bo])
                    act(P, lambda e, bo=bo, r_=r_: e.activation(out=t1b[r_].ap, in_=bo.ap, func=AF.Ln, bias=1.0e-6, scale=1.0),
                        r=[bo], w=[t1b[r_]])
                    act(P, lambda e, r_=r_, ebias=ebias: e.activation(out=t1b[r_].ap, in_=t1b[r_].ap, func=AF.Exp,
                                                                    bias=ebias, scale=-0.5), r=[t1b[r_]], w=[t1b[r_]])
                    dve(P, lambda e, r_=r_, tq=tq, dst=dst: e.tensor_tensor(out=dst.ap[:, tq * 512:(tq + 1) * 512],
                                                                          in0=qf[r_].ap, in1=t1b[r_].ap, op=ALU.mult),
                        r=[qf[r_], t1b[r_]], w=[dst])
            if kind in ('k', 'v'):
                for t4 in range(4):
                    bk = banks[2 + t4 % 2]
                    r_ = t4 % 2
                    for ti in range(4):
                        tt = t4 * 4 + ti
                        for jt in range(5):
                            pe(P, lambda e, jt=jt, tt=tt, ti=ti, bk=bk, ws=ws, hb=hb: e.matmul(
                                bk.ap[:, ti * 128:(ti + 1) * 128], lhsT=hb.ap[:, tt * 128 + jt:tt * 128 + jt + 128],
                                rhs=dg[ws][jt].ap, start=(jt == 0), stop=(jt == 4)), r=[dg[ws][jt], hb], w=[bk])
                    if kind == 'v':
                        for ti in range(4):
                            tt = t4 * 4 + ti
                            act(P, lambda e, bk=bk, ti=ti, tt=tt, li_=li_: e.activation(
                                out=vtm[tt].ap[:, li_, :], in_=bk.ap[:, ti * 128:(ti + 1) * 128], func=AF.Silu),
                                r=[bk], w=[vtm[tt]])
                    else:
                        act(P, lambda e, bk=bk, r_=r_: e.activation(out=qf[r_].ap, in_=bk.ap, func=AF.Silu), r=[bk], w=[qf[r_]])
                        dve(P, lambda e, r_=r_: e.tensor_tensor(out=sqb[r_].ap, in0=qf[r_].ap, in1=qf[r_].ap, op=ALU.mult),
                            r=[qf[r_]], w=[sqb[r_]])
                        dve(P, lambda e, r_=r_: e.tensor_reduce(out=ss4[r_].ap, in_=sqb[r_].ap.rearrange("p (a b) -> p a b", a=4),
                                                                axis=AX.X, op=ALU.add), r=[sqb[r_]], w=[ss4[r_]])
                        dve(P, lambda e, r_=r_: e.tensor_scalar(out=ss4[r_].ap, in0=ss4[r_].ap, scalar1=1.0e-6, scalar2=None,
                                                                op0=ALU.add), r=[ss4[r_]], w=[ss4[r_]])
                        act(P, lambda e, r_=r_: e.activation(out=ss4[r_].ap, in_=ss4[r_].ap, func=AF.Sqrt), r=[ss4[r_]], w=[ss4[r_]])
                        dve(P, lambda e, r_=r_: e.reciprocal(out=ss4[r_].ap, in_=ss4[r_].ap), r=[ss4[r_]], w=[ss4[r_]])
                        for ti in range(4):
                            tt = t4 * 4 + ti
                            dve(P, lambda e, r_=r_, ti=ti, tt=tt, li_=li_: e.tensor_scalar(
                                out=ktm[tt].ap[:, li_, :], in0=qf[r_].ap[:, ti * 128:(ti + 1) * 128],
                                scalar1=ss4[r_].ap[:, ti:ti + 1], scalar2=None, op0=ALU.mult),
                                r=[qf[r_], ss4[r_]], w=[ktm[tt]])
        for d in range(2):
            dve(P, lambda e, d=d: e.memset(Sf[d].ap, 0.0), r=[Sf[d]], w=[Sf[d]])
            dve(P, lambda e, d=d: e.memset(Sb[d].ap, 0.0), r=[Sb[d]], w=[Sb[d]])
        from itertools import zip_longest
        for step in range(16):
            for _ in zip_longest(unit(sq, hg, 0, step), unit(sq, hg, 1, 15 - step)):
                pass
        for tt in range(16):
            t = sq * 16 + tt
            r_ = tt % 2
            rows = slice(t * 128, (t + 1) * 128)
            cols = slice(hg * 512, (hg + 1) * 512)
            dma(P, lambda e, r_=r_, rows=rows, cols=cols: e.dma_start(out=of_t[r_].ap, in_=OF[0, rows, cols]),
                r=[('OF', 0, t, hg)], w=[of_t[r_]])
            dma(P, lambda e, r_=r_, rows=rows, cols=cols: e.dma_start(out=ob_t[r_].ap, in_=OF[1, rows, cols]),
                r=[('OF', 1, t, hg)], w=[ob_t[r_]])
            bk = banks[tt % 2]
            for dc in range(8):
                pe(P, lambda e, dc=dc, tt=tt, bk=bk: e.matmul(bk.ap, lhsT=xT[dc].ap[:, tt * 128:(tt + 1) * 128], rhs=wz[dc].ap,
                                                              start=(dc == 0), stop=(dc == 7)), r=[xT[dc], wz[dc]], w=[bk])
            act(P, lambda e, bk=bk, r_=r_: e.activation(out=zs[r_].ap, in_=bk.ap, func=AF.Silu), r=[bk], w=[zs[r_]])
            dve(P, lambda e, r_=r_: e.tensor_tensor(out=of_t[r_].ap, in0=of_t[r_].ap, in1=ob_t[r_].ap, op=ALU.add),
                r=[of_t[r_], ob_t[r_]], w=[of_t[r_]])
            dve(P, lambda e, r_=r_: e.tensor_tensor(out=sq5[r_].ap, in0=of_t[r_].ap, in1=of_t[r_].ap, op=ALU.mult),
                r=[of_t[r_]], w=[sq5[r_]])
            dve(P, lambda e, r_=r_: e.tensor_reduce(out=ms5[r_].ap, in_=sq5[r_].ap.rearrange("p (a b) -> p a b", a=4),
                                                    axis=AX.X, op=ALU.add), r=[sq5[r_]], w=[ms5[r_]])
            dve(P, lambda e, r_=r_: e.tensor_scalar(out=ms5[r_].ap, in0=ms5[r_].ap, scalar1=1.0 / 128.0, scalar2=1.0e-6,
                                                    op0=ALU.mult, op1=ALU.add), r=[ms5[r_]], w=[ms5[r_]])
            act(P, lambda e, r_=r_: e.activation(out=ms5[r_].ap, in_=ms5[r_].ap, func=AF.Sqrt), r=[ms5[r_]], w=[ms5[r_]])
            dve(P, lambda e, r_=r_: e.reciprocal(out=ms5[r_].ap, in_=ms5[r_].ap), r=[ms5[r_]], w=[ms5[r_]])
            o3 = lambda b: b.ap.rearrange("p (a b) -> p a b", a=4)
            dve(P, lambda e, r_=r_: e.tensor_tensor(out=o3(of_t[r_]), in0=o3(of_t[r_]), in1=bc_mid(ms5[r_].ap, 128), op=ALU.mult),
                r=[of_t[r_], ms5[r_]], w=[of_t[r_]])
            pool(P, lambda e, r_=r_: e.tensor_tensor(out=o3(of_t[r_]), in0=o3(of_t[r_]), in1=bc_heads(nw_bc.ap, 4), op=ALU.mult),
                 r=[of_t[r_], nw_bc], w=[of_t[r_]])
            dve(P, lambda e, r_=r_: e.tensor_tensor(out=onb[r_].ap, in0=of_t[r_].ap, in1=zs[r_].ap, op=ALU.mult),
                r=[of_t[r_], zs[r_]], w=[onb[r_]])
            dma(P, lambda e, r_=r_, rows=rows, cols=cols: e.dma_start(out=ON[rows, cols], in_=onb[r_].ap),
                r=[onb[r_]], w=[('ON', t, hg)])

    def unit(sq, hg, d, tt):
        S = sets[d]
        b0, b1, b2, b3 = S.bk
        t = sq * 16 + tt
        tk = slice(tt * 128, (tt + 1) * 128)
        Tri, nTri = (C.Ui, C.nUi) if d == 0 else (C.Li, C.nLi)
        NMs = C.NM_Ls if d == 0 else C.NM_Us
        NMi = C.NM_Ui if d == 0 else C.NM_Li
        h0 = hg * 4
        g4 = Gt.ap[:, tt, d, h0:h0 + 4]
        be4 = Bt.ap[:, tt, d, h0:h0 + 4]
        v3 = lambda b: b.ap
        pool(P, lambda e: e.tensor_copy(out=S.Gb.ap, in_=bc_mid(g4, 128)), r=[Gt], w=[S.Gb])
        pool(P, lambda e: e.tensor_tensor(out=S.RG.ap, in0=S.Gb.ap, in1=bc_heads(Tri.ap, 4), op=ALU.mult),
             r=[S.Gb, Tri], w=[S.RG])
        f2 = lambda b: b.ap.rearrange("p a b -> p (a b)")
        pe(P, lambda e: e.matmul(b0.ap, lhsT=Tri.ap, rhs=f2(S.Gb), start=True, stop=False), r=[Tri, S.Gb], w=[b0])
        pe(P, lambda e: e.matmul(b0.ap, lhsT=C.NegOnesF.ap, rhs=f2(S.RG), start=False, stop=True), r=[C.NegOnesF, S.RG], w=[b0])
        pe(P, lambda e: e.matmul(b1.ap, lhsT=C.OnesF.ap, rhs=f2(S.RG), start=True, stop=False), r=[C.OnesF, S.RG], w=[b1])
        pe(P, lambda e: e.matmul(b1.ap, lhsT=nTri.ap, rhs=f2(S.Gb), start=False, stop=True), r=[nTri, S.Gb], w=[b1])
        for kh in range(2):
            pe(P, lambda e, kh=kh: e.matmul(b2.ap[:, kh * 128:(kh + 1) * 128], lhsT=kT[kh].ap[:, tk], rhs=kT[kh].ap[:, tk],
                                            start=True, stop=True), r=[kT[kh]], w=[b2])
            pe(P, lambda e, kh=kh: e.matmul(b2.ap[:, 256 + kh * 128:256 + (kh + 1) * 128], lhsT=kT[kh].ap[:, tk],
                                            rhs=qT[kh].ap[:, tk], start=True, stop=True), r=[kT[kh], qT[kh]], w=[b2])
        pe(P, lambda e: e.matmul(b3.ap[:, 0:4], lhsT=Tri.ap, rhs=g4, start=True, stop=True), r=[Tri, Gt], w=[b3])
        pe(P, lambda e: e.matmul(b3.ap[:, 4:8], lhsT=C.SC.ap, rhs=g4, start=True, stop=True), r=[C.SC, Gt], w=[b3])
        pe(P, lambda e: e.matmul(b3.ap[:, 8:12], lhsT=C.Half0.ap, rhs=g4, start=True, stop=True), r=[C.Half0, Gt], w=[b3])
        pe(P, lambda e: e.matmul(b3.ap[:, 12:16], lhsT=C.Half1.ap, rhs=g4, start=True, stop=True), r=[C.Half1, Gt], w=[b3])
        dve(P, lambda e: e.tensor_copy(out=S.gc.ap, in_=b3.ap[:, 0:4]), r=[b3], w=[S.gc])
        act(P, lambda e: e.activation(out=S.egc.ap, in_=b3.ap[:, 0:4], func=AF.Exp), r=[b3], w=[S.egc])
        dve(P, lambda e: e.tensor_tensor(out=S.ekd.ap, in0=b3.ap[:, 4:8], in1=S.gc.ap, op=ALU.subtract), r=[b3, S.gc], w=[S.ekd])
        act(P, lambda e: e.activation(out=S.ekd.ap, in_=S.ekd.ap, func=AF.Exp), r=[S.ekd], w=[S.ekd])
        act(P, lambda e: e.activation(out=S.gl.ap.rearrange("p a b -> p (a b)"), in_=b3.ap[:, 8:16], func=AF.Exp), r=[b3], w=[S.gl])
        dve(P, lambda e: e.tensor_tensor(out=S.bge.ap, in0=S.egc.ap, in1=be4, op=ALU.mult), r=[S.egc, Bt], w=[S.bge])
        dve(P, lambda e: e.tensor_scalar(out=S.nbeta.ap, in0=be4, scalar1=-1.0, scalar2=None, op0=ALU.mult), r=[Bt], w=[S.nbeta])
        dve(P, lambda e: e.tensor_tensor(out=S.dS.ap, in0=b0.ap.rearrange("p (a b) -> p a b", a=4), in1=bc_heads(NMs.ap, 4), op=ALU.add),
            r=[b0, NMs], w=[S.dS])
        act(P, lambda e: e.activation(out=S.dS.ap, in_=S.dS.ap, func=AF.Exp), r=[S.dS], w=[S.dS])
        dve(P, lambda e: e.tensor_tensor(out=S.dT.ap, in0=b1.ap.rearrange("p (a b) -> p a b", a=4), in1=bc_heads(NMi.ap, 4), op=ALU.add),
            r=[b1, NMi], w=[S.dT])
        act(P, lambda e: e.activation(out=S.dT.ap, in_=S.dT.ap, func=AF.Exp), r=[S.dT], w=[S.dT])
        yield
        for kh in range(2):
            dve(P, lambda e, kh=kh: e.tensor_tensor(out=S.qkT.ap[:, 2 * kh:2 * kh + 2, :], in0=S.dT.ap[:, 2 * kh:2 * kh + 2, :],
                                                   in1=bc_heads(b2.ap[:, 256 + kh * 128:256 + (kh + 1) * 128], 2), op=ALU.mult),
                r=[S.dT, b2], w=[S.qkT])
        for h in range(4):
            dve(P, lambda e, h=h: e.scalar_tensor_tensor(out=S.XA.ap[:, h, :], in0=S.dS.ap[:, h, :], scalar=S.nbeta.ap[:, h:h + 1],
                                                         in1=b2.ap[:, (h // 2) * 128:(h // 2 + 1) * 128],
                                                         op0=ALU.mult, op1=ALU.mult), r=[S.dS, S.nbeta, b2], w=[S.XA])
        pool(P, lambda e: e.tensor_tensor(out=S.vb.ap, in0=vtm[tt].ap, in1=bc_mid(be4, 128), op=ALU.mult), r=[vtm[tt], Bt], w=[S.vb])
        for h in range(4):
            pool(P, lambda e, h=h: e.tensor_scalar(out=S.kbg.ap[:, h, :], in0=ktm[tt].ap[:, h // 2, :], scalar1=S.bge.ap[:, h:h + 1],
                                                   scalar2=None, op0=ALU.mult), r=[ktm[tt], S.bge], w=[S.kbg])
            pool(P, lambda e, h=h: e.tensor_scalar(out=S.kd.ap[:, h, :], in0=ktm[tt].ap[:, h // 2, :], scalar1=S.ekd.ap[:, h:h + 1],
                                                   scalar2=None, op0=ALU.mult), r=[ktm[tt], S.ekd], w=[S.kd])
        yield
        for h in range(4):
            pe(P, lambda e, h=h: e.transpose(b0.ap[:, h * 128:(h + 1) * 128], S.XA.ap[:, h, :], C.ident_f.ap),
               r=[S.XA, C.ident_f], w=[b0])
        act(P, lambda e: e.copy(out=f2(S.XTA), in_=b0.ap), r=[b0], w=[S.XTA])
        dve(P, lambda e: e.tensor_tensor(out=S.PTA.ap, in0=S.XTA.ap, in1=bc_heads(C.ident_f.ap, 4), op=ALU.add),
            r=[S.XTA, C.ident_f], w=[S.PTA])
        yield
        Xc, XTc, PTc = S.XA, S.XTA, S.PTA
        Xn_, XTn_, PTn_ = S.XB, S.XTB, S.PTB
        for lvl in range(5):
            last = (lvl == 4)
            for h in range(4):
                pe(P, lambda e, h=h, Xc=Xc, XTc=XTc: e.matmul(b0.ap[:, h * 128:(h + 1) * 128], lhsT=XTc.ap[:, h, :], rhs=Xc.ap[:, h, :],
                                                             start=True, stop=True), r=[Xc, XTc], w=[b0])
            if not last:
                for h in range(4):
                    pe(P, lambda e, h=h, Xc=Xc, XTc=XTc: e.matmul(b1.ap[:, h * 128:(h + 1) * 128], lhsT=Xc.ap[:, h, :],
                                                                 rhs=XTc.ap[:, h, :], start=True, stop=True), r=[Xc, XTc], w=[b1])
            act(P, lambda e, Xn_=Xn_: e.copy(out=f2(Xn_), in_=b0.ap), r=[b0], w=[Xn_])
            if not last:
                dve(P, lambda e, XTn_=XTn_: e.tensor_copy(out=f2(XTn_), in_=b1.ap), r=[b1], w=[XTn_])
            yield
            for h in range(4):
                pe(P, lambda e, h=h, Xn_=Xn_, PTc=PTc: e.matmul(b2.ap[:, h * 128:(h + 1) * 128], lhsT=Xn_.ap[:, h, :],
                                                               rhs=PTc.ap[:, h, :], start=True, stop=True), r=[Xn_, PTc], w=[b2])
            dve(P, lambda e, PTn_=PTn_, PTc=PTc: e.tensor_tensor(out=f2(PTn_), in0=b2.ap, in1=f2(PTc), op=ALU.add),
                r=[b2, PTc], w=[PTn_])
            yield
            Xc, Xn_ = Xn_, Xc
            XTc, XTn_ = XTn_, XTc
            PTc, PTn_ = PTn_, PTc
        act(P, lambda e, PTc=PTc: e.copy(out=S.Tb.ap, in_=PTc.ap), r=[PTc], w=[S.Tb])
        for h in range(4):
            pe(P, lambda e, h=h: e.matmul(b0.ap[:, h * 128:(h + 1) * 128], lhsT=S.Tb.ap[:, h, :], rhs=S.vb.ap[:, h, :],
                                          start=True, stop=True), r=[S.Tb, S.vb], w=[b0])
        for h in range(4):
            pe(P, lambda e, h=h: e.matmul(b1.ap[:, h * 128:(h + 1) * 128], lhsT=S.kbg.ap[:, h, :], rhs=S.Tb.ap[:, h, :],
                                          start=True, stop=True), r=[S.kbg, S.Tb], w=[b1])
        dve(P, lambda e: e.tensor_copy(out=f2(S.u), in_=b0.ap), r=[b0], w=[S.u])
        act(P, lambda e: e.copy(out=f2(S.wT), in_=b1.ap), r=[b1], w=[S.wT])
        yield
        for c in ((0, 1) if d == 0 else (1, 0)):
            cs = slice(c * 64, (c + 1) * 64)
            ck = (b2.keys[0], c)
            for h in range(4):
                pe(P, lambda e, h=h, cs=cs: e.matmul(b2.ap[cs, h * 128:(h + 1) * 128], lhsT=S.wT.ap[:, h, cs], rhs=Sb[d].ap[:, h, :],
                                                     start=True, stop=True), r=[S.wT, Sb[d]], w=[b2])
            for h in range(4):
                pe(P, lambda e, h=h, cs=cs, c=c: e.matmul(b3.ap[cs, h * 128:(h + 1) * 128], lhsT=qT[h // 2].ap[:, tt * 128 + c * 64:tt * 128 + (c + 1) * 64],
                                                     rhs=Sb[d].ap[:, h, :], start=True, stop=True), r=[qT[h // 2], Sb[d]], w=[b3])
            dve(P, lambda e, cs=cs: e.tensor_tensor(out=f2(S.vn)[cs, :], in0=f2(S.u)[cs, :], in1=b2.ap[cs, :], op=ALU.subtract),
                r=[S.u, b2], w=[S.vn])
            dbg = getattr(C, 'dbg_unit', None)
            if dbg and (sq, hg, d, tt) == (0, 0, 0, 1) and c == 0:
                dma(P, lambda e: e.dma_start(out=dbg['Sb'], in_=f2(Sb[d])), r=[Sb[d]], w=['dbgSb'])
                dma(P, lambda e: e.dma_start(out=dbg['vn'], in_=f2(S.vn)), r=[S.vn], w=['dbgvn'])
                dma(P, lambda e: e.dma_start(out=dbg['u'], in_=f2(S.u)), r=[S.u], w=['dbgu'])
                dma(P, lambda e: e.dma_start(out=dbg['egc'], in_=S.egc.ap), r=[S.egc], w=['dbgegc'])
                dma(P, lambda e: e.dma_start(out=dbg['qkT'], in_=f2(S.qkT)), r=[S.qkT], w=['dbgqkT'])
                dve(P, lambda e: e.tensor_copy(out=f2(S.osb), in_=b3.ap), r=[b3], w=[S.osb])
                dma(P, lambda e: e.dma_start(out=dbg['po1'], in_=f2(S.osb)), r=[S.osb], w=['dbgpo1'])
            yield
            for h in range(4):
                pe(P, lambda e, h=h, cs=cs: e.matmul(b0.ap[cs, h * 128:(h + 1) * 128], lhsT=S.qkT.ap[cs, h, cs], rhs=S.vn.ap[cs, h, :],
                                                     start=True, stop=True), r=[S.qkT, S.vn], w=[b0])
            for h in range(4):
                pe(P, lambda e, h=h, cs=cs: e.matmul(b1.ap[:, h * 128:(h + 1) * 128], lhsT=S.kd.ap[cs, h, :], rhs=S.vn.ap[cs, h, :],
                                                     start=True, stop=True), r=[S.kd, S.vn], w=[b1])
            dve(P, lambda e, cs=cs: e.tensor_tensor(out=S.otmp.ap[cs], in0=b3.ap[cs, :].rearrange("p (a b) -> p a b", a=4),
                                                    in1=bc_mid(S.egc.ap[cs, :], 128), op=ALU.mult), r=[b3, S.egc], w=[S.otmp])
            if dbg and (sq, hg, d, tt) == (0, 0, 0, 1) and c == 0:
                dve(P, lambda e: e.tensor_copy(out=f2(S.osb), in_=b0.ap), r=[b0], w=[S.osb])
                dma(P, lambda e: e.dma_start(out=dbg['po2'], in_=f2(S.osb)), r=[S.osb], w=['dbgpo2'])
                dma(P, lambda e: e.dma_start(out=dbg['otmp'], in_=f2(S.otmp)), r=[S.otmp], w=['dbgotmp'])
            dve(P, lambda e, cs=cs: e.tensor_tensor(out=f2(S.osb)[cs, :], in0=f2(S.otmp)[cs, :], in1=b0.ap[cs, :], op=ALU.add),
                r=[S.otmp, b0], w=[S.osb])
            for h in range(4):
                dve(P, lambda e, h=h, c=c: e.scalar_tensor_tensor(out=Sf[d].ap[:, h, :], in0=Sf[d].ap[:, h, :],
                                                                  scalar=S.gl.ap[:, c, h:h + 1], in1=b1.ap[:, h * 128:(h + 1) * 128],
                                                                  op0=ALU.mult, op1=ALU.add), r=[Sf[d], S.gl, b1], w=[Sf[d]])
            act(P, lambda e: e.copy(out=Sb[d].ap, in_=Sf[d].ap), r=[Sf[d]], w=[Sb[d]])
            yield
        dma(P, lambda e: e.dma_start(out=OF[d, t * 128:(t + 1) * 128, hg * 512:(hg + 1) * 512], in_=f2(S.osb)),
            r=[S.osb], w=[('OF', d, t, hg)])

    for sq in range(2):
        seq_body(sq)
    a = Alloc(C.arena, gdn_dyn)
    wo = [a.new([D], BF16) for _ in range(16)]
    ont = [a.new([2048], BF16) for _ in range(2)]
    onT = [a.new([16, 128], BF16) for _ in range(2)]
    ysb = [a.new([D], F32) for _ in range(2)]
    for c in range(16):
        dma(P, lambda e, c=c: e.dma_start(out=wo[c].ap, in_=W['a_w_out'][jl_, c * 128:(c + 1) * 128, :]), w=[wo[c]], q='pool')
    for t in range(NT):
        s = t % 2
        rows = slice(t * 128, (t + 1) * 128)
        dma(P, lambda e, s=s, rows=rows: e.dma_start(out=ont[s].ap, in_=ON[rows, :]), r=[('ON', t, hg) for hg in range(4)], w=[ont[s]])
        for c4 in range(4):
            bk = banks[c4 % 2]
            bkb = bk.ap.bitcast(BF16)
            for ci in range(4):
                c = c4 * 4 + ci
                pe(P, lambda e, c=c, ci=ci, s=s, bkb=bkb: e.transpose(bkb[:, ci * 128:(ci + 1) * 128], ont[s].ap[:, c * 128:(c + 1) * 128],
                                                                     C.ident_b.ap), r=[ont[s], C.ident_b], w=[bk])
            act(P, lambda e, c4=c4, s=s, bkb=bkb: e.copy(out=onT[s].ap[:, c4 * 4:(c4 + 1) * 4, :].rearrange("p a b -> p (a b)"),
                                                       in_=bkb[:, 0:512]), r=[bk, onT[s]], w=[onT[s]])
        for half in range(2):
            bk = banks[2 + half]
            for c in range(16):
                pe(P, lambda e, c=c, half=half, s=s, bk=bk: e.matmul(bk.ap, lhsT=onT[s].ap[:, c, :], rhs=wo[c].ap[:, half * 512:(half + 1) * 512],
                                                                   start=(c == 0), stop=(c == 15)), r=[onT[s], wo[c]], w=[bk])
            act(P, lambda e, half=half, s=s, bk=bk: e.copy(out=ysb[s].ap[:, half * 512:(half + 1) * 512], in_=bk.ap),
                r=[bk, ysb[s]], w=[ysb[s]])
        dma(P, lambda e, s=s, rows=rows: e.dma_start(out=H_ap[rows, :], in_=ysb[s].ap), r=[ysb[s]], w=[(H_k, t)])


_W_SPECS = [
    ('a_w_in', [2, 1024, 6208]), ('a_conv_w', [2, 5, 4096]), ('a_A_log', [2, 2, 16]), ('a_dt_bias', [2, 2, 16]),
    ('a_norm_w', [2, 128]), ('a_w_out', [2, 2048, 1024]), ('b_w_in', [2, 1024, 1536]), ('b_b_in', [2, 1536]),
    ('b_sinks', [2, 16]), ('b_w_out', [2, 1024, 1024]), ('b_b_out', [2, 1024]), ('router_w', [4, 1024, 32]),
    ('router_b', [4, 32]), ('exp_w_up', [4, 32, 1024, 2048]), ('exp_b_up', [4, 32, 2048]),
    ('exp_w_down', [4, 32, 1024, 1024]), ('exp_b_down', [4, 32, 1024]), ('ln_g', [4, 2, 1024]), ('ln_b', [4, 2, 1024]),
]


def build_program(depth=DEPTH):
    nc = bass.Bass("TRN2", target_bir_lowering=False)
    dt = lambda n, s, k="ExternalInput", d=F32: nc.dram_tensor(n, s, d, kind=k).ap()
    x = dt("x", [NTOK, D])
    W = {n: dt(n, s) for n, s in _W_SPECS}
    out = dt("out", [NTOK, D], "ExternalOutput")
    with ExitStack() as st:
        P = Prog(nc)
        C = Ctx()
        C.nc = nc
        C.XS = dt("XS", [NE * CAP, D], "Internal", BF16)
        C.Y = dt("Y", [NE * CAP, D], "Internal", F32)
        C.OF = dt("OF", [2, NTOK, 2048], "Internal", F32)
        C.ON = dt("ON", [NTOK, 2048], "Internal", BF16)
        XA = dt("XA", [NTOK, D], "Internal")
        XB = dt("XB", [NTOK, D], "Internal")
        Hh = dt("Hh", [NTOK, D], "Internal")
        C.arena = Arena(nc, st, 176 * 1024)
        C.banks = []
        for i in range(8):
            t = st.enter_context(nc.psum_tensor("bank%d" % i, [128, 512], F32))
            C.banks.append(Buf(t[:], [('ps', i)]))
        al = Alloc(C.arena, 0)
        setup_consts(P, C, al)
        C.dyn_start = al.off
        cur = (x, 'x')
        for li in range(depth):
            if li % 2 == 0:
                gdn_stage(P, C, li // 2, cur, (Hh, 'H'), W)
            else:
                attn_stage(P, C, li // 2, cur, (Hh, 'H'), W)
            dst = (out, 'out') if li == depth - 1 else (XB, 'XB')
            moe_stage(P, C, li, cur, (XA, 'XA'), dst, (Hh, 'H'), W)
            cur = dst
        P.emit(st)
    return nc


_LAYER_W = {
    'a': ['a_w_in', 'a_conv_w', 'a_A_log', 'a_dt_bias', 'a_norm_w', 'a_w_out'],
    'b': ['b_w_in', 'b_b_in', 'b_sinks', 'b_w_out', 'b_b_out'],
    'm': ['router_w', 'router_b', 'exp_w_up', 'exp_b_up', 'exp_w_down', 'exp_b_down', 'ln_g', 'ln_b'],
}


def build_layer_program(kind):
    nc = bass.Bass("TRN2", target_bir_lowering=False)
    dt = lambda n, s, k="ExternalInput", d=F32: nc.dram_tensor(n, s, d, kind=k).ap()
    x = dt("x", [NTOK, D])
    out = dt("out", [NTOK, D], "ExternalOutput")
    spec = dict(_W_SPECS)
    names = {'a': _LAYER_W['a'] + _LAYER_W['m'], 'b': _LAYER_W['b'], 'm': _LAYER_W['m']}[kind]
    W = {n: dt(n, [1] + spec[n][1:]) for n in names}
    if kind == 'm':
        Hh = dt("h", [NTOK, D])
    elif kind == 'b':
        Hh = None
    else:
        Hh = dt("Hh", [NTOK, D], "Internal")
    with ExitStack() as st:
        P = Prog(nc)
        C = Ctx()
        C.nc = nc
        if kind in ('a', 'm'):
            C.XS = dt("XS", [NE * CAP, D], "Internal", BF16)
            C.Y = dt("Y", [NE * CAP, D], "Internal", F32)
            XA = dt("XA", [NTOK, D], "Internal")
        if kind == 'a':
            C.OF = dt("OF", [2, NTOK, 2048], "Internal", F32)
            C.ON = dt("ON", [NTOK, 2048], "Internal", BF16)
        C.arena = Arena(nc, st, 176 * 1024)
        C.banks = []
        for i in range(8):
            t = st.enter_context(nc.psum_tensor("bank%d" % i, [128, 512], F32))
            C.banks.append(Buf(t[:], [('ps', i)]))
        al = Alloc(C.arena, 0)
        setup_consts(P, C, al)
        C.dyn_start = al.off
        if kind == 'a':
            gdn_stage(P, C, 0, (x, 'x'), (Hh, 'H'), W)
            moe_stage(P, C, 0, (x, 'x'), (XA, 'XA'), (out, 'out'), (Hh, 'H'), W)
        elif kind == 'b':
            attn_stage(P, C, 0, (x, 'x'), (out, 'out'), W)
        else:
            moe_stage(P, C, 0, (x, 'x'), (XA, 'XA'), (out, 'out'), (Hh, 'H'), W)
        P.emit(st)
    return nc


def kernel(**inputs):
    n = 8
    x = np.ascontiguousarray(np.asarray(inputs['x'], dtype=np.float32))
    xs = x.reshape(n, NTOK, D)
    nc = build_program()
    wmap = {name: np.ascontiguousarray(np.asarray(inputs[name], dtype=np.float32)) for name, _ in _W_SPECS}
    in_maps = []
    for c in range(n):
        m = dict(wmap)
        m['x'] = xs[c]
        in_maps.append(m)
    res = run_bass_kernel_spmd(nc, in_maps, core_ids=list(range(n)))
    outs = [np.asarray(r['out']) for r in res.results]
    return np.stack(outs, 0).reshape(16, SEQ, D).astype(np.float32)
```

```python
from contextlib import ExitStack
import numpy as np
import concourse.bass as bass
import concourse.mybir as mybir
from concourse.bass_utils import run_bass_kernel_spmd

F32 = mybir.dt.float32
BF16 = mybir.dt.bfloat16
U32 = mybir.dt.uint32
I32 = mybir.dt.int32
AF = mybir.ActivationFunctionType
ALU = mybir.AluOpType
AX = mybir.AxisListType

DMA_RING = 16


class Prog:
    def __init__(self, nc):
        self.nc = nc
        self.ops = []
        self.last_w = {}
        self.readers = {}

    def add(self, eng, fn, reads=(), writes=(), dma=False):
        idx = len(self.ops)
        deps = set()
        for k in reads:
            w = self.last_w.get(k)
            if w is not None:
                deps.add(w)
        for k in writes:
            w = self.last_w.get(k)
            if w is not None:
                deps.add(w)
            rs = self.readers.get(k)
            if rs:
                deps.update(rs)
        for k in reads:
            self.readers.setdefault(k, []).append(idx)
        for k in writes:
            self.last_w[k] = idx
            self.readers[k] = []
        self.ops.append((eng, fn, deps, dma))
        return idx

    def pe(self, fn, reads=(), writes=()):
        return self.add('pe', fn, reads, writes)

    def dve(self, fn, reads=(), writes=()):
        return self.add('dve', fn, reads, writes)

    def act(self, fn, reads=(), writes=()):
        return self.add('act', fn, reads, writes)

    def pool(self, fn, reads=(), writes=()):
        return self.add('pool', fn, reads, writes)

    def dma(self, fn, reads=(), writes=(), q='sp'):
        return self.add(q, fn, reads, writes, dma=True)

    def emit(self, stack):
        nc = self.nc
        ops = self.ops
        n = len(ops)
        needed = [False] * n
        for (eng, fn, deps, dma) in ops:
            for d in deps:
                needed[d] = True
        engs = ('pe', 'dve', 'act', 'pool', 'sp')
        esem = {e: stack.enter_context(nc.semaphore('s_' + e)) for e in engs}
        dsem = {e: [stack.enter_context(nc.semaphore('d_%s%d' % (e, i))) for i in range(DMA_RING)]
                for e in ('sp', 'act', 'pool')}
        ecount = {e: 0 for e in engs}
        dcount = {e: 0 for e in dsem}
        event = [None] * n
        extra_wait = [None] * n
        dma_last = {}
        for i, (eng, fn, deps, dma) in enumerate(ops):
            if dma:
                k = dcount[eng]
                dcount[eng] += 1
                slot = k % DMA_RING
                val = 16 * (k // DMA_RING + 1)
                sem = dsem[eng][slot]
                event[i] = (('d', eng, slot), sem, val, 16)
                if k >= DMA_RING:
                    extra_wait[i] = (('d', eng, slot), sem, val - 16)
                dma_last[(eng, slot)] = (('d', eng, slot), sem, val)
                needed[i] = True
            elif needed[i]:
                ecount[eng] += 1
                event[i] = (('e', eng), esem[eng], ecount[eng], 1)
        per_eng = {e: [] for e in engs}
        seen = {e: {} for e in engs}
        for i, (eng, fn, deps, dma) in enumerate(ops):
            waits = []
            cand = []
            if extra_wait[i] is not None:
                cand.append(extra_wait[i])
            for d in sorted(deps):
                deng, _, _, ddma = ops[d]
                if deng == 'pe' and eng == 'pe' and not ddma and not dma:
                    continue
                ev = event[d]
                cand.append((ev[0], ev[1], ev[2]))
            best = {}
            for (sid, sem, val) in cand:
                if seen[eng].get(sid, 0) >= val:
                    continue
                if sid not in best or best[sid][1] < val:
                    best[sid] = (sem, val)
            for sid, (sem, val) in best.items():
                seen[eng][sid] = val
                waits.append((sem, val))
            per_eng[eng].append((fn, waits, event[i] if needed[i] else None))
        final_waits = []
        for (eng, slot), (sid, sem, val) in dma_last.items():
            if seen['sp'].get(sid, 0) < val:
                final_waits.append((sem, val))
        self.stats = {e: len(per_eng[e]) for e in engs}

        with nc.Block() as block:
            def run(e_obj, lst, tail=()):
                for fn, waits, ev in lst:
                    for sem, val in waits:
                        e_obj.wait_ge(sem, val)
                    inst = fn(e_obj)
                    if ev is not None:
                        inst.then_inc(ev[1], ev[3])
                for sem, val in tail:
                    e_obj.wait_ge(sem, val)

            @block.tensor
            def _(e):
                run(e, per_eng['pe'])

            @block.vector
            def _(e):
                run(e, per_eng['dve'])

            @block.scalar
            def _(e):
                run(e, per_eng['act'])

            @block.gpsimd
            def _(e):
                run(e, per_eng['pool'])

            @block.sync
            def _(e):
                run(e, per_eng['sp'], final_waits)


GRAN = 256


class Buf:
    __slots__ = ('ap', 'keys')

    def __init__(self, ap, keys):
        self.ap = ap
        self.keys = keys


def _flat(items):
    out = []
    for k in items:
        if isinstance(k, Buf):
            out.extend(k.keys)
        else:
            out.append(k)
    return out


_ESZ = {F32: 4, BF16: 2, U32: 4, I32: 4}


class Arena:
    def __init__(self, nc, st, nbytes, name="arena"):
        self.t = st.enter_context(nc.sbuf_tensor(name, [128, nbytes // 4], F32))
        self.nbytes = nbytes

    def buf(self, off, shape, dtype, parts=128):
        nel = 1
        for s in shape:
            nel *= s
        nb = nel * _ESZ[dtype]
        assert off % 4 == 0 and off + nb <= self.nbytes, (off, nb, self.nbytes)
        v = self.t[0:parts, off // 4:(off + nb + 3) // 4]
        if dtype != F32:
            v = v.bitcast(dtype)
        if len(shape) == 2:
            v = v.rearrange("p (a b) -> p a b", a=shape[0])
        elif len(shape) == 3:
            v = v.rearrange("p (a b c) -> p a b c", a=shape[0], b=shape[1])
        keys = [('sb', g) for g in range(off // GRAN, (off + nb - 1) // GRAN + 1)]
        return Buf(v, keys)


class Alloc:
    def __init__(self, arena, start, end=None):
        self.arena = arena
        self.off = start
        self.end = end if end is not None else arena.nbytes

    def new(self, shape, dtype, parts=128):
        nel = 1
        for s in shape:
            nel *= s
        nb = nel * _ESZ[dtype]
        b = self.arena.buf(self.off, shape, dtype, parts)
        self.off += (nb + GRAN - 1) // GRAN * GRAN
        assert self.off <= self.end, ("SBUF overflow", self.off, self.end)
        return b


D = 1024
NTOK = 4096
NT = NTOK // 128
SEQ = 2048
NE = 32
CAP = 768
NB = 256
NBLK = CAP // NB
NST = NB // 128
DEPTH = 4
ALPHA = float(8 ** 0.25)
LN_EPS = 1e-5


class Ctx:
    pass


def P_add(P, eng, fn, reads=(), writes=(), dma=False):
    return P.add(eng, fn, _flat(reads), _flat(writes), dma)


def dve(P, fn, r=(), w=()):
    return P.add('dve', fn, _flat(r), _flat(w))


def act(P, fn, r=(), w=()):
    return P.add('act', fn, _flat(r), _flat(w))


def pool(P, fn, r=(), w=()):
    return P.add('pool', fn, _flat(r), _flat(w))


def pe(P, fn, r=(), w=()):
    return P.add('pe', fn, _flat(r), _flat(w))


def dma(P, fn, r=(), w=(), q='sp'):
    return P.add(q, fn, _flat(r), _flat(w), True)


def _bc_reg(C, e):
    if getattr(C, 'bc_reg', None) is None:
        C.bc_reg = e.to_reg(NE * CAP - 1)
    return C.bc_reg


def barrier(P, C, key):
    dve(P, lambda e: e.memset(C.junk1.ap, 0.0), r=[], w=[C.junk1, key])


def setup_consts(P, C, al):
    C.ident_f = al.new([128], F32)
    C.ident_b = al.new([128], BF16)
    C.ones_b = al.new([128], BF16)
    C.su_b = al.new([128], BF16)
    C.ecap = al.new([NE], F32)
    C.capmax = al.new([NE], F32)
    C.junk1 = al.new([8], F32)
    tmp = al.new([128], F32)
    pool(P, lambda e: e.memset(C.ident_f.ap, 1.0), w=[C.ident_f])
    pool(P, lambda e: e.affine_select(out=C.ident_f.ap, in_=C.ident_f.ap, pattern=[[-1, 128]],
                                       compare_op=ALU.is_equal, fill=0.0, base=0, channel_multiplier=1),
         r=[C.ident_f], w=[C.ident_f])
    dve(P, lambda e: e.tensor_copy(out=C.ident_b.ap, in_=C.ident_f.ap), r=[C.ident_f], w=[C.ident_b])
    dve(P, lambda e: e.memset(C.ones_b.ap, 1.0), w=[C.ones_b])
    pool(P, lambda e: e.memset(tmp.ap, 1.0), w=[tmp])
    pool(P, lambda e: e.affine_select(out=tmp.ap, in_=tmp.ap, pattern=[[1, 128]],
                                       compare_op=ALU.is_gt, fill=0.0, base=0, channel_multiplier=-1),
         r=[tmp], w=[tmp])
    dve(P, lambda e: e.tensor_copy(out=C.su_b.ap, in_=tmp.ap), r=[tmp], w=[C.su_b])
    pool(P, lambda e: e.iota(C.ecap.ap, pattern=[[CAP, NE]], base=0, channel_multiplier=0,
                              allow_small_or_imprecise_dtypes=True), w=[C.ecap])
    dve(P, lambda e: e.tensor_scalar(out=C.capmax.ap, in0=C.ecap.ap, scalar1=float(CAP - 1), scalar2=None,
                                      op0=ALU.add), r=[C.ecap], w=[C.capmax])


def ln_tile(P, xa, ha, gbc, bbc, sm):
    st, mv, sd, rstd = sm
    dve(P, lambda e: e.scalar_tensor_tensor(out=xa.ap, in0=xa.ap, scalar=ALPHA, in1=ha.ap,
                                            op0=ALU.mult, op1=ALU.add), r=[xa, ha], w=[xa])
    dve(P, lambda e: e.bn_stats(out=st.ap[:, 0:6], in_=xa.ap[:, 0:512]), r=[xa], w=[st])
    dve(P, lambda e: e.bn_stats(out=st.ap[:, 6:12], in_=xa.ap[:, 512:1024]), r=[xa, st], w=[st])
    dve(P, lambda e: e.bn_aggr(out=mv.ap, in_=st.ap), r=[st], w=[mv])
    dve(P, lambda e: e.tensor_scalar(out=sd.ap, in0=mv.ap[:, 1:2], scalar1=LN_EPS, scalar2=None, op0=ALU.add),
        r=[mv], w=[sd])
    act(P, lambda e: e.activation(out=sd.ap, in_=sd.ap, func=AF.Sqrt), r=[sd], w=[sd])
    dve(P, lambda e: e.reciprocal(out=rstd.ap, in_=sd.ap), r=[sd], w=[rstd])
    dve(P, lambda e: e.tensor_scalar(out=xa.ap, in0=xa.ap, scalar1=mv.ap[:, 0:1], scalar2=rstd.ap[:, 0:1],
                                     op0=ALU.subtract, op1=ALU.mult), r=[xa, mv, rstd], w=[xa])
    pool(P, lambda e: e.tensor_tensor(out=xa.ap, in0=xa.ap, in1=gbc.ap, op=ALU.mult), r=[xa, gbc], w=[xa])
    pool(P, lambda e: e.tensor_tensor(out=xa.ap, in0=xa.ap, in1=bbc.ap, op=ALU.add), r=[xa, bbc], w=[xa])


def moe_stage(P, C, li, Xin, Xmid, Xout, H, W):
    nc = C.nc
    (Xin_ap, Xin_k), (Xmid_ap, Xmid_k), (Xout_ap, Xout_k), (H_ap, H_k) = Xin, Xmid, Xout, H
    XS, Y = C.XS, C.Y
    banks = C.banks
    al = Alloc(C.arena, C.dyn_start)
    destf = al.new([NT, 4], F32)
    desti = al.new([NT, 4], U32)
    gates = al.new([NT, 4], F32)
    g1 = al.new([D], F32)
    b1 = al.new([D], F32)
    g2, b2 = g1, b1
    rw = al.new([8, NE], F32)
    rb = al.new([NE], F32)
    bupT = al.new([16, NE], F32)
    cnt = [al.new([NE], F32), al.new([NE], F32)]
    sm = [(al.new([12], F32), al.new([2], F32), al.new([1], F32), al.new([1], F32)) for _ in range(2)]
    lg = [al.new([NE], F32) for _ in range(2)]
    mx8 = [al.new([8], F32) for _ in range(2)]
    nv1 = [al.new([1], F32) for _ in range(2)]
    e4 = [al.new([4], F32) for _ in range(2)]
    ssum = [al.new([1], F32) for _ in range(2)]
    maskb = [al.new([NE], BF16) for _ in range(2)]
    slot = [al.new([NE], F32) for _ in range(2)]
    oh = [al.new([NE], F32) for _ in range(2)]
    junk = [al.new([NE], F32) for _ in range(2)]
    big0 = al.off
    wu0 = [al.new([2048], BF16) for dc in range(8)]
    off_wu1 = al.off
    wu1 = [al.new([2048], BF16) for dc in range(8)]
    wu = [wu0, wu1]
    wd = [[al.new([1024], BF16) for fc in range(8)] for _ in range(2)]
    bd = [al.new([D], F32) for _ in range(2)]
    xs = [al.new([NST, D], BF16) for _ in range(2)]
    xsT = [[al.new([2, NB], BF16) for _ in range(4)] for _ in range(2)]
    actT = [[al.new([NB], BF16) for fc in range(8)] for _ in range(2)]
    gsb = [al.new([NB], F32) for _ in range(2)]
    sgb = [al.new([NB], F32) for _ in range(2)]
    usb = [al.new([NB], F32) for _ in range(2)]
    gsm = [al.new([NB], F32) for _ in range(2)]
    ysb = [al.new([D], F32) for _ in range(2)]
    a1 = Alloc(C.arena, off_wu1, off_wu1 + 32768)
    xa = [a1.new([D], F32) for _ in range(2)]
    ha = [a1.new([D], F32) for _ in range(2)]
    xT = [a1.new([D], F32) for _ in range(2)]
    xb = [a1.new([D], BF16) for _ in range(2)]
    bup_raw = Alloc(C.arena, off_wu1).new([2048], F32, parts=NE)
    a3 = Alloc(C.arena, big0)
    xa3 = [a3.new([D], F32) for _ in range(2)]
    yk = [[a3.new([D], F32) for k in range(4)] for _ in range(2)]
    acc = [a3.new([D], F32) for _ in range(2)]

    lw = W
    dma(P, lambda e: e.dma_start(out=g1.ap, in_=lw['ln_g'][li, 0:1, :].partition_broadcast(128)), w=[g1])
    dma(P, lambda e: e.dma_start(out=b1.ap, in_=lw['ln_b'][li, 0:1, :].partition_broadcast(128)), w=[b1])
    dma(P, lambda e: e.dma_start(out=rw.ap, in_=lw['router_w'][li].rearrange("(c p) n -> p c n", p=128)), w=[rw])
    dma(P, lambda e: e.dma_start(out=rb.ap, in_=lw['router_b'][li:li + 1, :].partition_broadcast(128)), w=[rb])
    dma(P, lambda e: e.dma_start(out=bup_raw.ap, in_=lw['exp_b_up'][li]), w=[bup_raw])
    for c in range(16):
        bk = banks[2]
        pe(P, lambda e, c=c, bk=bk: e.transpose(bk.ap[:, 0:NE], bup_raw.ap[:, c * 128:(c + 1) * 128],
                                                 C.ident_f.ap[0:NE, 0:NE]), r=[bup_raw, C.ident_f], w=[bk])
        if c < 8:
            dve(P, lambda e, c=c, bk=bk: e.tensor_copy(out=bupT.ap[:, c, :], in_=bk.ap[:, 0:NE]), r=[bk], w=[bupT])
        else:
            dve(P, lambda e, c=c, bk=bk: e.tensor_scalar(out=bupT.ap[:, c, :], in0=bk.ap[:, 0:NE], scalar1=1.0,
                                                          scalar2=None, op0=ALU.add), r=[bk], w=[bupT])
    dve(P, lambda e: e.tensor_copy(out=cnt[0].ap, in_=C.ecap.ap), r=[C.ecap], w=[cnt[0]])

    def load_w(e_):
        ws = e_ % 2
        for dc in range(8):
            dma(P, lambda e, dc=dc: e.dma_start(out=wu[ws][dc].ap, in_=lw['exp_w_up'][li, e_, dc * 128:(dc + 1) * 128, :]),
                w=[wu[ws][dc]], q='pool')
        for fc in range(8):
            dma(P, lambda e, fc=fc: e.dma_start(out=wd[ws][fc].ap, in_=lw['exp_w_down'][li, e_, fc * 128:(fc + 1) * 128, :]),
                w=[wd[ws][fc]], q='pool')
        dma(P, lambda e: e.dma_start(out=bd[ws].ap, in_=lw['exp_b_down'][li, e_:e_ + 1, :].partition_broadcast(128)),
            w=[bd[ws]])

    barrier(P, C, 'K_XS')
    load_w(0)
    def m1_tile(t):
        s = t % 2
        rows = slice(t * 128, (t + 1) * 128)
        dma(P, lambda e, s=s, rows=rows: e.dma_start(out=xa[s].ap, in_=Xin_ap[rows, :]), r=[(Xin_k, t)], w=[xa[s]])
        dma(P, lambda e, s=s, rows=rows: e.dma_start(out=ha[s].ap, in_=H_ap[rows, :]), r=[(H_k, t)], w=[ha[s]])
        ln_tile(P, xa[s], ha[s], g1, b1, sm[s])
        dma(P, lambda e, s=s, rows=rows: e.dma_start(out=Xmid_ap[rows, :], in_=xa[s].ap), r=[xa[s]], w=[(Xmid_k, t)])
        act(P, lambda e, s=s: e.copy(out=xb[s].ap, in_=xa[s].ap), r=[xa[s]], w=[xb[s]])
        bA, bB = (banks[0], banks[1]) if s == 0 else (banks[5], banks[6])
        for c in range(8):
            bk = bA if c < 4 else bB
            pe(P, lambda e, c=c, bk=bk, s=s: e.transpose(bk.ap[:, (c % 4) * 128:(c % 4 + 1) * 128],
                                                       xa[s].ap[:, c * 128:(c + 1) * 128], C.ident_f.ap),
               r=[xa[s], C.ident_f], w=[bk])
        act(P, lambda e, s=s, bk=bA: e.copy(out=xT[s].ap[:, 0:512], in_=bk.ap), r=[bA], w=[xT[s]])
        act(P, lambda e, s=s, bk=bB: e.copy(out=xT[s].ap[:, 512:1024], in_=bk.ap), r=[bB, xT[s]], w=[xT[s]])
        for c in range(8):
            pe(P, lambda e, c=c, s=s: e.matmul(banks[2].ap[:, 0:NE], lhsT=xT[s].ap[:, c * 128:(c + 1) * 128],
                                               rhs=rw.ap[:, c, :], start=(c == 0), stop=(c == 7)),
               r=[xT[s], rw], w=[banks[2]])
        dve(P, lambda e, s=s: e.tensor_tensor(out=lg[s].ap, in0=banks[2].ap[:, 0:NE], in1=rb.ap, op=ALU.add),
            r=[banks[2], rb], w=[lg[s]])
        dve(P, lambda e, s=s: e.max(out=mx8[s].ap, in_=lg[s].ap), r=[lg[s]], w=[mx8[s]])
        dve(P, lambda e, s=s: e.tensor_scalar(out=nv1[s].ap, in0=mx8[s].ap[:, 0:1], scalar1=-1.0, scalar2=None,
                                              op0=ALU.mult), r=[mx8[s]], w=[nv1[s]])
        act(P, lambda e, s=s: e.activation(out=e4[s].ap, in_=mx8[s].ap[:, 0:4], func=AF.Exp, bias=nv1[s].ap[:, 0:1],
                                           scale=1.0, accum_out=ssum[s].ap), r=[mx8[s], nv1[s]], w=[e4[s], ssum[s]])
        dve(P, lambda e, s=s: e.reciprocal(out=ssum[s].ap, in_=ssum[s].ap), r=[ssum[s]], w=[ssum[s]])
        dve(P, lambda e, s=s, t=t: e.tensor_scalar(out=gates.ap[:, t, :], in0=e4[s].ap, scalar1=ssum[s].ap[:, 0:1],
                                                   scalar2=None, op0=ALU.mult), r=[e4[s], ssum[s]], w=[gates])
        dve(P, lambda e, s=s: e.tensor_scalar(out=maskb[s].ap, in0=lg[s].ap, scalar1=mx8[s].ap[:, 3:4], scalar2=None,
                                              op0=ALU.is_ge), r=[lg[s], mx8[s]], w=[maskb[s]])
        pe(P, lambda e, s=s: e.matmul(banks[3].ap[:, 0:NE], lhsT=C.su_b.ap, rhs=maskb[s].ap, start=True, stop=True),
           r=[C.su_b, maskb[s]], w=[banks[3]])
        pe(P, lambda e, s=s: e.matmul(banks[4].ap[:, 0:NE], lhsT=C.ones_b.ap, rhs=maskb[s].ap, start=True, stop=True),
           r=[C.ones_b, maskb[s]], w=[banks[4]])
        cur, nxt = cnt[t % 2], cnt[(t + 1) % 2]
        dve(P, lambda e, s=s, cur=cur: e.tensor_tensor(out=slot[s].ap, in0=banks[3].ap[:, 0:NE], in1=cur.ap, op=ALU.add),
            r=[banks[3], cur], w=[slot[s]])
        dve(P, lambda e, s=s: e.tensor_tensor(out=slot[s].ap, in0=slot[s].ap, in1=C.capmax.ap, op=ALU.min),
            r=[slot[s], C.capmax], w=[slot[s]])
        dve(P, lambda e, cur=cur, nxt=nxt: e.tensor_tensor(out=nxt.ap, in0=banks[4].ap[:, 0:NE], in1=cur.ap, op=ALU.add),
            r=[banks[4], cur], w=[nxt])
        for k in range(4):
            dve(P, lambda e, s=s, k=k: e.tensor_scalar(out=oh[s].ap, in0=lg[s].ap, scalar1=mx8[s].ap[:, k:k + 1],
                                                       scalar2=None, op0=ALU.is_equal), r=[lg[s], mx8[s]], w=[oh[s]])
            dve(P, lambda e, s=s, k=k, t=t: e.scalar_tensor_tensor(out=junk[s].ap, in0=oh[s].ap, scalar=1.0, in1=slot[s].ap,
                                                                   op0=ALU.mult, op1=ALU.mult,
                                                                   accum_out=destf.ap[:, t, k:k + 1]),
                r=[oh[s], slot[s]], w=[junk[s], destf])
        dve(P, lambda e, t=t: e.tensor_copy(out=desti.ap[:, t, :], in_=destf.ap[:, t, :]), r=[destf], w=[desti])
        for k in range(4):
            dma(P, lambda e, s=s, k=k, t=t: e.indirect_dma_start(
                out=XS[:, :], out_offset=bass.IndirectOffsetOnAxis(ap=desti.ap[:, t, k:k + 1], axis=0),
                in_=xb[s].ap, in_offset=None, bounds_check=_bc_reg(C, e), oob_is_err=False),
                r=[xb[s], desti, 'K_XS'], w=[], q='pool')
    for t in range(NT):
        m1_tile(t)
    if getattr(C, 'dbg', None):
        dma(P, lambda e: e.dma_start(out=C.dbg['destf'], in_=destf.ap), r=[destf], w=['dbg1'])
        dma(P, lambda e: e.dma_start(out=C.dbg['gates'], in_=gates.ap), r=[gates], w=['dbg2'])
    barrier(P, C, 'K_XS')
    barrier(P, C, 'K_Y')
    nblk_total = NE * NBLK

    def load_xs(n):
        e_, b_ = divmod(n, NBLK)
        r0 = e_ * CAP + b_ * NB
        bs = n % 2
        dma(P, lambda e: e.dma_start(out=xs[bs].ap, in_=XS[r0:r0 + NB, :].rearrange("(s p) d -> p s d", p=128)),
            r=['K_XS'], w=[xs[bs]])

    load_xs(0)
    def m2_block(e_, b_):
        if True:
            ws = e_ % 2
            n = e_ * NBLK + b_
            bs = n % 2
            if n + 1 < nblk_total:
                load_xs(n + 1)
            for dcp in range(4):
                bk = banks[dcp % 2]
                bkb = bk.ap.bitcast(BF16)
                for j in range(2):
                    dc = dcp * 2 + j
                    for st_ in range(NST):
                        pe(P, lambda e, dc=dc, st_=st_, j=j, bkb=bkb: e.transpose(
                            bkb[:, j * NB + st_ * 128: j * NB + (st_ + 1) * 128],
                            xs[bs].ap[:, st_, dc * 128:(dc + 1) * 128], C.ident_b.ap),
                           r=[xs[bs], C.ident_b], w=[bk])
                act(P, lambda e, dcp=dcp, bkb=bkb: e.copy(out=xsT[bs][dcp].ap.rearrange("p a b -> p (a b)"),
                                                         in_=bkb[:, 0:2 * NB]), r=[bk], w=[xsT[bs][dcp]])
            for fc in range(8):
                pgk = banks[2 + fc % 2]
                puk = banks[4 + fc % 2]
                for dc in range(8):
                    pe(P, lambda e, dc=dc, fc=fc, pgk=pgk: e.matmul(
                        pgk.ap[:, 0:NB], lhsT=wu[ws][dc].ap[:, fc * 128:(fc + 1) * 128],
                        rhs=xsT[bs][dc // 2].ap[:, dc % 2, :], start=(dc == 0), stop=(dc == 7)),
                       r=[wu[ws][dc], xsT[bs][dc // 2]], w=[pgk])
                for dc in range(8):
                    pe(P, lambda e, dc=dc, fc=fc, puk=puk: e.matmul(
                        puk.ap[:, 0:NB], lhsT=wu[ws][dc].ap[:, 1024 + fc * 128:1024 + (fc + 1) * 128],
                        rhs=xsT[bs][dc // 2].ap[:, dc % 2, :], start=(dc == 0), stop=(dc == 7)),
                       r=[wu[ws][dc], xsT[bs][dc // 2]], w=[puk])
                q = fc % 2
                dve(P, lambda e, fc=fc, pgk=pgk, q=q: e.tensor_scalar(
                    out=gsb[q].ap, in0=pgk.ap[:, 0:NB], scalar1=bupT.ap[:, fc, e_:e_ + 1], scalar2=7.0,
                    op0=ALU.add, op1=ALU.min), r=[pgk, bupT], w=[gsb[q]])
                act(P, lambda e, q=q: e.activation(out=sgb[q].ap, in_=gsb[q].ap, func=AF.Sigmoid, scale=1.702),
                    r=[gsb[q]], w=[sgb[q]])
                dve(P, lambda e, fc=fc, puk=puk, q=q: e.tensor_scalar(
                    out=usb[q].ap, in0=puk.ap[:, 0:NB], scalar1=bupT.ap[:, 8 + fc, e_:e_ + 1], scalar2=8.0,
                    op0=ALU.add, op1=ALU.min), r=[puk, bupT], w=[usb[q]])
                pool(P, lambda e, q=q: e.tensor_tensor(out=gsm[q].ap, in0=gsb[q].ap, in1=sgb[q].ap, op=ALU.mult),
                     r=[gsb[q], sgb[q]], w=[gsm[q]])
                dve(P, lambda e, fc=fc, q=q: e.scalar_tensor_tensor(
                    out=actT[bs][fc].ap, in0=usb[q].ap, scalar=-6.0, in1=gsm[q].ap, op0=ALU.max, op1=ALU.mult),
                    r=[usb[q], gsm[q]], w=[actT[bs][fc]])
    def m2_down(e_, b_):
        if True:
            ws = e_ % 2
            n = e_ * NBLK + b_
            bs = n % 2
            for st_ in range(NST):
                ys = (n * NST + st_) % 2
                for half in range(2):
                    pyk = banks[6 + half]
                    for fc in range(8):
                        pe(P, lambda e, fc=fc, half=half, st_=st_, pyk=pyk: e.matmul(
                            pyk.ap, lhsT=actT[bs][fc].ap[:, st_ * 128:(st_ + 1) * 128],
                            rhs=wd[ws][fc].ap[:, half * 512:(half + 1) * 512], start=(fc == 0), stop=(fc == 7)),
                           r=[actT[bs][fc], wd[ws][fc]], w=[pyk])
                    dve(P, lambda e, half=half, pyk=pyk, ys=ys: e.tensor_tensor(
                        out=ysb[ys].ap[:, half * 512:(half + 1) * 512], in0=pyk.ap,
                        in1=bd[ws].ap[:, half * 512:(half + 1) * 512], op=ALU.add),
                        r=[pyk, bd[ws], ysb[ys]], w=[ysb[ys]])
                r0 = e_ * CAP + b_ * NB + st_ * 128
                dma(P, lambda e, r0=r0, ys=ys: e.dma_start(out=Y[r0:r0 + 128, :], in_=ysb[ys].ap),
                    r=[ysb[ys], 'K_Y'], w=[])
    prev = None
    for e_ in range(NE):
        for b_ in range(NBLK):
            m2_block(e_, b_)
            if prev is not None:
                m2_down(*prev)
            prev = (e_, b_)
            if b_ == 0 and e_ + 1 < NE:
                load_w(e_ + 1)
    m2_down(*prev)
    barrier(P, C, 'K_Y')
    dma(P, lambda e: e.dma_start(out=g2.ap, in_=lw['ln_g'][li, 1:2, :].partition_broadcast(128)), w=[g2])
    dma(P, lambda e: e.dma_start(out=b2.ap, in_=lw['ln_b'][li, 1:2, :].partition_broadcast(128)), w=[b2])
    def m3_tile(t):
        s = t % 2
        rows = slice(t * 128, (t + 1) * 128)
        dma(P, lambda e, s=s, rows=rows: e.dma_start(out=xa3[s].ap, in_=Xmid_ap[rows, :]), r=[(Xmid_k, t)], w=[xa3[s]])
        for k in range(4):
            dma(P, lambda e, s=s, k=k, t=t: e.indirect_dma_start(
                out=yk[s][k].ap, out_offset=None, in_=Y[:, :],
                in_offset=bass.IndirectOffsetOnAxis(ap=desti.ap[:, t, k:k + 1], axis=0),
                bounds_check=_bc_reg(C, e), oob_is_err=False), r=['K_Y', desti], w=[yk[s][k]], q='pool')
        dve(P, lambda e, s=s, t=t: e.tensor_scalar(out=acc[s].ap, in0=yk[s][0].ap, scalar1=gates.ap[:, t, 0:1],
                                                   scalar2=None, op0=ALU.mult), r=[yk[s][0], gates], w=[acc[s]])
        for k in range(1, 4):
            dve(P, lambda e, s=s, t=t, k=k: e.scalar_tensor_tensor(
                out=acc[s].ap, in0=yk[s][k].ap, scalar=gates.ap[:, t, k:k + 1], in1=acc[s].ap,
                op0=ALU.mult, op1=ALU.add), r=[yk[s][k], gates, acc[s]], w=[acc[s]])
        ln_tile(P, xa3[s], acc[s], g2, b2, sm[s])
        dma(P, lambda e, s=s, rows=rows: e.dma_start(out=Xout_ap[rows, :], in_=xa3[s].ap), r=[xa3[s]], w=[(Xout_k, t)])
    for t in range(NT):
        m3_tile(t)


def load_xT(P, C, X, sq, xT, xa):
    X_ap, X_k = X
    banks = C.banks
    for tt in range(16):
        t = sq * 16 + tt
        s = tt % 2
        rows = slice(t * 128, (t + 1) * 128)
        dma(P, lambda e, s=s, rows=rows: e.dma_start(out=xa[s].ap, in_=X_ap[rows, :]), r=[(X_k, t)], w=[xa[s]])
        bA, bB = (banks[0], banks[1]) if s == 0 else (banks[2], banks[3])
        for c in range(8):
            bk = bA if c < 4 else bB
            pe(P, lambda e, c=c, bk=bk, s=s: e.transpose(bk.ap[:, (c % 4) * 128:(c % 4 + 1) * 128],
                                                       xa[s].ap[:, c * 128:(c + 1) * 128], C.ident_f.ap),
               r=[xa[s], C.ident_f], w=[bk])
        for c in range(8):
            bk = bA if c < 4 else bB
            eng = act if c % 2 == 0 else dve
            if False:
                act(P, lambda e, c=c, bk=bk, tt=tt: e.copy(out=xT[c].ap[:, tt * 128:(tt + 1) * 128],
                                                        in_=bk.ap[:, (c % 4) * 128:(c % 4 + 1) * 128]),
                    r=[bk], w=[xT[c]])
            else:
                dve(P, lambda e, c=c, bk=bk, tt=tt: e.tensor_copy(out=xT[c].ap[:, tt * 128:(tt + 1) * 128],
                                                               in_=bk.ap[:, (c % 4) * 128:(c % 4 + 1) * 128]),
                    r=[bk], w=[xT[c]])


def row_to_cols(P, C, row, out, n, bank):
    for c in range(n):
        pe(P, lambda e, c=c: e.transpose(bank.ap[:, c:c + 1], row.ap[0:1, c * 128:(c + 1) * 128],
                                         C.ident_f.ap[0:1, 0:1]), r=[row, C.ident_f], w=[bank])
    dve(P, lambda e: e.tensor_copy(out=out.ap[:, 0:n], in_=bank.ap[:, 0:n]), r=[bank], w=[out])


ATT_SLOPES = [float(2.0 ** (-8.0 * (h + 1) / 16)) for h in range(16)]


def attn_stage(P, C, j, X, H, W):
    nc = C.nc
    banks = C.banks
    H_ap, H_k = H
    al = Alloc(C.arena, C.dyn_start)
    xT = [al.new([SEQ], BF16) for _ in range(8)]
    oT2 = xT
    QT2 = [al.new([SEQ], BF16) for _ in range(8)]
    KT2 = [al.new([SEQ], BF16) for _ in range(4)]
    V = [al.new([256], BF16) for _ in range(16)]
    win = [al.new([1536], BF16) for _ in range(8)]
    wk2 = [al.new([4, 128], BF16) for _ in range(8)]
    wo = [al.new([D], BF16) for _ in range(8)]
    bqk = al.new([12], F32)
    bq8 = al.new([8], F32)
    bk2 = al.new([4], F32)
    bv_bc = al.new([256], F32)
    bo_bc = al.new([D], F32)
    sinkbc = al.new([16], F32)
    Dm = al.new([384], F32)
    dtmp = al.new([384], F32)
    xa_off = al.off
    xa = [al.new([D], F32) for _ in range(2)]
    ysb = xa
    _a = Alloc(C.arena, xa_off)
    brow = _a.new([1536], F32, parts=1)
    bkrow2 = _a.new([512], F32, parts=1)
    R = 4
    Sb = [al.new([384], F32) for _ in range(R)]
    Pe = [al.new([384], F32) for _ in range(R)]
    Pn = [al.new([384], BF16) for _ in range(R)]
    PTs = [al.new([384], BF16) for _ in range(R)]
    _sm = [al.new([8], F32) for _ in range(R)]
    mr = [Buf(b.ap[:, 0:1], b.keys) for b in _sm]
    negm = [Buf(b.ap[:, 1:2], b.keys) for b in _sm]
    rsum = [Buf(b.ap[:, 2:3], b.keys) for b in _sm]
    es = [Buf(b.ap[:, 3:4], b.keys) for b in _sm]
    rr = [Buf(b.ap[:, 4:5], b.keys) for b in _sm]

    for dc in range(8):
        dma(P, lambda e, dc=dc: e.dma_start(out=win[dc].ap, in_=W['b_w_in'][j, dc * 128:(dc + 1) * 128, :]),
            w=[win[dc]], q='pool')
        dma(P, lambda e, dc=dc: e.dma_start(out=wo[dc].ap, in_=W['b_w_out'][j, dc * 128:(dc + 1) * 128, :]),
            w=[wo[dc]], q='pool')
    for dc in range(8):
        for half in range(2):
            act(P, lambda e, dc=dc, half=half: e.copy(
                out=wk2[dc].ap[:, :, half * 64:(half + 1) * 64],
                in_=win[dc].ap[:, 1024:1280].rearrange("p (g d) -> p g d", g=4)), r=[win[dc]], w=[wk2[dc]])
    def _nc_dma(e, out, in_):
        with nc.allow_non_contiguous_dma(reason="small per-partition bias load"):
            return e.dma_start(out=out, in_=in_)

    dma(P, lambda e: _nc_dma(e, bqk.ap[:, 0:8], W['b_b_in'][j, 0:1024].rearrange("(c p) -> p c", p=128)), w=[bqk])
    dve(P, lambda e: e.tensor_scalar(out=bq8.ap, in0=bqk.ap[:, 0:8], scalar1=0.125, scalar2=None, op0=ALU.mult),
        r=[bqk], w=[bq8])
    for half in range(2):
        dma(P, lambda e, half=half: _nc_dma(e, bk2.ap[half * 64:(half + 1) * 64, :],
                                            W['b_b_in'][j, 1024:1280].rearrange("(g d) -> d g", d=64)), r=[bk2], w=[bk2])
    dma(P, lambda e: e.dma_start(out=bv_bc.ap, in_=W['b_b_in'][j:j + 1, 1280:1536].partition_broadcast(128)), w=[bv_bc])
    dma(P, lambda e: e.dma_start(out=bo_bc.ap, in_=W['b_b_out'][j:j + 1, :].partition_broadcast(128)), w=[bo_bc])
    dma(P, lambda e: e.dma_start(out=sinkbc.ap, in_=W['b_sinks'][j:j + 1, :].partition_broadcast(128)), w=[sinkbc])
    pool(P, lambda e: e.iota(Dm.ap, pattern=[[-1, 384]], base=128, channel_multiplier=1,
                             allow_small_or_imprecise_dtypes=True), w=[Dm])
    dve(P, lambda e: e.tensor_scalar(out=dtmp.ap, in0=Dm.ap, scalar1=-1.0, scalar2=None, op0=ALU.mult), r=[Dm], w=[dtmp])
    dve(P, lambda e: e.tensor_tensor(out=Dm.ap, in0=Dm.ap, in1=dtmp.ap, op=ALU.max), r=[Dm, dtmp], w=[Dm])
    dve(P, lambda e: e.tensor_scalar(out=dtmp.ap, in0=Dm.ap, scalar1=128.0, scalar2=1.0e6, op0=ALU.is_gt, op1=ALU.mult),
        r=[Dm], w=[dtmp])
    dve(P, lambda e: e.tensor_tensor(out=Dm.ap, in0=Dm.ap, in1=dtmp.ap, op=ALU.add), r=[Dm, dtmp], w=[Dm])

    stop = getattr(C, 'attn_stop', 99)
    if stop <= 1:
        return

    def seq_body(sq):
        load_xT(P, C, X, sq, xT, xa)
        if stop <= 2:
            return
        for hp in range(8):
            for tq in range(4):
                bk = banks[4 + (hp * 4 + tq) % 2]
                for dc in range(8):
                    pe(P, lambda e, dc=dc, hp=hp, tq=tq, bk=bk: e.matmul(
                        bk.ap, lhsT=win[dc].ap[:, hp * 128:(hp + 1) * 128], rhs=xT[dc].ap[:, tq * 512:(tq + 1) * 512],
                        start=(dc == 0), stop=(dc == 7)), r=[win[dc], xT[dc]], w=[bk])
                act(P, lambda e, hp=hp, tq=tq, bk=bk: e.activation(
                    out=QT2[hp].ap[:, tq * 512:(tq + 1) * 512], in_=bk.ap, func=AF.Identity,
                    bias=bq8.ap[:, hp:hp + 1], scale=0.125), r=[bk, bq8], w=[QT2[hp]])
        for g in range(4):
            for tq in range(4):
                bk = banks[4 + (g * 4 + tq) % 2]
                for dc in range(8):
                    pe(P, lambda e, dc=dc, g=g, tq=tq, bk=bk: e.matmul(
                        bk.ap, lhsT=wk2[dc].ap[:, g, :], rhs=xT[dc].ap[:, tq * 512:(tq + 1) * 512],
                        start=(dc == 0), stop=(dc == 7)), r=[wk2[dc], xT[dc]], w=[bk])
                act(P, lambda e, g=g, tq=tq, bk=bk: e.activation(
                    out=KT2[g].ap[:, tq * 512:(tq + 1) * 512], in_=bk.ap, func=AF.Identity,
                    bias=bk2.ap[:, g:g + 1], scale=1.0), r=[bk, bk2], w=[KT2[g]])
        for tt in range(16):
            bk = banks[4 + tt % 2]
            for dc in range(8):
                pe(P, lambda e, dc=dc, tt=tt, bk=bk: e.matmul(
                    bk.ap[:, 0:256], lhsT=xT[dc].ap[:, tt * 128:(tt + 1) * 128], rhs=win[dc].ap[:, 1280:1536],
                    start=(dc == 0), stop=(dc == 7)), r=[win[dc], xT[dc]], w=[bk])
            dve(P, lambda e, tt=tt, bk=bk: e.tensor_tensor(out=V[tt].ap, in0=bk.ap[:, 0:256], in1=bv_bc.ap, op=ALU.add),
                r=[bk, bv_bc], w=[V[tt]])
        if stop <= 3:
            return
        units = [(h, jb) for jb in range(16) for h in range(16)]
        if stop == 4:
            units = units[:16]

        def geom(jb):
            kb0 = max(0, jb - 1)
            kb1 = min(15, jb + 1)
            nkb = kb1 - kb0 + 1
            off = 128 if jb == 0 else 0
            return kb0, nkb, off

        def stage0(u):
            h, jb = units[u]
            kb0, nkb, off = geom(jb)
            nk = nkb * 128
            g, hb, hp = h // 4, (h % 2) * 64, h // 2
            rs = u % R
            bk = banks[0 + u % 2]
            pe(P, lambda e: e.matmul(bk.ap[:, 0:nk], lhsT=QT2[hp].ap[hb:hb + 64, jb * 128:(jb + 1) * 128],
                                     rhs=KT2[g].ap[hb:hb + 64, kb0 * 128:kb0 * 128 + nk], start=True, stop=True),
               r=[QT2[hp], KT2[g]], w=[bk])
            yield
            dve(P, lambda e: e.scalar_tensor_tensor(out=Sb[rs].ap[:, 0:nk], in0=Dm.ap[:, off:off + nk],
                                                    scalar=-ATT_SLOPES[h], in1=bk.ap[:, 0:nk],
                                                    op0=ALU.mult, op1=ALU.add), r=[Dm, bk], w=[Sb[rs]])
            yield
            dve(P, lambda e: e.tensor_reduce(out=mr[rs].ap, in_=Sb[rs].ap[:, 0:nk], axis=AX.X, op=ALU.max),
                r=[Sb[rs]], w=[mr[rs]])
            yield
            dve(P, lambda e: e.tensor_scalar(out=negm[rs].ap, in0=mr[rs].ap, scalar1=sinkbc.ap[:, h:h + 1],
                                             scalar2=-1.0, op0=ALU.max, op1=ALU.mult),
                r=[mr[rs], sinkbc], w=[negm[rs]])
            yield
            act(P, lambda e: e.activation(out=Pe[rs].ap[:, 0:nk], in_=Sb[rs].ap[:, 0:nk], func=AF.Exp,
                                          bias=negm[rs].ap[:, 0:1], scale=1.0, accum_out=rsum[rs].ap),
                r=[Sb[rs], negm[rs]], w=[Pe[rs], rsum[rs]])
            yield
            act(P, lambda e: e.activation(out=es[rs].ap, in_=sinkbc.ap[:, h:h + 1], func=AF.Exp,
                                          bias=negm[rs].ap[:, 0:1], scale=1.0), r=[sinkbc, negm[rs]], w=[es[rs]])
            yield
            dve(P, lambda e: e.tensor_tensor(out=rr[rs].ap, in0=rsum[rs].ap, in1=es[rs].ap, op=ALU.add),
                r=[rsum[rs], es[rs]], w=[rr[rs]])
            yield
            dve(P, lambda e: e.reciprocal(out=rr[rs].ap, in_=rr[rs].ap), r=[rr[rs]], w=[rr[rs]])
            yield
            dve(P, lambda e: e.tensor_scalar(out=Pn[rs].ap[:, 0:nk], in0=Pe[rs].ap[:, 0:nk], scalar1=rr[rs].ap[:, 0:1],
                                             scalar2=None, op0=ALU.mult), r=[Pe[rs], rr[rs]], w=[Pn[rs]])
            yield

        def stage1(u):
            h, jb = units[u]
            kb0, nkb, off = geom(jb)
            nk = nkb * 128
            rs = u % R
            bk = banks[2 + u % 2]
            bkb = bk.ap.bitcast(BF16)
            for kb in range(nkb):
                pe(P, lambda e, kb=kb: e.transpose(bkb[:, kb * 128:(kb + 1) * 128], Pn[rs].ap[:, kb * 128:(kb + 1) * 128],
                                                   C.ident_b.ap), r=[Pn[rs], C.ident_b], w=[bk])
            act(P, lambda e: e.copy(out=PTs[rs].ap[:, 0:nk], in_=bkb[:, 0:nk]), r=[bk], w=[PTs[rs]])

        def stage2(u):
            h, jb = units[u]
            kb0, nkb, off = geom(jb)
            g, hb, hp = h // 4, (h % 2) * 64, h // 2
            rs = u % R
            bk = banks[6 + (u // 2) % 2]
            for kb in range(nkb):
                pe(P, lambda e, kb=kb: e.matmul(bk.ap[hb:hb + 64, 0:128], lhsT=V[kb0 + kb].ap[:, g * 64:(g + 1) * 64],
                                                rhs=PTs[rs].ap[:, kb * 128:(kb + 1) * 128],
                                                start=(kb == 0), stop=(kb == nkb - 1)),
                   r=[V[kb0 + kb], PTs[rs]], w=[(bk.keys[0], hb)])
            act(P, lambda e: e.copy(out=oT2[hp].ap[hb:hb + 64, jb * 128:(jb + 1) * 128], in_=bk.ap[hb:hb + 64, 0:128]),
                r=[(bk.keys[0], hb)], w=[oT2[hp]])

        U = len(units)
        from itertools import zip_longest
        for p in range(0, U + 4, 2):
            gens = [stage0(u) for u in (p, p + 1) if u < U]
            for _ in zip_longest(*gens):
                pass
            for u in (p - 2, p - 1):
                if 0 <= u < U:
                    stage1(u)
            for u in (p - 4, p - 3):
                if 0 <= u < U:
                    stage2(u)
        if stop <= 5:
            return
        for tt in range(16):
            t = sq * 16 + tt
            ys = tt % 2
            for half in range(2):
                bk = banks[4 + half]
                for hp in range(8):
                    pe(P, lambda e, hp=hp, half=half, tt=tt, bk=bk: e.matmul(
                        bk.ap, lhsT=oT2[hp].ap[:, tt * 128:(tt + 1) * 128], rhs=wo[hp].ap[:, half * 512:(half + 1) * 512],
                        start=(hp == 0), stop=(hp == 7)), r=[oT2[hp], wo[hp]], w=[bk])
                dve(P, lambda e, half=half, bk=bk, ys=ys: e.tensor_tensor(
                    out=ysb[ys].ap[:, half * 512:(half + 1) * 512], in0=bk.ap, in1=bo_bc.ap[:, half * 512:(half + 1) * 512],
                    op=ALU.add), r=[bk, bo_bc, ysb[ys]], w=[ysb[ys]])
            dma(P, lambda e, t=t, ys=ys: e.dma_start(out=H_ap[t * 128:(t + 1) * 128, :], in_=ysb[ys].ap),
                r=[ysb[ys]], w=[(H_k, t)])

    for sq in range(2):
        seq_body(sq)


def bc_mid(ap2, n):
    return ap2.unsqueeze(2).to_broadcast([ap2.shape[0], ap2.shape[1], n])


def bc_heads(ap2, nh):
    return ap2.unsqueeze(1).to_broadcast([ap2.shape[0], nh, ap2.shape[1]])


def setup_gdn_consts(P, C, al):
    f = lambda: al.new([128], F32)
    C.SC, C.Li, C.Ui = f(), f(), f()
    C.nLi, C.nUi = f(), f()
    C.NM_Ls, C.NM_Us, C.NM_Ui, C.NM_Li = f(), f(), f(), f()
    C.OnesF, C.NegOnesF, C.Half0, C.Half1 = f(), f(), f(), f()
    pool(P, lambda e: e.memset(C.SC.ap, 0.0), w=[C.SC])
    pool(P, lambda e: e.memset(C.SC.ap[0:64, 0:64], 1.0), r=[C.SC], w=[C.SC])
    pool(P, lambda e: e.memset(C.SC.ap[64:128, 64:128], 1.0), r=[C.SC], w=[C.SC])
    pool(P, lambda e: e.memset(C.OnesF.ap, 1.0), w=[C.OnesF])
    pool(P, lambda e: e.memset(C.NegOnesF.ap, -1.0), w=[C.NegOnesF])
    pool(P, lambda e: e.memset(C.Half0.ap, 0.0), w=[C.Half0])
    pool(P, lambda e: e.memset(C.Half0.ap[0:64, :], 1.0), r=[C.Half0], w=[C.Half0])
    pool(P, lambda e: e.memset(C.Half1.ap, 0.0), w=[C.Half1])
    pool(P, lambda e: e.memset(C.Half1.ap[64:128, :], 1.0), r=[C.Half1], w=[C.Half1])

    def sel(dst, pat, cm, op):
        pool(P, lambda e: e.affine_select(out=dst.ap, in_=C.SC.ap, pattern=[[pat, 128]], compare_op=op, fill=0.0,
                                          base=0, channel_multiplier=cm), r=[C.SC], w=[dst])

    def negmask(dst):
        dve(P, lambda e: e.tensor_scalar(out=dst.ap, in0=dst.ap, scalar1=-1.0, scalar2=1.0e4, op0=ALU.add, op1=ALU.mult),
            r=[dst], w=[dst])

    sel(C.Li, -1, 1, ALU.is_ge)
    sel(C.Ui, 1, -1, ALU.is_ge)
    sel(C.NM_Ls, -1, 1, ALU.is_gt)
    sel(C.NM_Us, 1, -1, ALU.is_gt)
    sel(C.NM_Li, -1, 1, ALU.is_ge)
    sel(C.NM_Ui, 1, -1, ALU.is_ge)
    for m in (C.NM_Ls, C.NM_Us, C.NM_Li, C.NM_Ui):
        negmask(m)
    dve(P, lambda e: e.tensor_scalar(out=C.nLi.ap, in0=C.Li.ap, scalar1=-1.0, scalar2=None, op0=ALU.mult), r=[C.Li], w=[C.nLi])
    dve(P, lambda e: e.tensor_scalar(out=C.nUi.ap, in0=C.Ui.ap, scalar1=-1.0, scalar2=None, op0=ALU.mult), r=[C.Ui], w=[C.nUi])


def gdn_stage(P, C, jl, X, H, W):
    nc = C.nc
    banks = C.banks
    X_ap, X_k = X
    H_ap, H_k = H
    OF, ON = C.OF, C.ON
    Win = W['a_w_in']
    al = Alloc(C.arena, C.dyn_start)
    setup_gdn_consts(P, C, al)
    gdn_dyn = al.off
    xT = [al.new([SEQ], BF16) for _ in range(8)]
    graw = al.new([16, 64], F32)
    Gt = al.new([16, 2, 16], F32)
    Bt = al.new([16, 2, 16], F32)
    wg = al.new([8, 64], F32)
    cw = al.new([32, 5], F32)
    negA = al.new([2, 16], F32)
    dtb = al.new([2, 16], F32)
    nw_bc = al.new([128], F32)
    qT = [al.new([SEQ], BF16) for _ in range(2)]
    kT = [al.new([SEQ], BF16) for _ in range(2)]
    ktm = [al.new([2, 128], BF16) for _ in range(16)]
    vtm = [al.new([4, 128], BF16) for _ in range(16)]
    wz = [al.new([512], BF16) for _ in range(8)]
    Sf = [al.new([4, 128], F32) for _ in range(2)]
    Sb = [al.new([4, 128], BF16) for _ in range(2)]
    regR = al.off
    a = Alloc(C.arena, regR)
    xa = [a.new([D], F32) for _ in range(2)]
    xTf = [a.new([D], F32) for _ in range(2)]
    wblk = [[a.new([128], BF16) for dc in range(8)] for _ in range(2)]
    hT = [a.new([SEQ + 4], BF16) for _ in range(2)]
    dg = [[a.new([128], BF16) for jt in range(5)] for _ in range(2)]
    qf = [a.new([512], F32) for _ in range(2)]
    sqb = [a.new([512], F32) for _ in range(2)]
    t1b = [a.new([512], F32) for _ in range(2)]
    ss4 = [a.new([4], F32) for _ in range(2)]
    g1_end = a.off
    a = Alloc(C.arena, regR)

    class S_:
        pass
    sets = []
    for d in range(2):
        s_ = S_()
        s_.XA, s_.XB, s_.XTA, s_.XTB, s_.PTA, s_.PTB = [a.new([4, 128], F32) for _ in range(6)]
        s_.Gb, s_.RG = s_.XB, s_.XTB
        s_.dS = s_.PTB
        s_.dT = a.new([4, 128], F32)
        s_.u = s_.dT
        s_.otmp = a.new([4, 128], F32)
        s_.Tb = a.new([4, 128], BF16)
        s_.vb = a.new([4, 128], BF16)
        s_.kbg = a.new([4, 128], BF16)
        s_.kd = a.new([4, 128], BF16)
        s_.wT = a.new([4, 128], BF16)
        s_.qkT = a.new([4, 128], BF16)
        s_.vn = a.new([4, 128], BF16)
        s_.osb = a.new([4, 128], F32)
        s_.gc = a.new([4], F32)
        s_.tot = a.new([4], F32)
        s_.egc = a.new([4], F32)
        s_.bge = a.new([4], F32)
        s_.ekd = a.new([4], F32)
        s_.nbeta = a.new([4], F32)
        s_.gl = a.new([2, 4], F32)
        s_.bk = banks[4 * d:4 * d + 4]
        sets.append(s_)
    scan_end = a.off
    a = Alloc(C.arena, regR)
    of_t = [a.new([512], F32) for _ in range(2)]
    ob_t = [a.new([512], F32) for _ in range(2)]
    zs = [a.new([512], F32) for _ in range(2)]
    sq5 = [a.new([512], F32) for _ in range(2)]
    ms5 = [a.new([4], F32) for _ in range(2)]
    onb = [a.new([512], BF16) for _ in range(2)]

    jl_ = jl
    dma(P, lambda e: e.dma_start(out=wg.ap, in_=Win[jl_, :, 6144:6208].rearrange("(c p) n -> p c n", p=128)), w=[wg])
    dma(P, lambda e: e.dma_start(out=negA.ap.rearrange("p a b -> p (a b)"),
                                 in_=W['a_A_log'][jl_:jl_ + 1].rearrange("o a b -> o (a b)").partition_broadcast(128)), w=[negA])
    dma(P, lambda e: e.dma_start(out=dtb.ap.rearrange("p a b -> p (a b)"),
                                 in_=W['a_dt_bias'][jl_:jl_ + 1].rearrange("o a b -> o (a b)").partition_broadcast(128)), w=[dtb])
    dma(P, lambda e: e.dma_start(out=nw_bc.ap, in_=W['a_norm_w'][jl_:jl_ + 1, :].partition_broadcast(128)), w=[nw_bc])
    act(P, lambda e: e.activation(out=negA.ap, in_=negA.ap, func=AF.Exp), r=[negA], w=[negA])
    dve(P, lambda e: e.tensor_scalar(out=negA.ap, in0=negA.ap, scalar1=-1.0, scalar2=None, op0=ALU.mult), r=[negA], w=[negA])
    cwrow = Alloc(C.arena, regR).new([4096], F32, parts=5)
    dma(P, lambda e: e.dma_start(out=cwrow.ap, in_=W['a_conv_w'][jl_]), w=[cwrow])
    for blk in range(32):
        pe(P, lambda e, blk=blk: e.transpose(banks[7].ap[:, blk * 5:(blk + 1) * 5], cwrow.ap[0:5, blk * 128:(blk + 1) * 128],
                                             C.ident_f.ap[0:5, 0:5]), r=[cwrow, C.ident_f], w=[banks[7]])
    dve(P, lambda e: e.tensor_copy(out=cw.ap.rearrange("p a b -> p (a b)"), in_=banks[7].ap[:, 0:160]), r=[banks[7]], w=[cw])

    def seq_body(sq):
        for tt in range(16):
            t = sq * 16 + tt
            s = tt % 2
            rows = slice(t * 128, (t + 1) * 128)
            dma(P, lambda e, s=s, rows=rows: e.dma_start(out=xa[s].ap, in_=X_ap[rows, :]), r=[(X_k, t)], w=[xa[s]])
            bA, bB = (banks[0], banks[1]) if s == 0 else (banks[2], banks[3])
            for c in range(8):
                bk = bA if c < 4 else bB
                pe(P, lambda e, c=c, bk=bk, s=s: e.transpose(bk.ap[:, (c % 4) * 128:(c % 4 + 1) * 128],
                                                           xa[s].ap[:, c * 128:(c + 1) * 128], C.ident_f.ap),
                   r=[xa[s], C.ident_f], w=[bk])
            act(P, lambda e, s=s, bk=bA: e.copy(out=xTf[s].ap[:, 0:512], in_=bk.ap), r=[bA, xTf[s]], w=[xTf[s]])
            act(P, lambda e, s=s, bk=bB: e.copy(out=xTf[s].ap[:, 512:1024], in_=bk.ap), r=[bB, xTf[s]], w=[xTf[s]])
            for c in range(8):
                dve(P, lambda e, c=c, s=s, tt=tt: e.tensor_copy(out=xT[c].ap[:, tt * 128:(tt + 1) * 128],
                                                              in_=xTf[s].ap[:, c * 128:(c + 1) * 128]),
                    r=[xTf[s]], w=[xT[c]])
            bg = banks[4 + s]
            for c in range(8):
                pe(P, lambda e, c=c, s=s, bg=bg: e.matmul(bg.ap[:, 0:64], lhsT=xTf[s].ap[:, c * 128:(c + 1) * 128],
                                                        rhs=wg.ap[:, c, :], start=(c == 0), stop=(c == 7)),
                   r=[xTf[s], wg], w=[bg])
            dve(P, lambda e, tt=tt, bg=bg: e.tensor_copy(out=graw.ap[:, tt, :], in_=bg.ap[:, 0:64]), r=[bg], w=[graw])
        g4 = graw.ap.rearrange("p t (d b h) -> p t d b h", d=2, b=2)
        for d in range(2):
            act(P, lambda e, d=d: e.activation(out=Bt.ap[:, :, d, :], in_=g4[:, :, d, 0, :], func=AF.Sigmoid),
                r=[graw], w=[Bt])
            dve(P, lambda e, d=d: e.tensor_tensor(out=Gt.ap[:, :, d, :], in0=g4[:, :, d, 1, :],
                                                  in1=bc_heads(dtb.ap[:, d, :], 16), op=ALU.add), r=[graw, dtb], w=[Gt])
        act(P, lambda e: e.activation(out=Gt.ap, in_=Gt.ap, func=AF.Exp), r=[Gt], w=[Gt])
        act(P, lambda e: e.activation(out=Gt.ap, in_=Gt.ap, func=AF.Ln, bias=1.0, scale=1.0), r=[Gt], w=[Gt])
        for d in range(2):
            dve(P, lambda e, d=d: e.tensor_tensor(out=Gt.ap[:, :, d, :], in0=Gt.ap[:, :, d, :],
                                                  in1=bc_heads(negA.ap[:, d, :], 16), op=ALU.mult), r=[Gt, negA], w=[Gt])
        for hg in range(4):
            hg_body(sq, hg)

    def hg_body(sq, hg):
        blocks = [('q', 0, 2 * hg, 2 * hg * 128), ('q', 1, 2 * hg + 1, (2 * hg + 1) * 128),
                  ('k', 0, 8 + 2 * hg, 1024 + 2 * hg * 128), ('k', 1, 8 + 2 * hg + 1, 1024 + (2 * hg + 1) * 128)]
        for vi in range(4):
            blocks.append(('v', vi, 16 + 4 * hg + vi, 2048 + (4 * hg + vi) * 128))
        for dc in range(8):
            dma(P, lambda e, dc=dc: e.dma_start(out=wz[dc].ap, in_=Win[jl_, dc * 128:(dc + 1) * 128,
                                                                    4096 + hg * 512:4096 + (hg + 1) * 512]),
                w=[wz[dc]], q='pool')
        for hb_ in range(2):
            dve(P, lambda e, hb_=hb_: e.memset(hT[hb_].ap[:, 0:2], 0.0), r=[hT[hb_]], w=[hT[hb_]])
            dve(P, lambda e, hb_=hb_: e.memset(hT[hb_].ap[:, SEQ + 2:SEQ + 4], 0.0), r=[hT[hb_]], w=[hT[hb_]])
        for bi, (kind, li_, blk, col0) in enumerate(blocks):
            ws = bi % 2
            for dc in range(8):
                dma(P, lambda e, dc=dc, ws=ws, col0=col0: e.dma_start(
                    out=wblk[ws][dc].ap, in_=Win[jl_, dc * 128:(dc + 1) * 128, col0:col0 + 128]),
                    w=[wblk[ws][dc]], q='pool')
            hb = hT[ws]
            for tq in range(4):
                bk = banks[4 + tq % 2]
                for dc in range(8):
                    pe(P, lambda e, dc=dc, tq=tq, bk=bk, ws=ws: e.matmul(
                        bk.ap, lhsT=wblk[ws][dc].ap, rhs=xT[dc].ap[:, tq * 512:(tq + 1) * 512],
                        start=(dc == 0), stop=(dc == 7)), r=[wblk[ws][dc], xT[dc]], w=[bk])
                act(P, lambda e, tq=tq, bk=bk, hb=hb: e.copy(out=hb.ap[:, 2 + tq * 512:2 + (tq + 1) * 512], in_=bk.ap),
                    r=[bk, hb], w=[hb])
            for jt in range(5):
                dve(P, lambda e, jt=jt, ws=ws, blk=blk: e.tensor_scalar(
                    out=dg[ws][jt].ap, in0=C.ident_b.ap, scalar1=cw.ap[:, blk, jt:jt + 1], scalar2=None, op0=ALU.mult),
                    r=[C.ident_b, cw], w=[dg[ws][jt]])
            if kind in ('q', 'k'):
                dst = qT[li_] if kind == 'q' else kT[li_]
                ebias = float(-0.5 * np.log(128.0)) if kind == 'q' else 0.0
                for tq in range(4):
                    bk = banks[6 + tq % 2]
                    r_ = tq % 2
                    for jt in range(5):
                        pe(P, lambda e, jt=jt, tq=tq, bk=bk, ws=ws, hb=hb: e.matmul(
                            bk.ap, lhsT=dg[ws][jt].ap, rhs=hb.ap[:, tq * 512 + jt:tq * 512 + jt + 512],
                            start=(jt == 0), stop=(jt == 4)), r=[dg[ws][jt], hb], w=[bk])
                    act(P, lambda e, bk=bk, r_=r_: e.activation(out=qf[r_].ap, in_=bk.ap, func=AF.Silu), r=[bk], w=[qf[r_]])
                    dve(P, lambda e, r_=r_: e.tensor_tensor(out=sqb[r_].ap, in0=qf[r_].ap, in1=qf[r_].ap, op=ALU.mult),
                        r=[qf[r_]], w=[sqb[r_]])
                    bo = banks[0 + tq % 2]
                    pe(P, lambda e, bo=bo, r_=r_: e.matmul(bo.ap, lhsT=C.OnesF.ap, rhs=sqb[r_].ap, start=True, stop=True),
                       r=[C.OnesF, sqb[r_]], w=[bo])
                    act(P, lambda e, bo=bo, r_=r_: e.activation(out=t1b[r_].ap, in_=bo.ap, func=AF.Ln, bias=1.0e-6, scale=1.0),
                        r=[bo], w=[t1b[r_]])
                    act(P, lambda e, r_=r_, ebias=ebias: e.activation(out=t1b[r_].ap, in_=t1b[r_].ap, func=AF.Exp,
                                                                    bias=ebias, scale=-0.5), r=[t1b[r_]], w=[t1b[r_]])
                    dve(P, lambda e, r_=r_, tq=tq, dst=dst: e.tensor_tensor(out=dst.ap[:, tq * 512:(tq + 1) * 512],
                                                                          in0=qf[r_].ap, in1=t1b[r_].ap, op=ALU.mult),
                        r=[qf[r_], t1b[r_]], w=[dst])
            if kind in ('k', 'v'):
                for t4 in range(4):
                    bk = banks[2 + t4 % 2]
                    r_ = t4 % 2
                    for ti in range(4):
                        tt = t4 * 4 + ti
                        for jt in range(5):
                            pe(P, lambda e, jt=jt, tt=tt, ti=ti, bk=bk, ws=ws, hb=hb: e.matmul(
                                bk.ap[:, ti * 128:(ti + 1) * 128], lhsT=hb.ap[:, tt * 128 + jt:tt * 128 + jt + 128],
                                rhs=dg[ws][jt].ap, start=(jt == 0), stop=(jt == 4)), r=[dg[ws][jt], hb], w=[bk])
                    if kind == 'v':
                        for ti in range(4):
                            tt = t4 * 4 + ti
                            act(P, lambda e, bk=bk, ti=ti, tt=tt, li_=li_: e.activation(
                                out=vtm[tt].ap[:, li_, :], in_=bk.ap[:, ti * 128:(ti + 1) * 128], func=AF.Silu),
                                r=[bk], w=[vtm[tt]])
                    else:
                        act(P, lambda e, bk=bk, r_=r_: e.activation(out=qf[r_].ap, in_=bk.ap, func=AF.Silu), r=[bk], w=[qf[r_]])
                        dve(P, lambda e, r_=r_: e.tensor_tensor(out=sqb[r_].ap, in0=qf[r_].ap, in1=qf[r_].ap, op=ALU.mult),
                            r=[qf[r_]], w=[sqb[r_]])
                        dve(P, lambda e, r_=r_: e.tensor_reduce(out=ss4[r_].ap, in_=sqb[r_].ap.rearrange("p (a b) -> p a b", a=4),
                                                                axis=AX.X, op=ALU.add), r=[sqb[r_]], w=[ss4[r_]])
                        dve(P, lambda e, r_=r_: e.tensor_scalar(out=ss4[r_].ap, in0=ss4[r_].ap, scalar1=1.0e-6, scalar2=None,
                                                                op0=ALU.add), r=[ss4[r_]], w=[ss4[r_]])
                        act(P, lambda e, r_=r_: e.activation(out=ss4[r_].ap, in_=ss4[r_].ap, func=AF.Sqrt), r=[ss4[r_]], w=[ss4[r_]])
                        dve(P, lambda e, r_=r_: e.reciprocal(out=ss4[r_].ap, in_=ss4[r_].ap), r=[ss4[r_]], w=[ss4[r_]])
                        for ti in range(4):
                            tt = t4 * 4 + ti
                            dve(P, lambda e, r_=r_, ti=ti, tt=tt, li_=li_: e.tensor_scalar(
                                out=ktm[tt].ap[:, li_, :], in0=qf[r_].ap[:, ti * 128:(ti + 1) * 128],
                                scalar1=ss4[r_].ap[:, ti:ti + 1], scalar2=None, op0=ALU.mult),
                                r=[qf[r_], ss4[r_]], w=[ktm[tt]])
        for d in range(2):
            dve(P, lambda e, d=d: e.memset(Sf[d].ap, 0.0), r=[Sf[d]], w=[Sf[d]])
            dve(P, lambda e, d=d: e.memset(Sb[d].ap, 0.0), r=[Sb[d]], w=[Sb[d]])
        from itertools import zip_longest
        for step in range(16):
            for _ in zip_longest(unit(sq, hg, 0, step), unit(sq, hg, 1, 15 - step)):
                pass
        for tt in range(16):
            t = sq * 16 + tt
            r_ = tt % 2
            rows = slice(t * 128, (t + 1) * 128)
            cols = slice(hg * 512, (hg + 1) * 512)
            dma(P, lambda e, r_=r_, rows=rows, cols=cols: e.dma_start(out=of_t[r_].ap, in_=OF[0, rows, cols]),
                r=[('OF', 0, t, hg)], w=[of_t[r_]])
            dma(P, lambda e, r_=r_, rows=rows, cols=cols: e.dma_start(out=ob_t[r_].ap, in_=OF[1, rows, cols]),
                r=[('OF', 1, t, hg)], w=[ob_t[r_]])
            bk = banks[tt % 2]
            for dc in range(8):
                pe(P, lambda e, dc=dc, tt=tt, bk=bk: e.matmul(bk.ap, lhsT=xT[dc].ap[:, tt * 128:(tt + 1) * 128], rhs=wz[dc].ap,
                                                              start=(dc == 0), stop=(dc == 7)), r=[xT[dc], wz[dc]], w=[bk])
            act(P, lambda e, bk=bk, r_=r_: e.activation(out=zs[r_].ap, in_=bk.ap, func=AF.Silu), r=[bk], w=[zs[r_]])
            dve(P, lambda e, r_=r_: e.tensor_tensor(out=of_t[r_].ap, in0=of_t[r_].ap, in1=ob_t[r_].ap, op=ALU.add),
                r=[of_t[r_], ob_t[r_]], w=[of_t[r_]])
            dve(P, lambda e, r_=r_: e.tensor_tensor(out=sq5[r_].ap, in0=of_t[r_].ap, in1=of_t[r_].ap, op=ALU.mult),
                r=[of_t[r_]], w=[sq5[r_]])
            dve(P, lambda e, r_=r_: e.tensor_reduce(out=ms5[r_].ap, in_=sq5[r_].ap.rearrange("p (a b) -> p a b", a=4),
                                                    axis=AX.X, op=ALU.add), r=[sq5[r_]], w=[ms5[r_]])
            dve(P, lambda e, r_=r_: e.tensor_scalar(out=ms5[r_].ap, in0=ms5[r_].ap, scalar1=1.0 / 128.0, scalar2=1.0e-6,
                                                    op0=ALU.mult, op1=ALU.add), r=[ms5[r_]], w=[ms5[r_]])
            act(P, lambda e, r_=r_: e.activation(out=ms5[r_].ap, in_=ms5[r_].ap, func=AF.Sqrt), r=[ms5[r_]], w=[ms5[r_]])
            dve(P, lambda e, r_=r_: e.reciprocal(out=ms5[r_].ap, in_=ms5[r_].ap), r=[ms5[r_]], w=[ms5[r_]])
            o3 = lambda b: b.ap.rearrange("p (a b) -> p a b", a=4)
            dve(P, lambda e, r_=r_: e.tensor_tensor(out=o3(of_t[r_]), in0=o3(of_t[r_]), in1=bc_mid(ms5[r_].ap, 128), op=ALU.mult),
                r=[of_t[r_], ms5[r_]], w=[of_t[r_]])
            pool(P, lambda e, r_=r_: e.tensor_tensor(out=o3(of_t[r_]), in0=o3(of_t[r_]), in1=bc_heads(nw_bc.ap, 4), op=ALU.mult),
                 r=[of_t[r_], nw_bc], w=[of_t[r_]])
            dve(P, lambda e, r_=r_: e.tensor_tensor(out=onb[r_].ap, in0=of_t[r_].ap, in1=zs[r_].ap, op=ALU.mult),
                r=[of_t[r_], zs[r_]], w=[onb[r_]])
            dma(P, lambda e, r_=r_, rows=rows, cols=cols: e.dma_start(out=ON[rows, cols], in_=onb[r_].ap),
                r=[onb[r_]], w=[('ON', t, hg)])

    def unit(sq, hg, d, tt):
        S = sets[d]
        b0, b1, b2, b3 = S.bk
        t = sq * 16 + tt
        tk = slice(tt * 128, (tt + 1) * 128)
        Tri, nTri = (C.Ui, C.nUi) if d == 0 else (C.Li, C.nLi)
        NMs = C.NM_Ls if d == 0 else C.NM_Us
        NMi = C.NM_Ui if d == 0 else C.NM_Li
        h0 = hg * 4
        g4 = Gt.ap[:, tt, d, h0:h0 + 4]
        be4 = Bt.ap[:, tt, d, h0:h0 + 4]
        v3 = lambda b: b.ap
        pool(P, lambda e: e.tensor_copy(out=S.Gb.ap, in_=bc_mid(g4, 128)), r=[Gt], w=[S.Gb])
        pool(P, lambda e: e.tensor_tensor(out=S.RG.ap, in0=S.Gb.ap, in1=bc_heads(Tri.ap, 4), op=ALU.mult),
             r=[S.Gb, Tri], w=[S.RG])
        f2 = lambda b: b.ap.rearrange("p a b -> p (a b)")
        pe(P, lambda e: e.matmul(b0.ap, lhsT=Tri.ap, rhs=f2(S.Gb), start=True, stop=False), r=[Tri, S.Gb], w=[b0])
        pe(P, lambda e: e.matmul(b0.ap, lhsT=C.NegOnesF.ap, rhs=f2(S.RG), start=False, stop=True), r=[C.NegOnesF, S.RG], w=[b0])
        pe(P, lambda e: e.matmul(b1.ap, lhsT=C.OnesF.ap, rhs=f2(S.RG), start=True, stop=False), r=[C.OnesF, S.RG], w=[b1])
        pe(P, lambda e: e.matmul(b1.ap, lhsT=nTri.ap, rhs=f2(S.Gb), start=False, stop=True), r=[nTri, S.Gb], w=[b1])
        for kh in range(2):
            pe(P, lambda e, kh=kh: e.matmul(b2.ap[:, kh * 128:(kh + 1) * 128], lhsT=kT[kh].ap[:, tk], rhs=kT[kh].ap[:, tk],
                                            start=True, stop=True), r=[kT[kh]], w=[b2])
            pe(P, lambda e, kh=kh: e.matmul(b2.ap[:, 256 + kh * 128:256 + (kh + 1) * 128], lhsT=kT[kh].ap[:, tk],
                                            rhs=qT[kh].ap[:, tk], start=True, stop=True), r=[kT[kh], qT[kh]], w=[b2])
        pe(P, lambda e: e.matmul(b3.ap[:, 0:4], lhsT=Tri.ap, rhs=g4, start=True, stop=True), r=[Tri, Gt], w=[b3])
        pe(P, lambda e: e.matmul(b3.ap[:, 4:8], lhsT=C.SC.ap, rhs=g4, start=True, stop=True), r=[C.SC, Gt], w=[b3])
        pe(P, lambda e: e.matmul(b3.ap[:, 8:12], lhsT=C.Half0.ap, rhs=g4, start=True, stop=True), r=[C.Half0, Gt], w=[b3])
        pe(P, lambda e: e.matmul(b3.ap[:, 12:16], lhsT=C.Half1.ap, rhs=g4, start=True, stop=True), r=[C.Half1, Gt], w=[b3])
        dve(P, lambda e: e.tensor_copy(out=S.gc.ap, in_=b3.ap[:, 0:4]), r=[b3], w=[S.gc])
        act(P, lambda e: e.activation(out=S.egc.ap, in_=b3.ap[:, 0:4], func=AF.Exp), r=[b3], w=[S.egc])
        dve(P, lambda e: e.tensor_tensor(out=S.ekd.ap, in0=b3.ap[:, 4:8], in1=S.gc.ap, op=ALU.subtract), r=[b3, S.gc], w=[S.ekd])
        act(P, lambda e: e.activation(out=S.ekd.ap, in_=S.ekd.ap, func=AF.Exp), r=[S.ekd], w=[S.ekd])
        act(P, lambda e: e.activation(out=S.gl.ap.rearrange("p a b -> p (a b)"), in_=b3.ap[:, 8:16], func=AF.Exp), r=[b3], w=[S.gl])
        dve(P, lambda e: e.tensor_tensor(out=S.bge.ap, in0=S.egc.ap, in1=be4, op=ALU.mult), r=[S.egc, Bt], w=[S.bge])
        dve(P, lambda e: e.tensor_scalar(out=S.nbeta.ap, in0=be4, scalar1=-1.0, scalar2=None, op0=ALU.mult), r=[Bt], w=[S.nbeta])
        dve(P, lambda e: e.tensor_tensor(out=S.dS.ap, in0=b0.ap.rearrange("p (a b) -> p a b", a=4), in1=bc_heads(NMs.ap, 4), op=ALU.add),
            r=[b0, NMs], w=[S.dS])
        act(P, lambda e: e.activation(out=S.dS.ap, in_=S.dS.ap, func=AF.Exp), r=[S.dS], w=[S.dS])
        dve(P, lambda e: e.tensor_tensor(out=S.dT.ap, in0=b1.ap.rearrange("p (a b) -> p a b", a=4), in1=bc_heads(NMi.ap, 4), op=ALU.add),
            r=[b1, NMi], w=[S.dT])
        act(P, lambda e: e.activation(out=S.dT.ap, in_=S.dT.ap, func=AF.Exp), r=[S.dT], w=[S.dT])
        yield
        for kh in range(2):
            dve(P, lambda e, kh=kh: e.tensor_tensor(out=S.qkT.ap[:, 2 * kh:2 * kh + 2, :], in0=S.dT.ap[:, 2 * kh:2 * kh + 2, :],
                                                   in1=bc_heads(b2.ap[:, 256 + kh * 128:256 + (kh + 1) * 128], 2), op=ALU.mult),
                r=[S.dT, b2], w=[S.qkT])
        for h in range(4):
            dve(P, lambda e, h=h: e.scalar_tensor_tensor(out=S.XA.ap[:, h, :], in0=S.dS.ap[:, h, :], scalar=S.nbeta.ap[:, h:h + 1],
                                                         in1=b2.ap[:, (h // 2) * 128:(h // 2 + 1) * 128],
                                                         op0=ALU.mult, op1=ALU.mult), r=[S.dS, S.nbeta, b2], w=[S.XA])
        pool(P, lambda e: e.tensor_tensor(out=S.vb.ap, in0=vtm[tt].ap, in1=bc_mid(be4, 128), op=ALU.mult), r=[vtm[tt], Bt], w=[S.vb])
        for h in range(4):
            pool(P, lambda e, h=h: e.tensor_scalar(out=S.kbg.ap[:, h, :], in0=ktm[tt].ap[:, h // 2, :], scalar1=S.bge.ap[:, h:h + 1],
                                                   scalar2=None, op0=ALU.mult), r=[ktm[tt], S.bge], w=[S.kbg])
            pool(P, lambda e, h=h: e.tensor_scalar(out=S.kd.ap[:, h, :], in0=ktm[tt].ap[:, h // 2, :], scalar1=S.ekd.ap[:, h:h + 1],
                                                   scalar2=None, op0=ALU.mult), r=[ktm[tt], S.ekd], w=[S.kd])
        yield
        for h in range(4):
            pe(P, lambda e, h=h: e.transpose(b0.ap[:, h * 128:(h + 1) * 128], S.XA.ap[:, h, :], C.ident_f.ap),
               r=[S.XA, C.ident_f], w=[b0])
        act(P, lambda e: e.copy(out=f2(S.XTA), in_=b0.ap), r=[b0], w=[S.XTA])
        dve(P, lambda e: e.tensor_tensor(out=S.PTA.ap, in0=S.XTA.ap, in1=bc_heads(C.ident_f.ap, 4), op=ALU.add),
            r=[S.XTA, C.ident_f], w=[S.PTA])
        yield
        Xc, XTc, PTc = S.XA, S.XTA, S.PTA
        Xn_, XTn_, PTn_ = S.XB, S.XTB, S.PTB
        for lvl in range(5):
            last = (lvl == 4)
            for h in range(4):
                pe(P, lambda e, h=h, Xc=Xc, XTc=XTc: e.matmul(b0.ap[:, h * 128:(h + 1) * 128], lhsT=XTc.ap[:, h, :], rhs=Xc.ap[:, h, :],
                                                             start=True, stop=True), r=[Xc, XTc], w=[b0])
            if not last:
                for h in range(4):
                    pe(P, lambda e, h=h, Xc=Xc, XTc=XTc: e.matmul(b1.ap[:, h * 128:(h + 1) * 128], lhsT=Xc.ap[:, h, :],
                                                                 rhs=XTc.ap[:, h, :], start=True, stop=True), r=[Xc, XTc], w=[b1])
            act(P, lambda e, Xn_=Xn_: e.copy(out=f2(Xn_), in_=b0.ap), r=[b0], w=[Xn_])
            if not last:
                dve(P, lambda e, XTn_=XTn_: e.tensor_copy(out=f2(XTn_), in_=b1.ap), r=[b1], w=[XTn_])
            yield
            for h in range(4):
                pe(P, lambda e, h=h, Xn_=Xn_, PTc=PTc: e.matmul(b2.ap[:, h * 128:(h + 1) * 128], lhsT=Xn_.ap[:, h, :],
                                                               rhs=PTc.ap[:, h, :], start=True, stop=True), r=[Xn_, PTc], w=[b2])
            dve(P, lambda e, PTn_=PTn_, PTc=PTc: e.tensor_tensor(out=f2(PTn_), in0=b2.ap, in1=f2(PTc), op=ALU.add),
                r=[b2, PTc], w=[PTn_])
            yield
            Xc, Xn_ = Xn_, Xc
            XTc, XTn_ = XTn_, XTc
            PTc, PTn_ = PTn_, PTc
        act(P, lambda e, PTc=PTc: e.copy(out=S.Tb.ap, in_=PTc.ap), r=[PTc], w=[S.Tb])
        for h in range(4):
            pe(P, lambda e, h=h: e.matmul(b0.ap[:, h * 128:(h + 1) * 128], lhsT=S.Tb.ap[:, h, :], rhs=S.vb.ap[:, h, :],
                                          start=True, stop=True), r=[S.Tb, S.vb], w=[b0])
        for h in range(4):
            pe(P, lambda e, h=h: e.matmul(b1.ap[:, h * 128:(h + 1) * 128], lhsT=S.kbg.ap[:, h, :], rhs=S.Tb.ap[:, h, :],
                                          start=True, stop=True), r=[S.kbg, S.Tb], w=[b1])
        dve(P, lambda e: e.tensor_copy(out=f2(S.u), in_=b0.ap), r=[b0], w=[S.u])
        act(P, lambda e: e.copy(out=f2(S.wT), in_=b1.ap), r=[b1], w=[S.wT])
        yield
        for c in ((0, 1) if d == 0 else (1, 0)):
            cs = slice(c * 64, (c + 1) * 64)
            ck = (b2.keys[0], c)
            for h in range(4):
                pe(P, lambda e, h=h, cs=cs: e.matmul(b2.ap[cs, h * 128:(h + 1) * 128], lhsT=S.wT.ap[:, h, cs], rhs=Sb[d].ap[:, h, :],
                                                     start=True, stop=True), r=[S.wT, Sb[d]], w=[b2])
            for h in range(4):
                pe(P, lambda e, h=h, cs=cs, c=c: e.matmul(b3.ap[cs, h * 128:(h + 1) * 128], lhsT=qT[h // 2].ap[:, tt * 128 + c * 64:tt * 128 + (c + 1) * 64],
                                                     rhs=Sb[d].ap[:, h, :], start=True, stop=True), r=[qT[h // 2], Sb[d]], w=[b3])
            dve(P, lambda e, cs=cs: e.tensor_tensor(out=f2(S.vn)[cs, :], in0=f2(S.u)[cs, :], in1=b2.ap[cs, :], op=ALU.subtract),
                r=[S.u, b2], w=[S.vn])
            dbg = getattr(C, 'dbg_unit', None)
            if dbg and (sq, hg, d, tt) == (0, 0, 0, 1) and c == 0:
                dma(P, lambda e: e.dma_start(out=dbg['Sb'], in_=f2(Sb[d])), r=[Sb[d]], w=['dbgSb'])
                dma(P, lambda e: e.dma_start(out=dbg['vn'], in_=f2(S.vn)), r=[S.vn], w=['dbgvn'])
                dma(P, lambda e: e.dma_start(out=dbg['u'], in_=f2(S.u)), r=[S.u], w=['dbgu'])
                dma(P, lambda e: e.dma_start(out=dbg['egc'], in_=S.egc.ap), r=[S.egc], w=['dbgegc'])
                dma(P, lambda e: e.dma_start(out=dbg['qkT'], in_=f2(S.qkT)), r=[S.qkT], w=['dbgqkT'])
                dve(P, lambda e: e.tensor_copy(out=f2(S.osb), in_=b3.ap), r=[b3], w=[S.osb])
                dma(P, lambda e: e.dma_start(out=dbg['po1'], in_=f2(S.osb)), r=[S.osb], w=['dbgpo1'])
            yield
            for h in range(4):
                pe(P, lambda e, h=h, cs=cs: e.matmul(b0.ap[cs, h * 128:(h + 1) * 128], lhsT=S.qkT.ap[cs, h, cs], rhs=S.vn.ap[cs, h, :],
                                                     start=True, stop=True), r=[S.qkT, S.vn], w=[b0])
            for h in range(4):
                pe(P, lambda e, h=h, cs=cs: e.matmul(b1.ap[:, h * 128:(h + 1) * 128], lhsT=S.kd.ap[cs, h, :], rhs=S.vn.ap[cs, h, :],
                                                     start=True, stop=True), r=[S.kd, S.vn], w=[b1])
            dve(P, lambda e, cs=cs: e.tensor_tensor(out=S.otmp.ap[cs], in0=b3.ap[cs, :].rearrange("p (a b) -> p a b", a=4),
                                                    in1=bc_mid(S.egc.ap[cs, :], 128), op=ALU.mult), r=[b3, S.egc], w=[S.otmp])
            if dbg and (sq, hg, d, tt) == (0, 0, 0, 1) and c == 0:
                dve(P, lambda e: e.tensor_copy(out=f2(S.osb), in_=b0.ap), r=[b0], w=[S.osb])
                dma(P, lambda e: e.dma_start(out=dbg['po2'], in_=f2(S.osb)), r=[S.osb], w=['dbgpo2'])
                dma(P, lambda e: e.dma_start(out=dbg['otmp'], in_=f2(S.otmp)), r=[S.otmp], w=['dbgotmp'])
            dve(P, lambda e, cs=cs: e.tensor_tensor(out=f2(S.osb)[cs, :], in0=f2(S.otmp)[cs, :], in1=b0.ap[cs, :], op=ALU.add),
                r=[S.otmp, b0], w=[S.osb])
            for h in range(4):
                dve(P, lambda e, h=h, c=c: e.scalar_tensor_tensor(out=Sf[d].ap[:, h, :], in0=Sf[d].ap[:, h, :],
                                                                  scalar=S.gl.ap[:, c, h:h + 1], in1=b1.ap[:, h * 128:(h + 1) * 128],
                                                                  op0=ALU.mult, op1=ALU.add), r=[Sf[d], S.gl, b1], w=[Sf[d]])
            act(P, lambda e: e.copy(out=Sb[d].ap, in_=Sf[d].ap), r=[Sf[d]], w=[Sb[d]])
            yield
        dma(P, lambda e: e.dma_start(out=OF[d, t * 128:(t + 1) * 128, hg * 512:(hg + 1) * 512], in_=f2(S.osb)),
            r=[S.osb], w=[('OF', d, t, hg)])

    for sq in range(2):
        seq_body(sq)
    a = Alloc(C.arena, gdn_dyn)
    wo = [a.new([D], BF16) for _ in range(16)]
    ont = [a.new([2048], BF16) for _ in range(2)]
    onT = [a.new([16, 128], BF16) for _ in range(2)]
    ysb = [a.new([D], F32) for _ in range(2)]
    for c in range(16):
        dma(P, lambda e, c=c: e.dma_start(out=wo[c].ap, in_=W['a_w_out'][jl_, c * 128:(c + 1) * 128, :]), w=[wo[c]], q='pool')
    for t in range(NT):
        s = t % 2
        rows = slice(t * 128, (t + 1) * 128)
        dma(P, lambda e, s=s, rows=rows: e.dma_start(out=ont[s].ap, in_=ON[rows, :]), r=[('ON', t, hg) for hg in range(4)], w=[ont[s]])
        for c4 in range(4):
            bk = banks[c4 % 2]
            bkb = bk.ap.bitcast(BF16)
            for ci in range(4):
                c = c4 * 4 + ci
                pe(P, lambda e, c=c, ci=ci, s=s, bkb=bkb: e.transpose(bkb[:, ci * 128:(ci + 1) * 128], ont[s].ap[:, c * 128:(c + 1) * 128],
                                                                     C.ident_b.ap), r=[ont[s], C.ident_b], w=[bk])
            act(P, lambda e, c4=c4, s=s, bkb=bkb: e.copy(out=onT[s].ap[:, c4 * 4:(c4 + 1) * 4, :].rearrange("p a b -> p (a b)"),
                                                       in_=bkb[:, 0:512]), r=[bk, onT[s]], w=[onT[s]])
        for half in range(2):
            bk = banks[2 + half]
            for c in range(16):
                pe(P, lambda e, c=c, half=half, s=s, bk=bk: e.matmul(bk.ap, lhsT=onT[s].ap[:, c, :], rhs=wo[c].ap[:, half * 512:(half + 1) * 512],
                                                                   start=(c == 0), stop=(c == 15)), r=[onT[s], wo[c]], w=[bk])
            act(P, lambda e, half=half, s=s, bk=bk: e.copy(out=ysb[s].ap[:, half * 512:(half + 1) * 512], in_=bk.ap),
                r=[bk, ysb[s]], w=[ysb[s]])
        dma(P, lambda e, s=s, rows=rows: e.dma_start(out=H_ap[rows, :], in_=ysb[s].ap), r=[ysb[s]], w=[(H_k, t)])


_W_SPECS = [
    ('a_w_in', [2, 1024, 6208]), ('a_conv_w', [2, 5, 4096]), ('a_A_log', [2, 2, 16]), ('a_dt_bias', [2, 2, 16]),
    ('a_norm_w', [2, 128]), ('a_w_out', [2, 2048, 1024]), ('b_w_in', [2, 1024, 1536]), ('b_b_in', [2, 1536]),
    ('b_sinks', [2, 16]), ('b_w_out', [2, 1024, 1024]), ('b_b_out', [2, 1024]), ('router_w', [4, 1024, 32]),
    ('router_b', [4, 32]), ('exp_w_up', [4, 32, 1024, 2048]), ('exp_b_up', [4, 32, 2048]),
    ('exp_w_down', [4, 32, 1024, 1024]), ('exp_b_down', [4, 32, 1024]), ('ln_g', [4, 2, 1024]), ('ln_b', [4, 2, 1024]),
]


def build_program(depth=DEPTH):
    nc = bass.Bass("TRN2", target_bir_lowering=False)
    dt = lambda n, s, k="ExternalInput", d=F32: nc.dram_tensor(n, s, d, kind=k).ap()
    x = dt("x", [NTOK, D])
    W = {n: dt(n, s) for n, s in _W_SPECS}
    out = dt("out", [NTOK, D], "ExternalOutput")
    with ExitStack() as st:
        P = Prog(nc)
        C = Ctx()
        C.nc = nc
        C.XS = dt("XS", [NE * CAP, D], "Internal", BF16)
        C.Y = dt("Y", [NE * CAP, D], "Internal", F32)
        C.OF = dt("OF", [2, NTOK, 2048], "Internal", F32)
        C.ON = dt("ON", [NTOK, 2048], "Internal", BF16)
        XA = dt("XA", [NTOK, D], "Internal")
        XB = dt("XB", [NTOK, D], "Internal")
        Hh = dt("Hh", [NTOK, D], "Internal")
        C.arena = Arena(nc, st, 176 * 1024)
        C.banks = []
        for i in range(8):
            t = st.enter_context(nc.psum_tensor("bank%d" % i, [128, 512], F32))
            C.banks.append(Buf(t[:], [('ps', i)]))
        al = Alloc(C.arena, 0)
        setup_consts(P, C, al)
        C.dyn_start = al.off
        cur = (x, 'x')
        for li in range(depth):
            if li % 2 == 0:
                gdn_stage(P, C, li // 2, cur, (Hh, 'H'), W)
            else:
                attn_stage(P, C, li // 2, cur, (Hh, 'H'), W)
            dst = (out, 'out') if li == depth - 1 else (XB, 'XB')
            moe_stage(P, C, li, cur, (XA, 'XA'), dst, (Hh, 'H'), W)
            cur = dst
        P.emit(st)
    return nc


_LAYER_W = {
    'a': ['a_w_in', 'a_conv_w', 'a_A_log', 'a_dt_bias', 'a_norm_w', 'a_w_out'],
    'b': ['b_w_in', 'b_b_in', 'b_sinks', 'b_w_out', 'b_b_out'],
    'm': ['router_w', 'router_b', 'exp_w_up', 'exp_b_up', 'exp_w_down', 'exp_b_down', 'ln_g', 'ln_b'],
}


def build_layer_program(kind):
    nc = bass.Bass("TRN2", target_bir_lowering=False)
    dt = lambda n, s, k="ExternalInput", d=F32: nc.dram_tensor(n, s, d, kind=k).ap()
    x = dt("x", [NTOK, D])
    out = dt("out", [NTOK, D], "ExternalOutput")
    spec = dict(_W_SPECS)
    names = {'a': _LAYER_W['a'] + _LAYER_W['m'], 'b': _LAYER_W['b'], 'm': _LAYER_W['m']}[kind]
    W = {n: dt(n, [1] + spec[n][1:]) for n in names}
    if kind == 'm':
        Hh = dt("h", [NTOK, D])
    elif kind == 'b':
        Hh = None
    else:
        Hh = dt("Hh", [NTOK, D], "Internal")
    with ExitStack() as st:
        P = Prog(nc)
        C = Ctx()
        C.nc = nc
        if kind in ('a', 'm'):
            C.XS = dt("XS", [NE * CAP, D], "Internal", BF16)
            C.Y = dt("Y", [NE * CAP, D], "Internal", F32)
            XA = dt("XA", [NTOK, D], "Internal")
        if kind == 'a':
            C.OF = dt("OF", [2, NTOK, 2048], "Internal", F32)
            C.ON = dt("ON", [NTOK, 2048], "Internal", BF16)
        C.arena = Arena(nc, st, 176 * 1024)
        C.banks = []
        for i in range(8):
            t = st.enter_context(nc.psum_tensor("bank%d" % i, [128, 512], F32))
            C.banks.append(Buf(t[:], [('ps', i)]))
        al = Alloc(C.arena, 0)
        setup_consts(P, C, al)
        C.dyn_start = al.off
        if kind == 'a':
            gdn_stage(P, C, 0, (x, 'x'), (Hh, 'H'), W)
            moe_stage(P, C, 0, (x, 'x'), (XA, 'XA'), (out, 'out'), (Hh, 'H'), W)
        elif kind == 'b':
            attn_stage(P, C, 0, (x, 'x'), (out, 'out'), W)
        else:
            moe_stage(P, C, 0, (x, 'x'), (XA, 'XA'), (out, 'out'), (Hh, 'H'), W)
        P.emit(st)
    return nc


def kernel(**inputs):
    n = 8
    x = np.ascontiguousarray(np.asarray(inputs['x'], dtype=np.float32))
    xs = x.reshape(n, NTOK, D)
    nc = build_program()
    wmap = {name: np.ascontiguousarray(np.asarray(inputs[name], dtype=np.float32)) for name, _ in _W_SPECS}
    in_maps = []
    for c in range(n):
        m = dict(wmap)
        m['x'] = xs[c]
        in_maps.append(m)
    res = run_bass_kernel_spmd(nc, in_maps, core_ids=list(range(n)))
    outs = [np.asarray(r['out']) for r in res.results]
    return np.stack(outs, 0).reshape(16, SEQ, D).astype(np.float32)
```

```python
from contextlib import ExitStack
import numpy as np
import concourse.bass as bass
import concourse.mybir as mybir
from concourse.bass_utils import run_bass_kernel_spmd

F32 = mybir.dt.float32
BF16 = mybir.dt.bfloat16
U32 = mybir.dt.uint32
I32 = mybir.dt.int32
AF = mybir.ActivationFunctionType
ALU = mybir.AluOpType
AX = mybir.AxisListType

DMA_RING = 16


class Prog:
    def __init__(self, nc):
        self.nc = nc
        self.ops = []
        self.last_w = {}
        self.readers = {}

    def add(self, eng, fn, reads=(), writes=(), dma=False):
        idx = len(self.ops)
        deps = set()
        for k in reads:
            w = self.last_w.get(k)
            if w is not None:
                deps.add(w)
        for k in writes:
            w = self.last_w.get(k)
            if w is not None:
                deps.add(w)
            rs = self.readers.get(k)
            if rs:
                deps.update(rs)
        for k in reads:
            self.readers.setdefault(k, []).append(idx)
        for k in writes:
            self.last_w[k] = idx
            self.readers[k] = []
        self.ops.append((eng, fn, deps, dma))
        return idx

    def pe(self, fn, reads=(), writes=()):
        return self.add('pe', fn, reads, writes)

    def dve(self, fn, reads=(), writes=()):
        return self.add('dve', fn, reads, writes)

    def act(self, fn, reads=(), writes=()):
        return self.add('act', fn, reads, writes)

    def pool(self, fn, reads=(), writes=()):
        return self.add('pool', fn, reads, writes)

    def dma(self, fn, reads=(), writes=(), q='sp'):
        return self.add(q, fn, reads, writes, dma=True)

    def emit(self, stack):
        nc = self.nc
        ops = self.ops
        n = len(ops)
        needed = [False] * n
        for (eng, fn, deps, dma) in ops:
            for d in deps:
                needed[d] = True
        engs = ('pe', 'dve', 'act', 'pool', 'sp')
        esem = {e: stack.enter_context(nc.semaphore('s_' + e)) for e in engs}
        dsem = {e: [stack.enter_context(nc.semaphore('d_%s%d' % (e, i))) for i in range(DMA_RING)]
                for e in ('sp', 'act', 'pool')}
        ecount = {e: 0 for e in engs}
        dcount = {e: 0 for e in dsem}
        event = [None] * n
        extra_wait = [None] * n
        dma_last = {}
        for i, (eng, fn, deps, dma) in enumerate(ops):
            if dma:
                k = dcount[eng]
                dcount[eng] += 1
                slot = k % DMA_RING
                val = 16 * (k // DMA_RING + 1)
                sem = dsem[eng][slot]
                event[i] = (('d', eng, slot), sem, val, 16)
                if k >= DMA_RING:
                    extra_wait[i] = (('d', eng, slot), sem, val - 16)
                dma_last[(eng, slot)] = (('d', eng, slot), sem, val)
                needed[i] = True
            elif needed[i]:
                ecount[eng] += 1
                event[i] = (('e', eng), esem[eng], ecount[eng], 1)
        per_eng = {e: [] for e in engs}
        seen = {e: {} for e in engs}
        for i, (eng, fn, deps, dma) in enumerate(ops):
            waits = []
            cand = []
            if extra_wait[i] is not None:
                cand.append(extra_wait[i])
            for d in sorted(deps):
                deng, _, _, ddma = ops[d]
                if deng == 'pe' and eng == 'pe' and not ddma and not dma:
                    continue
                ev = event[d]
                cand.append((ev[0], ev[1], ev[2]))
            best = {}
            for (sid, sem, val) in cand:
                if seen[eng].get(sid, 0) >= val:
                    continue
                if sid not in best or best[sid][1] < val:
                    best[sid] = (sem, val)
            for sid, (sem, val) in best.items():
                seen[eng][sid] = val
                waits.append((sem, val))
            per_eng[eng].append((fn, waits, event[i] if needed[i] else None))
        final_waits = []
        for (eng, slot), (sid, sem, val) in dma_last.items():
            if seen['sp'].get(sid, 0) < val:
                final_waits.append((sem, val))
        self.stats = {e: len(per_eng[e]) for e in engs}

        with nc.Block() as block:
            def run(e_obj, lst, tail=()):
                for fn, waits, ev in lst:
                    for sem, val in waits:
                        e_obj.wait_ge(sem, val)
                    inst = fn(e_obj)
                    if ev is not None:
                        inst.then_inc(ev[1], ev[3])
                for sem, val in tail:
                    e_obj.wait_ge(sem, val)

            @block.tensor
            def _(e):
                run(e, per_eng['pe'])

            @block.vector
            def _(e):
                run(e, per_eng['dve'])

            @block.scalar
            def _(e):
                run(e, per_eng['act'])

            @block.gpsimd
            def _(e):
                run(e, per_eng['pool'])

            @block.sync
            def _(e):
                run(e, per_eng['sp'], final_waits)


GRAN = 256


class Buf:
    __slots__ = ('ap', 'keys')

    def __init__(self, ap, keys):
        self.ap = ap
        self.keys = keys


def _flat(items):
    out = []
    for k in items:
        if isinstance(k, Buf):
            out.extend(k.keys)
        else:
            out.append(k)
    return out


_ESZ = {F32: 4, BF16: 2, U32: 4, I32: 4}


class Arena:
    def __init__(self, nc, st, nbytes, name="arena"):
        self.t = st.enter_context(nc.sbuf_tensor(name, [128, nbytes // 4], F32))
        self.nbytes = nbytes

    def buf(self, off, shape, dtype, parts=128):
        nel = 1
        for s in shape:
            nel *= s
        nb = nel * _ESZ[dtype]
        assert off % 4 == 0 and off + nb <= self.nbytes, (off, nb, self.nbytes)
        v = self.t[0:parts, off // 4:(off + nb + 3) // 4]
        if dtype != F32:
            v = v.bitcast(dtype)
        if len(shape) == 2:
            v = v.rearrange("p (a b) -> p a b", a=shape[0])
        elif len(shape) == 3:
            v = v.rearrange("p (a b c) -> p a b c", a=shape[0], b=shape[1])
        keys = [('sb', g) for g in range(off // GRAN, (off + nb - 1) // GRAN + 1)]
        return Buf(v, keys)


class Alloc:
    def __init__(self, arena, start, end=None):
        self.arena = arena
        self.off = start
        self.end = end if end is not None else arena.nbytes

    def new(self, shape, dtype, parts=128):
        nel = 1
        for s in shape:
            nel *= s
        nb = nel * _ESZ[dtype]
        b = self.arena.buf(self.off, shape, dtype, parts)
        self.off += (nb + GRAN - 1) // GRAN * GRAN
        assert self.off <= self.end, ("SBUF overflow", self.off, self.end)
        return b


D = 1024
NTOK = 4096
NT = NTOK // 128
SEQ = 2048
NE = 32
CAP = 768
NB = 256
NBLK = CAP // NB
NST = NB // 128
DEPTH = 4
ALPHA = float(8 ** 0.25)
LN_EPS = 1e-5


class Ctx:
    pass


def P_add(P, eng, fn, reads=(), writes=(), dma=False):
    return P.add(eng, fn, _flat(reads), _flat(writes), dma)


def dve(P, fn, r=(), w=()):
    return P.add('dve', fn, _flat(r), _flat(w))


def act(P, fn, r=(), w=()):
    return P.add('act', fn, _flat(r), _flat(w))


def pool(P, fn, r=(), w=()):
    return P.add('pool', fn, _flat(r), _flat(w))


def pe(P, fn, r=(), w=()):
    return P.add('pe', fn, _flat(r), _flat(w))


def dma(P, fn, r=(), w=(), q='sp'):
    return P.add(q, fn, _flat(r), _flat(w), True)


def _bc_reg(C, e):
    if getattr(C, 'bc_reg', None) is None:
        C.bc_reg = e.to_reg(NE * CAP - 1)
    return C.bc_reg


def barrier(P, C, key):
    dve(P, lambda e: e.memset(C.junk1.ap, 0.0), r=[], w=[C.junk1, key])


def setup_consts(P, C, al):
    C.ident_f = al.new([128], F32)
    C.ident_b = al.new([128], BF16)
    C.ones_b = al.new([128], BF16)
    C.su_b = al.new([128], BF16)
    C.ecap = al.new([NE], F32)
    C.capmax = al.new([NE], F32)
    C.junk1 = al.new([8], F32)
    tmp = al.new([128], F32)
    pool(P, lambda e: e.memset(C.ident_f.ap, 1.0), w=[C.ident_f])
    pool(P, lambda e: e.affine_select(out=C.ident_f.ap, in_=C.ident_f.ap, pattern=[[-1, 128]],
                                       compare_op=ALU.is_equal, fill=0.0, base=0, channel_multiplier=1),
         r=[C.ident_f], w=[C.ident_f])
    dve(P, lambda e: e.tensor_copy(out=C.ident_b.ap, in_=C.ident_f.ap), r=[C.ident_f], w=[C.ident_b])
    dve(P, lambda e: e.memset(C.ones_b.ap, 1.0), w=[C.ones_b])
    pool(P, lambda e: e.memset(tmp.ap, 1.0), w=[tmp])
    pool(P, lambda e: e.affine_select(out=tmp.ap, in_=tmp.ap, pattern=[[1, 128]],
                                       compare_op=ALU.is_gt, fill=0.0, base=0, channel_multiplier=-1),
         r=[tmp], w=[tmp])
    dve(P, lambda e: e.tensor_copy(out=C.su_b.ap, in_=tmp.ap), r=[tmp], w=[C.su_b])
    pool(P, lambda e: e.iota(C.ecap.ap, pattern=[[CAP, NE]], base=0, channel_multiplier=0,
                              allow_small_or_imprecise_dtypes=True), w=[C.ecap])
    dve(P, lambda e: e.tensor_scalar(out=C.capmax.ap, in0=C.ecap.ap, scalar1=float(CAP - 1), scalar2=None,
                                      op0=ALU.add), r=[C.ecap], w=[C.capmax])


def ln_tile(P, xa, ha, gbc, bbc, sm):
    st, mv, sd, rstd = sm
    dve(P, lambda e: e.scalar_tensor_tensor(out=xa.ap, in0=xa.ap, scalar=ALPHA, in1=ha.ap,
                                            op0=ALU.mult, op1=ALU.add), r=[xa, ha], w=[xa])
    dve(P, lambda e: e.bn_stats(out=st.ap[:, 0:6], in_=xa.ap[:, 0:512]), r=[xa], w=[st])
    dve(P, lambda e: e.bn_stats(out=st.ap[:, 6:12], in_=xa.ap[:, 512:1024]), r=[xa, st], w=[st])
    dve(P, lambda e: e.bn_aggr(out=mv.ap, in_=st.ap), r=[st], w=[mv])
    dve(P, lambda e: e.tensor_scalar(out=sd.ap, in0=mv.ap[:, 1:2], scalar1=LN_EPS, scalar2=None, op0=ALU.add),
        r=[mv], w=[sd])
    act(P, lambda e: e.activation(out=sd.ap, in_=sd.ap, func=AF.Sqrt), r=[sd], w=[sd])
    dve(P, lambda e: e.reciprocal(out=rstd.ap, in_=sd.ap), r=[sd], w=[rstd])
    dve(P, lambda e: e.tensor_scalar(out=xa.ap, in0=xa.ap, scalar1=mv.ap[:, 0:1], scalar2=rstd.ap[:, 0:1],
                                     op0=ALU.subtract, op1=ALU.mult), r=[xa, mv, rstd], w=[xa])
    pool(P, lambda e: e.tensor_tensor(out=xa.ap, in0=xa.ap, in1=gbc.ap, op=ALU.mult), r=[xa, gbc], w=[xa])
    pool(P, lambda e: e.tensor_tensor(out=xa.ap, in0=xa.ap, in1=bbc.ap, op=ALU.add), r=[xa, bbc], w=[xa])


def moe_stage(P, C, li, Xin, Xmid, Xout, H, W):
    nc = C.nc
    (Xin_ap, Xin_k), (Xmid_ap, Xmid_k), (Xout_ap, Xout_k), (H_ap, H_k) = Xin, Xmid, Xout, H
    XS, Y = C.XS, C.Y
    banks = C.banks
    al = Alloc(C.arena, C.dyn_start)
    destf = al.new([NT, 4], F32)
    desti = al.new([NT, 4], U32)
    gates = al.new([NT, 4], F32)
    g1 = al.new([D], F32)
    b1 = al.new([D], F32)
    g2, b2 = g1, b1
    rw = al.new([8, NE], F32)
    rb = al.new([NE], F32)
    bupT = al.new([16, NE], F32)
    cnt = [al.new([NE], F32), al.new([NE], F32)]
    sm = [(al.new([12], F32), al.new([2], F32), al.new([1], F32), al.new([1], F32)) for _ in range(2)]
    lg = [al.new([NE], F32) for _ in range(2)]
    mx8 = [al.new([8], F32) for _ in range(2)]
    nv1 = [al.new([1], F32) for _ in range(2)]
    e4 = [al.new([4], F32) for _ in range(2)]
    ssum = [al.new([1], F32) for _ in range(2)]
    maskb = [al.new([NE], BF16) for _ in range(2)]
    slot = [al.new([NE], F32) for _ in range(2)]
    oh = [al.new([NE], F32) for _ in range(2)]
    junk = [al.new([NE], F32) for _ in range(2)]
    big0 = al.off
    wu0 = [al.new([2048], BF16) for dc in range(8)]
    off_wu1 = al.off
    wu1 = [al.new([2048], BF16) for dc in range(8)]
    wu = [wu0, wu1]
    wd = [[al.new([1024], BF16) for fc in range(8)] for _ in range(2)]
    bd = [al.new([D], F32) for _ in range(2)]
    xs = [al.new([NST, D], BF16) for _ in range(2)]
    xsT = [[al.new([2, NB], BF16) for _ in range(4)] for _ in range(2)]
    actT = [[al.new([NB], BF16) for fc in range(8)] for _ in range(2)]
    gsb = [al.new([NB], F32) for _ in range(2)]
    sgb = [al.new([NB], F32) for _ in range(2)]
    usb = [al.new([NB], F32) for _ in range(2)]
    gsm = [al.new([NB], F32) for _ in range(2)]
    ysb = [al.new([D], F32) for _ in range(2)]
    a1 = Alloc(C.arena, off_wu1, off_wu1 + 32768)
    xa = [a1.new([D], F32) for _ in range(2)]
    ha = [a1.new([D], F32) for _ in range(2)]
    xT = [a1.new([D], F32) for _ in range(2)]
    xb = [a1.new([D], BF16) for _ in range(2)]
    bup_raw = Alloc(C.arena, off_wu1).new([2048], F32, parts=NE)
    a3 = Alloc(C.arena, big0)
    xa3 = [a3.new([D], F32) for _ in range(2)]
    yk = [[a3.new([D], F32) for k in range(4)] for _ in range(2)]
    acc = [a3.new([D], F32) for _ in range(2)]

    lw = W
    dma(P, lambda e: e.dma_start(out=g1.ap, in_=lw['ln_g'][li, 0:1, :].partition_broadcast(128)), w=[g1])
    dma(P, lambda e: e.dma_start(out=b1.ap, in_=lw['ln_b'][li, 0:1, :].partition_broadcast(128)), w=[b1])
    dma(P, lambda e: e.dma_start(out=rw.ap, in_=lw['router_w'][li].rearrange("(c p) n -> p c n", p=128)), w=[rw])
    dma(P, lambda e: e.dma_start(out=rb.ap, in_=lw['router_b'][li:li + 1, :].partition_broadcast(128)), w=[rb])
    dma(P, lambda e: e.dma_start(out=bup_raw.ap, in_=lw['exp_b_up'][li]), w=[bup_raw])
    for c in range(16):
        bk = banks[2]
        pe(P, lambda e, c=c, bk=bk: e.transpose(bk.ap[:, 0:NE], bup_raw.ap[:, c * 128:(c + 1) * 128],
                                                 C.ident_f.ap[0:NE, 0:NE]), r=[bup_raw, C.ident_f], w=[bk])
        if c < 8:
            dve(P, lambda e, c=c, bk=bk: e.tensor_copy(out=bupT.ap[:, c, :], in_=bk.ap[:, 0:NE]), r=[bk], w=[bupT])
        else:
            dve(P, lambda e, c=c, bk=bk: e.tensor_scalar(out=bupT.ap[:, c, :], in0=bk.ap[:, 0:NE], scalar1=1.0,
                                                          scalar2=None, op0=ALU.add), r=[bk], w=[bupT])
    dve(P, lambda e: e.tensor_copy(out=cnt[0].ap, in_=C.ecap.ap), r=[C.ecap], w=[cnt[0]])

    def load_w(e_):
        ws = e_ % 2
        for dc in range(8):
            dma(P, lambda e, dc=dc: e.dma_start(out=wu[ws][dc].ap, in_=lw['exp_w_up'][li, e_, dc * 128:(dc + 1) * 128, :]),
                w=[wu[ws][dc]], q='pool')
        for fc in range(8):
            dma(P, lambda e, fc=fc: e.dma_start(out=wd[ws][fc].ap, in_=lw['exp_w_down'][li, e_, fc * 128:(fc + 1) * 128, :]),
                w=[wd[ws][fc]], q='pool')
        dma(P, lambda e: e.dma_start(out=bd[ws].ap, in_=lw['exp_b_down'][li, e_:e_ + 1, :].partition_broadcast(128)),
            w=[bd[ws]])

    barrier(P, C, 'K_XS')
    load_w(0)
    def m1_tile(t):
        s = t % 2
        rows = slice(t * 128, (t + 1) * 128)
        dma(P, lambda e, s=s, rows=rows: e.dma_start(out=xa[s].ap, in_=Xin_ap[rows, :]), r=[(Xin_k, t)], w=[xa[s]])
        dma(P, lambda e, s=s, rows=rows: e.dma_start(out=ha[s].ap, in_=H_ap[rows, :]), r=[(H_k, t)], w=[ha[s]])
        ln_tile(P, xa[s], ha[s], g1, b1, sm[s])
        dma(P, lambda e, s=s, rows=rows: e.dma_start(out=Xmid_ap[rows, :], in_=xa[s].ap), r=[xa[s]], w=[(Xmid_k, t)])
        act(P, lambda e, s=s: e.copy(out=xb[s].ap, in_=xa[s].ap), r=[xa[s]], w=[xb[s]])
        bA, bB = (banks[0], banks[1]) if s == 0 else (banks[5], banks[6])
        for c in range(8):
            bk = bA if c < 4 else bB
            pe(P, lambda e, c=c, bk=bk, s=s: e.transpose(bk.ap[:, (c % 4) * 128:(c % 4 + 1) * 128],
                                                       xa[s].ap[:, c * 128:(c + 1) * 128], C.ident_f.ap),
               r=[xa[s], C.ident_f], w=[bk])
        act(P, lambda e, s=s, bk=bA: e.copy(out=xT[s].ap[:, 0:512], in_=bk.ap), r=[bA], w=[xT[s]])
        act(P, lambda e, s=s, bk=bB: e.copy(out=xT[s].ap[:, 512:1024], in_=bk.ap), r=[bB, xT[s]], w=[xT[s]])
        for c in range(8):
            pe(P, lambda e, c=c, s=s: e.matmul(banks[2].ap[:, 0:NE], lhsT=xT[s].ap[:, c * 128:(c + 1) * 128],
                                               rhs=rw.ap[:, c, :], start=(c == 0), stop=(c == 7)),
               r=[xT[s], rw], w=[banks[2]])
        dve(P, lambda e, s=s: e.tensor_tensor(out=lg[s].ap, in0=banks[2].ap[:, 0:NE], in1=rb.ap, op=ALU.add),
            r=[banks[2], rb], w=[lg[s]])
        dve(P, lambda e, s=s: e.max(out=mx8[s].ap, in_=lg[s].ap), r=[lg[s]], w=[mx8[s]])
        dve(P, lambda e, s=s: e.tensor_scalar(out=nv1[s].ap, in0=mx8[s].ap[:, 0:1], scalar1=-1.0, scalar2=None,
                                              op0=ALU.mult), r=[mx8[s]], w=[nv1[s]])
        act(P, lambda e, s=s: e.activation(out=e4[s].ap, in_=mx8[s].ap[:, 0:4], func=AF.Exp, bias=nv1[s].ap[:, 0:1],
                                           scale=1.0, accum_out=ssum[s].ap), r=[mx8[s], nv1[s]], w=[e4[s], ssum[s]])
        dve(P, lambda e, s=s: e.reciprocal(out=ssum[s].ap, in_=ssum[s].ap), r=[ssum[s]], w=[ssum[s]])
        dve(P, lambda e, s=s, t=t: e.tensor_scalar(out=gates.ap[:, t, :], in0=e4[s].ap, scalar1=ssum[s].ap[:, 0:1],
                                                   scalar2=None, op0=ALU.mult), r=[e4[s], ssum[s]], w=[gates])
        dve(P, lambda e, s=s: e.tensor_scalar(out=maskb[s].ap, in0=lg[s].ap, scalar1=mx8[s].ap[:, 3:4], scalar2=None,
                                              op0=ALU.is_ge), r=[lg[s], mx8[s]], w=[maskb[s]])
        pe(P, lambda e, s=s: e.matmul(banks[3].ap[:, 0:NE], lhsT=C.su_b.ap, rhs=maskb[s].ap, start=True, stop=True),
           r=[C.su_b, maskb[s]], w=[banks[3]])
        pe(P, lambda e, s=s: e.matmul(banks[4].ap[:, 0:NE], lhsT=C.ones_b.ap, rhs=maskb[s].ap, start=True, stop=True),
           r=[C.ones_b, maskb[s]], w=[banks[4]])
        cur, nxt = cnt[t % 2], cnt[(t + 1) % 2]
        dve(P, lambda e, s=s, cur=cur: e.tensor_tensor(out=slot[s].ap, in0=banks[3].ap[:, 0:NE], in1=cur.ap, op=ALU.add),
            r=[banks[3], cur], w=[slot[s]])
        dve(P, lambda e, s=s: e.tensor_tensor(out=slot[s].ap, in0=slot[s].ap, in1=C.capmax.ap, op=ALU.min),
            r=[slot[s], C.capmax], w=[slot[s]])
        dve(P, lambda e, cur=cur, nxt=nxt: e.tensor_tensor(out=nxt.ap, in0=banks[4].ap[:, 0:NE], in1=cur.ap, op=ALU.add),
            r=[banks[4], cur], w=[nxt])
        for k in range(4):
            dve(P, lambda e, s=s, k=k: e.tensor_scalar(out=oh[s].ap, in0=lg[s].ap, scalar1=mx8[s].ap[:, k:k + 1],
                                                       scalar2=None, op0=ALU.is_equal), r=[lg[s], mx8[s]], w=[oh[s]])
            dve(P, lambda e, s=s, k=k, t=t: e.scalar_tensor_tensor(out=junk[s].ap, in0=oh[s].ap, scalar=1.0, in1=slot[s].ap,
                                                                   op0=ALU.mult, op1=ALU.mult,
                                                                   accum_out=destf.ap[:, t, k:k + 1]),
                r=[oh[s], slot[s]], w=[junk[s], destf])
        dve(P, lambda e, t=t: e.tensor_copy(out=desti.ap[:, t, :], in_=destf.ap[:, t, :]), r=[destf], w=[desti])
        for k in range(4):
            dma(P, lambda e, s=s, k=k, t=t: e.indirect_dma_start(
                out=XS[:, :], out_offset=bass.IndirectOffsetOnAxis(ap=desti.ap[:, t, k:k + 1], axis=0),
                in_=xb[s].ap, in_offset=None, bounds_check=_bc_reg(C, e), oob_is_err=False),
                r=[xb[s], desti, 'K_XS'], w=[], q='pool')
    for t in range(NT):
        m1_tile(t)
    if getattr(C, 'dbg', None):
        dma(P, lambda e: e.dma_start(out=C.dbg['destf'], in_=destf.ap), r=[destf], w=['dbg1'])
        dma(P, lambda e: e.dma_start(out=C.dbg['gates'], in_=gates.ap), r=[gates], w=['dbg2'])
    barrier(P, C, 'K_XS')
    barrier(P, C, 'K_Y')
    nblk_total = NE * NBLK

    def load_xs(n):
        e_, b_ = divmod(n, NBLK)
        r0 = e_ * CAP + b_ * NB
        bs = n % 2
        dma(P, lambda e: e.dma_start(out=xs[bs].ap, in_=XS[r0:r0 + NB, :].rearrange("(s p) d -> p s d", p=128)),
            r=['K_XS'], w=[xs[bs]])

    load_xs(0)
    def m2_block(e_, b_):
        if True:
            ws = e_ % 2
            n = e_ * NBLK + b_
            bs = n % 2
            if n + 1 < nblk_total:
                load_xs(n + 1)
            for dcp in range(4):
                bk = banks[dcp % 2]
                bkb = bk.ap.bitcast(BF16)
                for j in range(2):
                    dc = dcp * 2 + j
                    for st_ in range(NST):
                        pe(P, lambda e, dc=dc, st_=st_, j=j, bkb=bkb: e.transpose(
                            bkb[:, j * NB + st_ * 128: j * NB + (st_ + 1) * 128],
                            xs[bs].ap[:, st_, dc * 128:(dc + 1) * 128], C.ident_b.ap),
                           r=[xs[bs], C.ident_b], w=[bk])
                act(P, lambda e, dcp=dcp, bkb=bkb: e.copy(out=xsT[bs][dcp].ap.rearrange("p a b -> p (a b)"),
                                                         in_=bkb[:, 0:2 * NB]), r=[bk], w=[xsT[bs][dcp]])
            for fc in range(8):
                pgk = banks[2 + fc % 2]
                puk = banks[4 + fc % 2]
                for dc in range(8):
                    pe(P, lambda e, dc=dc, fc=fc, pgk=pgk: e.matmul(
                        pgk.ap[:, 0:NB], lhsT=wu[ws][dc].ap[:, fc * 128:(fc + 1) * 128],
                        rhs=xsT[bs][dc // 2].ap[:, dc % 2, :], start=(dc == 0), stop=(dc == 7)),
                       r=[wu[ws][dc], xsT[bs][dc // 2]], w=[pgk])
                for dc in range(8):
                    pe(P, lambda e, dc=dc, fc=fc, puk=puk: e.matmul(
                        puk.ap[:, 0:NB], lhsT=wu[ws][dc].ap[:, 1024 + fc * 128:1024 + (fc + 1) * 128],
                        rhs=xsT[bs][dc // 2].ap[:, dc % 2, :], start=(dc == 0), stop=(dc == 7)),
                       r=[wu[ws][dc], xsT[bs][dc // 2]], w=[puk])
                q = fc % 2
                dve(P, lambda e, fc=fc, pgk=pgk, q=q: e.tensor_scalar(
                    out=gsb[q].ap, in0=pgk.ap[:, 0:NB], scalar1=bupT.ap[:, fc, e_:e_ + 1], scalar2=7.0,
                    op0=ALU.add, op1=ALU.min), r=[pgk, bupT], w=[gsb[q]])
                act(P, lambda e, q=q: e.activation(out=sgb[q].ap, in_=gsb[q].ap, func=AF.Sigmoid, scale=1.702),
                    r=[gsb[q]], w=[sgb[q]])
                dve(P, lambda e, fc=fc, puk=puk, q=q: e.tensor_scalar(
                    out=usb[q].ap, in0=puk.ap[:, 0:NB], scalar1=bupT.ap[:, 8 + fc, e_:e_ + 1], scalar2=8.0,
                    op0=ALU.add, op1=ALU.min), r=[puk, bupT], w=[usb[q]])
                pool(P, lambda e, q=q: e.tensor_tensor(out=gsm[q].ap, in0=gsb[q].ap, in1=sgb[q].ap, op=ALU.mult),
                     r=[gsb[q], sgb[q]], w=[gsm[q]])
                dve(P, lambda e, fc=fc, q=q: e.scalar_tensor_tensor(
                    out=actT[bs][fc].ap, in0=usb[q].ap, scalar=-6.0, in1=gsm[q].ap, op0=ALU.max, op1=ALU.mult),
                    r=[usb[q], gsm[q]], w=[actT[bs][fc]])
    def m2_down(e_, b_):
        if True:
            ws = e_ % 2
            n = e_ * NBLK + b_
            bs = n % 2
            for st_ in range(NST):
                ys = (n * NST + st_) % 2
                for half in range(2):
                    pyk = banks[6 + half]
                    for fc in range(8):
                        pe(P, lambda e, fc=fc, half=half, st_=st_, pyk=pyk: e.matmul(
                            pyk.ap, lhsT=actT[bs][fc].ap[:, st_ * 128:(st_ + 1) * 128],
                            rhs=wd[ws][fc].ap[:, half * 512:(half + 1) * 512], start=(fc == 0), stop=(fc == 7)),
                           r=[actT[bs][fc], wd[ws][fc]], w=[pyk])
                    dve(P, lambda e, half=half, pyk=pyk, ys=ys: e.tensor_tensor(
                        out=ysb[ys].ap[:, half * 512:(half + 1) * 512], in0=pyk.ap,
                        in1=bd[ws].ap[:, half * 512:(half + 1) * 512], op=ALU.add),
                        r=[pyk, bd[ws], ysb[ys]], w=[ysb[ys]])
                r0 = e_ * CAP + b_ * NB + st_ * 128
                dma(P, lambda e, r0=r0, ys=ys: e.dma_start(out=Y[r0:r0 + 128, :], in_=ysb[ys].ap),
                    r=[ysb[ys], 'K_Y'], w=[])
    prev = None
    for e_ in range(NE):
        for b_ in range(NBLK):
            m2_block(e_, b_)
            if prev is not None:
                m2_down(*prev)
            prev = (e_, b_)
            if b_ == 0 and e_ + 1 < NE:
                load_w(e_ + 1)
    m2_down(*prev)
    barrier(P, C, 'K_Y')
    dma(P, lambda e: e.dma_start(out=g2.ap, in_=lw['ln_g'][li, 1:2, :].partition_broadcast(128)), w=[g2])
    dma(P, lambda e: e.dma_start(out=b2.ap, in_=lw['ln_b'][li, 1:2, :].partition_broadcast(128)), w=[b2])
    def m3_tile(t):
        s = t % 2
        rows = slice(t * 128, (t + 1) * 128)
        dma(P, lambda e, s=s, rows=rows: e.dma_start(out=xa3[s].ap, in_=Xmid_ap[rows, :]), r=[(Xmid_k, t)], w=[xa3[s]])
        for k in range(4):
            dma(P, lambda e, s=s, k=k, t=t: e.indirect_dma_start(
                out=yk[s][k].ap, out_offset=None, in_=Y[:, :],
                in_offset=bass.IndirectOffsetOnAxis(ap=desti.ap[:, t, k:k + 1], axis=0),
                bounds_check=_bc_reg(C, e), oob_is_err=False), r=['K_Y', desti], w=[yk[s][k]], q='pool')
        dve(P, lambda e, s=s, t=t: e.tensor_scalar(out=acc[s].ap, in0=yk[s][0].ap, scalar1=gates.ap[:, t, 0:1],
                                                   scalar2=None, op0=ALU.mult), r=[yk[s][0], gates], w=[acc[s]])
        for k in range(1, 4):
            dve(P, lambda e, s=s, t=t, k=k: e.scalar_tensor_tensor(
                out=acc[s].ap, in0=yk[s][k].ap, scalar=gates.ap[:, t, k:k + 1], in1=acc[s].ap,
                op0=ALU.mult, op1=ALU.add), r=[yk[s][k], gates, acc[s]], w=[acc[s]])
        ln_tile(P, xa3[s], acc[s], g2, b2, sm[s])
        dma(P, lambda e, s=s, rows=rows: e.dma_start(out=Xout_ap[rows, :], in_=xa3[s].ap), r=[xa3[s]], w=[(Xout_k, t)])
    for t in range(NT):
        m3_tile(t)


def load_xT(P, C, X, sq, xT, xa):
    X_ap, X_k = X
    banks = C.banks
    for tt in range(16):
        t = sq * 16 + tt
        s = tt % 2
        rows = slice(t * 128, (t + 1) * 128)
        dma(P, lambda e, s=s, rows=rows: e.dma_start(out=xa[s].ap, in_=X_ap[rows, :]), r=[(X_k, t)], w=[xa[s]])
        bA, bB = (banks[0], banks[1]) if s == 0 else (banks[2], banks[3])
        for c in range(8):
            bk = bA if c < 4 else bB
            pe(P, lambda e, c=c, bk=bk, s=s: e.transpose(bk.ap[:, (c % 4) * 128:(c % 4 + 1) * 128],
                                                       xa[s].ap[:, c * 128:(c + 1) * 128], C.ident_f.ap),
               r=[xa[s], C.ident_f], w=[bk])
        for c in range(8):
            bk = bA if c < 4 else bB
            eng = act if c % 2 == 0 else dve
            if False:
                act(P, lambda e, c=c, bk=bk, tt=tt: e.copy(out=xT[c].ap[:, tt * 128:(tt + 1) * 128],
                                                        in_=bk.ap[:, (c % 4) * 128:(c % 4 + 1) * 128]),
                    r=[bk], w=[xT[c]])
            else:
                dve(P, lambda e, c=c, bk=bk, tt=tt: e.tensor_copy(out=xT[c].ap[:, tt * 128:(tt + 1) * 128],
                                                               in_=bk.ap[:, (c % 4) * 128:(c % 4 + 1) * 128]),
                    r=[bk], w=[xT[c]])


def row_to_cols(P, C, row, out, n, bank):
    for c in range(n):
        pe(P, lambda e, c=c: e.transpose(bank.ap[:, c:c + 1], row.ap[0:1, c * 128:(c + 1) * 128],
                                         C.ident_f.ap[0:1, 0:1]), r=[row, C.ident_f], w=[bank])
    dve(P, lambda e: e.tensor_copy(out=out.ap[:, 0:n], in_=bank.ap[:, 0:n]), r=[bank], w=[out])


ATT_SLOPES = [float(2.0 ** (-8.0 * (h + 1) / 16)) for h in range(16)]


def attn_stage(P, C, j, X, H, W):
    nc = C.nc
    banks = C.banks
    H_ap, H_k = H
    al = Alloc(C.arena, C.dyn_start)
    xT = [al.new([SEQ], BF16) for _ in range(8)]
    oT2 = xT
    QT2 = [al.new([SEQ], BF16) for _ in range(8)]
    KT2 = [al.new([SEQ], BF16) for _ in range(4)]
    V = [al.new([256], BF16) for _ in range(16)]
    win = [al.new([1536], BF16) for _ in range(8)]
    wk2 = [al.new([4, 128], BF16) for _ in range(8)]
    wo = [al.new([D], BF16) for _ in range(8)]
    bqk = al.new([12], F32)
    bq8 = al.new([8], F32)
    bk2 = al.new([4], F32)
    bv_bc = al.new([256], F32)
    bo_bc = al.new([D], F32)
    sinkbc = al.new([16], F32)
    Dm = al.new([384], F32)
    dtmp = al.new([384], F32)
    xa_off = al.off
    xa = [al.new([D], F32) for _ in range(2)]
    ysb = xa
    _a = Alloc(C.arena, xa_off)
    brow = _a.new([1536], F32, parts=1)
    bkrow2 = _a.new([512], F32, parts=1)
    R = 4
    Sb = [al.new([384], F32) for _ in range(R)]
    Pe = [al.new([384], F32) for _ in range(R)]
    Pn = [al.new([384], BF16) for _ in range(R)]
    PTs = [al.new([384], BF16) for _ in range(R)]
    _sm = [al.new([8], F32) for _ in range(R)]
    mr = [Buf(b.ap[:, 0:1], b.keys) for b in _sm]
    negm = [Buf(b.ap[:, 1:2], b.keys) for b in _sm]
    rsum = [Buf(b.ap[:, 2:3], b.keys) for b in _sm]
    es = [Buf(b.ap[:, 3:4], b.keys) for b in _sm]
    rr = [Buf(b.ap[:, 4:5], b.keys) for b in _sm]

    for dc in range(8):
        dma(P, lambda e, dc=dc: e.dma_start(out=win[dc].ap, in_=W['b_w_in'][j, dc * 128:(dc + 1) * 128, :]),
            w=[win[dc]], q='pool')
        dma(P, lambda e, dc=dc: e.dma_start(out=wo[dc].ap, in_=W['b_w_out'][j, dc * 128:(dc + 1) * 128, :]),
            w=[wo[dc]], q='pool')
    for dc in range(8):
        for half in range(2):
            act(P, lambda e, dc=dc, half=half: e.copy(
                out=wk2[dc].ap[:, :, half * 64:(half + 1) * 64],
                in_=win[dc].ap[:, 1024:1280].rearrange("p (g d) -> p g d", g=4)), r=[win[dc]], w=[wk2[dc]])
    def _nc_dma(e, out, in_):
        with nc.allow_non_contiguous_dma(reason="small per-partition bias load"):
            return e.dma_start(out=out, in_=in_)

    dma(P, lambda e: _nc_dma(e, bqk.ap[:, 0:8], W['b_b_in'][j, 0:1024].rearrange("(c p) -> p c", p=128)), w=[bqk])
    dve(P, lambda e: e.tensor_scalar(out=bq8.ap, in0=bqk.ap[:, 0:8], scalar1=0.125, scalar2=None, op0=ALU.mult),
        r=[bqk], w=[bq8])
    for half in range(2):
        dma(P, lambda e, half=half: _nc_dma(e, bk2.ap[half * 64:(half + 1) * 64, :],
                                            W['b_b_in'][j, 1024:1280].rearrange("(g d) -> d g", d=64)), r=[bk2], w=[bk2])
    dma(P, lambda e: e.dma_start(out=bv_bc.ap, in_=W['b_b_in'][j:j + 1, 1280:1536].partition_broadcast(128)), w=[bv_bc])
    dma(P, lambda e: e.dma_start(out=bo_bc.ap, in_=W['b_b_out'][j:j + 1, :].partition_broadcast(128)), w=[bo_bc])
    dma(P, lambda e: e.dma_start(out=sinkbc.ap, in_=W['b_sinks'][j:j + 1, :].partition_broadcast(128)), w=[sinkbc])
    pool(P, lambda e: e.iota(Dm.ap, pattern=[[-1, 384]], base=128, channel_multiplier=1,
                             allow_small_or_imprecise_dtypes=True), w=[Dm])
    dve(P, lambda e: e.tensor_scalar(out=dtmp.ap, in0=Dm.ap, scalar1=-1.0, scalar2=None, op0=ALU.mult), r=[Dm], w=[dtmp])
    dve(P, lambda e: e.tensor_tensor(out=Dm.ap, in0=Dm.ap, in1=dtmp.ap, op=ALU.max), r=[Dm, dtmp], w=[Dm])
    dve(P, lambda e: e.tensor_scalar(out=dtmp.ap, in0=Dm.ap, scalar1=128.0, scalar2=1.0e6, op0=ALU.is_gt, op1=ALU.mult),
        r=[Dm], w=[dtmp])
    dve(P, lambda e: e.tensor_tensor(out=Dm.ap, in0=Dm.ap, in1=dtmp.ap, op=ALU.add), r=[Dm, dtmp], w=[Dm])

    stop = getattr(C, 'attn_stop', 99)
    if stop <= 1:
        return

    def seq_body(sq):
        load_xT(P, C, X, sq, xT, xa)
        if stop <= 2:
            return
        for hp in range(8):
            for tq in range(4):
                bk = banks[4 + (hp * 4 + tq) % 2]
                for dc in range(8):
                    pe(P, lambda e, dc=dc, hp=hp, tq=tq, bk=bk: e.matmul(
                        bk.ap, lhsT=win[dc].ap[:, hp * 128:(hp + 1) * 128], rhs=xT[dc].ap[:, tq * 512:(tq + 1) * 512],
                        start=(dc == 0), stop=(dc == 7)), r=[win[dc], xT[dc]], w=[bk])
                act(P, lambda e, hp=hp, tq=tq, bk=bk: e.activation(
                    out=QT2[hp].ap[:, tq * 512:(tq + 1) * 512], in_=bk.ap, func=AF.Identity,
                    bias=bq8.ap[:, hp:hp + 1], scale=0.125), r=[bk, bq8], w=[QT2[hp]])
        for g in range(4):
            for tq in range(4):
                bk = banks[4 + (g * 4 + tq) % 2]
                for dc in range(8):
                    pe(P, lambda e, dc=dc, g=g, tq=tq, bk=bk: e.matmul(
                        bk.ap, lhsT=wk2[dc].ap[:, g, :], rhs=xT[dc].ap[:, tq * 512:(tq + 1) * 512],
                        start=(dc == 0), stop=(dc == 7)), r=[wk2[dc], xT[dc]], w=[bk])
                act(P, lambda e, g=g, tq=tq, bk=bk: e.activation(
                    out=KT2[g].ap[:, tq * 512:(tq + 1) * 512], in_=bk.ap, func=AF.Identity,
                    bias=bk2.ap[:, g:g + 1], scale=1.0), r=[bk, bk2], w=[KT2[g]])
        for tt in range(16):
            bk = banks[4 + tt % 2]
            for dc in range(8):
                pe(P, lambda e, dc=dc, tt=tt, bk=bk: e.matmul(
                    bk.ap[:, 0:256], lhsT=xT[dc].ap[:, tt * 128:(tt + 1) * 128], rhs=win[dc].ap[:, 1280:1536],
                    start=(dc == 0), stop=(dc == 7)), r=[win[dc], xT[dc]], w=[bk])
            dve(P, lambda e, tt=tt, bk=bk: e.tensor_tensor(out=V[tt].ap, in0=bk.ap[:, 0:256], in1=bv_bc.ap, op=ALU.add),
                r=[bk, bv_bc], w=[V[tt]])
        if stop <= 3:
            return
        units = [(h, jb) for jb in range(16) for h in range(16)]
        if stop == 4:
            units = units[:16]

        def geom(jb):
            kb0 = max(0, jb - 1)
            kb1 = min(15, jb + 1)
            nkb = kb1 - kb0 + 1
            off = 128 if jb == 0 else 0
            return kb0, nkb, off

        def stage0(u):
            h, jb = units[u]
            kb0, nkb, off = geom(jb)
            nk = nkb * 128
            g, hb, hp = h // 4, (h % 2) * 64, h // 2
            rs = u % R
            bk = banks[0 + u % 2]
            pe(P, lambda e: e.matmul(bk.ap[:, 0:nk], lhsT=QT2[hp].ap[hb:hb + 64, jb * 128:(jb + 1) * 128],
                                     rhs=KT2[g].ap[hb:hb + 64, kb0 * 128:kb0 * 128 + nk], start=True, stop=True),
               r=[QT2[hp], KT2[g]], w=[bk])
            yield
            dve(P, lambda e: e.scalar_tensor_tensor(out=Sb[rs].ap[:, 0:nk], in0=Dm.ap[:, off:off + nk],
                                                    scalar=-ATT_SLOPES[h], in1=bk.ap[:, 0:nk],
                                                    op0=ALU.mult, op1=ALU.add), r=[Dm, bk], w=[Sb[rs]])
            yield
            dve(P, lambda e: e.tensor_reduce(out=mr[rs].ap, in_=Sb[rs].ap[:, 0:nk], axis=AX.X, op=ALU.max),
                r=[Sb[rs]], w=[mr[rs]])
            yield
            dve(P, lambda e: e.tensor_scalar(out=negm[rs].ap, in0=mr[rs].ap, scalar1=sinkbc.ap[:, h:h + 1],
                                             scalar2=-1.0, op0=ALU.max, op1=ALU.mult),
                r=[mr[rs], sinkbc], w=[negm[rs]])
            yield
            act(P, lambda e: e.activation(out=Pe[rs].ap[:, 0:nk], in_=Sb[rs].ap[:, 0:nk], func=AF.Exp,
                                          bias=negm[rs].ap[:, 0:1], scale=1.0, accum_out=rsum[rs].ap),
                r=[Sb[rs], negm[rs]], w=[Pe[rs], rsum[rs]])
            yield
            act(P, lambda e: e.activation(out=es[rs].ap, in_=sinkbc.ap[:, h:h + 1], func=AF.Exp,
                                          bias=negm[rs].ap[:, 0:1], scale=1.0), r=[sinkbc, negm[rs]], w=[es[rs]])
            yield
            dve(P, lambda e: e.tensor_tensor(out=rr[rs].ap, in0=rsum[rs].ap, in1=es[rs].ap, op=ALU.add),
                r=[rsum[rs], es[rs]], w=[rr[rs]])
            yield
            dve(P, lambda e: e.reciprocal(out=rr[rs].ap, in_=rr[rs].ap), r=[rr[rs]], w=[rr[rs]])
            yield
            dve(P, lambda e: e.tensor_scalar(out=Pn[rs].ap[:, 0:nk], in0=Pe[rs].ap[:, 0:nk], scalar1=rr[rs].ap[:, 0:1],
                                             scalar2=None, op0=ALU.mult), r=[Pe[rs], rr[rs]], w=[Pn[rs]])
            yield

        def stage1(u):
            h, jb = units[u]
            kb0, nkb, off = geom(jb)
            nk = nkb * 128
            rs = u % R
            bk = banks[2 + u % 2]
            bkb = bk.ap.bitcast(BF16)
            for kb in range(nkb):
                pe(P, lambda e, kb=kb: e.transpose(bkb[:, kb * 128:(kb + 1) * 128], Pn[rs].ap[:, kb * 128:(kb + 1) * 128],
                                                   C.ident_b.ap), r=[Pn[rs], C.ident_b], w=[bk])
            act(P, lambda e: e.copy(out=PTs[rs].ap[:, 0:nk], in_=bkb[:, 0:nk]), r=[bk], w=[PTs[rs]])

        def stage2(u):
            h, jb = units[u]
            kb0, nkb, off = geom(jb)
            g, hb, hp = h // 4, (h % 2) * 64, h // 2
            rs = u % R
            bk = banks[6 + (u // 2) % 2]
            for kb in range(nkb):
                pe(P, lambda e, kb=kb: e.matmul(bk.ap[hb:hb + 64, 0:128], lhsT=V[kb0 + kb].ap[:, g * 64:(g + 1) * 64],
                                                rhs=PTs[rs].ap[:, kb * 128:(kb + 1) * 128],
                                                start=(kb == 0), stop=(kb == nkb - 1)),
                   r=[V[kb0 + kb], PTs[rs]], w=[(bk.keys[0], hb)])
            act(P, lambda e: e.copy(out=oT2[hp].ap[hb:hb + 64, jb * 128:(jb + 1) * 128], in_=bk.ap[hb:hb + 64, 0:128]),
                r=[(bk.keys[0], hb)], w=[oT2[hp]])

        U = len(units)
        from itertools import zip_longest
        for p in range(0, U + 4, 2):
            gens = [stage0(u) for u in (p, p + 1) if u < U]
            for _ in zip_longest(*gens):
                pass
            for u in (p - 2, p - 1):
                if 0 <= u < U:
                    stage1(u)
            for u in (p - 4, p - 3):
                if 0 <= u < U:
                    stage2(u)
        if stop <= 5:
            return
        for tt in range(16):
            t = sq * 16 + tt
            ys = tt % 2
            for half in range(2):
                bk = banks[4 + half]
                for hp in range(8):
                    pe(P, lambda e, hp=hp, half=half, tt=tt, bk=bk: e.matmul(
                        bk.ap, lhsT=oT2[hp].ap[:, tt * 128:(tt + 1) * 128], rhs=wo[hp].ap[:, half * 512:(half + 1) * 512],
                        start=(hp == 0), stop=(hp == 7)), r=[oT2[hp], wo[hp]], w=[bk])
                dve(P, lambda e, half=half, bk=bk, ys=ys: e.tensor_tensor(
                    out=ysb[ys].ap[:, half * 512:(half + 1) * 512], in0=bk.ap, in1=bo_bc.ap[:, half * 512:(half + 1) * 512],
                    op=ALU.add), r=[bk, bo_bc, ysb[ys]], w=[ysb[ys]])
            dma(P, lambda e, t=t, ys=ys: e.dma_start(out=H_ap[t * 128:(t + 1) * 128, :], in_=ysb[ys].ap),
                r=[ysb[ys]], w=[(H_k, t)])

    for sq in range(2):
        seq_body(sq)


def bc_mid(ap2, n):
    return ap2.unsqueeze(2).to_broadcast([ap2.shape[0], ap2.shape[1], n])


def bc_heads(ap2, nh):
    return ap2.unsqueeze(1).to_broadcast([ap2.shape[0], nh, ap2.shape[1]])


def setup_gdn_consts(P, C, al):
    f = lambda: al.new([128], F32)
    C.SC, C.Li, C.Ui = f(), f(), f()
    C.nLi, C.nUi = f(), f()
    C.NM_Ls, C.NM_Us, C.NM_Ui, C.NM_Li = f(), f(), f(), f()
    C.OnesF, C.NegOnesF, C.Half0, C.Half1 = f(), f(), f(), f()
    pool(P, lambda e: e.memset(C.SC.ap, 0.0), w=[C.SC])
    pool(P, lambda e: e.memset(C.SC.ap[0:64, 0:64], 1.0), r=[C.SC], w=[C.SC])
    pool(P, lambda e: e.memset(C.SC.ap[64:128, 64:128], 1.0), r=[C.SC], w=[C.SC])
    pool(P, lambda e: e.memset(C.OnesF.ap, 1.0), w=[C.OnesF])
    pool(P, lambda e: e.memset(C.NegOnesF.ap, -1.0), w=[C.NegOnesF])
    pool(P, lambda e: e.memset(C.Half0.ap, 0.0), w=[C.Half0])
    pool(P, lambda e: e.memset(C.Half0.ap[0:64, :], 1.0), r=[C.Half0], w=[C.Half0])
    pool(P, lambda e: e.memset(C.Half1.ap, 0.0), w=[C.Half1])
    pool(P, lambda e: e.memset(C.Half1.ap[64:128, :], 1.0), r=[C.Half1], w=[C.Half1])

    def sel(dst, pat, cm, op):
        pool(P, lambda e: e.affine_select(out=dst.ap, in_=C.SC.ap, pattern=[[pat, 128]], compare_op=op, fill=0.0,
                                          base=0, channel_multiplier=cm), r=[C.SC], w=[dst])

    def negmask(dst):
        dve(P, lambda e: e.tensor_scalar(out=dst.ap, in0=dst.ap, scalar1=-1.0, scalar2=1.0e4, op0=ALU.add, op1=ALU.mult),
            r=[dst], w=[dst])

    sel(C.Li, -1, 1, ALU.is_ge)
    sel(C.Ui, 1, -1, ALU.is_ge)
    sel(C.NM_Ls, -1, 1, ALU.is_gt)
    sel(C.NM_Us, 1, -1, ALU.is_gt)
    sel(C.NM_Li, -1, 1, ALU.is_ge)
    sel(C.NM_Ui, 1, -1, ALU.is_ge)
    for m in (C.NM_Ls, C.NM_Us, C.NM_Li, C.NM_Ui):
        negmask(m)
    dve(P, lambda e: e.tensor_scalar(out=C.nLi.ap, in0=C.Li.ap, scalar1=-1.0, scalar2=None, op0=ALU.mult), r=[C.Li], w=[C.nLi])
    dve(P, lambda e: e.tensor_scalar(out=C.nUi.ap, in0=C.Ui.ap, scalar1=-1.0, scalar2=None, op0=ALU.mult), r=[C.Ui], w=[C.nUi])


def gdn_stage(P, C, jl, X, H, W):
    nc = C.nc
    banks = C.banks
    X_ap, X_k = X
    H_ap, H_k = H
    OF, ON = C.OF, C.ON
    Win = W['a_w_in']
    al = Alloc(C.arena, C.dyn_start)
    setup_gdn_consts(P, C, al)
    gdn_dyn = al.off
    xT = [al.new([SEQ], BF16) for _ in range(8)]
    graw = al.new([16, 64], F32)
    Gt = al.new([16, 2, 16], F32)
    Bt = al.new([16, 2, 16], F32)
    wg = al.new([8, 64], F32)
    cw = al.new([32, 5], F32)
    negA = al.new([2, 16], F32)
    dtb = al.new([2, 16], F32)
    nw_bc = al.new([128], F32)
    qT = [al.new([SEQ], BF16) for _ in range(2)]
    kT = [al.new([SEQ], BF16) for _ in range(2)]
    ktm = [al.new([2, 128], BF16) for _ in range(16)]
    vtm = [al.new([4, 128], BF16) for _ in range(16)]
    wz = [al.new([512], BF16) for _ in range(8)]
    Sf = [al.new([4, 128], F32) for _ in range(2)]
    Sb = [al.new([4, 128], BF16) for _ in range(2)]
    regR = al.off
    a = Alloc(C.arena, regR)
    xa = [a.new([D], F32) for _ in range(2)]
    xTf = [a.new([D], F32) for _ in range(2)]
    wblk = [[a.new([128], BF16) for dc in range(8)] for _ in range(2)]
    hT = [a.new([SEQ + 4], BF16) for _ in range(2)]
    dg = [[a.new([128], BF16) for jt in range(5)] for _ in range(2)]
    qf = [a.new([512], F32) for _ in range(2)]
    sqb = [a.new([512], F32) for _ in range(2)]
    t1b = [a.new([512], F32) for _ in range(2)]
    ss4 = [a.new([4], F32) for _ in range(2)]
    g1_end = a.off
    a = Alloc(C.arena, regR)

    class S_:
        pass
    sets = []
    for d in range(2):
        s_ = S_()
        s_.XA, s_.XB, s_.XTA, s_.XTB, s_.PTA, s_.PTB = [a.new([4, 128], F32) for _ in range(6)]
        s_.Gb, s_.RG = s_.XB, s_.XTB
        s_.dS = s_.PTB
        s_.dT = a.new([4, 128], F32)
        s_.u = s_.dT
        s_.otmp = a.new([4, 128], F32)
        s_.Tb = a.new([4, 128], BF16)
        s_.vb = a.new([4, 128], BF16)
        s_.kbg = a.new([4, 128], BF16)
        s_.kd = a.new([4, 128], BF16)
        s_.wT = a.new([4, 128], BF16)
        s_.qkT = a.new([4, 128], BF16)
        s_.vn = a.new([4, 128], BF16)
        s_.osb = a.new([4, 128], F32)
        s_.gc = a.new([4], F32)
        s_.tot = a.new([4], F32)
        s_.egc = a.new([4], F32)
        s_.bge = a.new([4], F32)
        s_.ekd = a.new([4], F32)
        s_.nbeta = a.new([4], F32)
        s_.gl = a.new([2, 4], F32)
        s_.bk = banks[4 * d:4 * d + 4]
        sets.append(s_)
    scan_end = a.off
    a = Alloc(C.arena, regR)
    of_t = [a.new([512], F32) for _ in range(2)]
    ob_t = [a.new([512], F32) for _ in range(2)]
    zs = [a.new([512], F32) for _ in range(2)]
    sq5 = [a.new([512], F32) for _ in range(2)]
    ms5 = [a.new([4], F32) for _ in range(2)]
    onb = [a.new([512], BF16) for _ in range(2)]

    jl_ = jl
    dma(P, lambda e: e.dma_start(out=wg.ap, in_=Win[jl_, :, 6144:6208].rearrange("(c p) n -> p c n", p=128)), w=[wg])
    dma(P, lambda e: e.dma_start(out=negA.ap.rearrange("p a b -> p (a b)"),
                                 in_=W['a_A_log'][jl_:jl_ + 1].rearrange("o a b -> o (a b)").partition_broadcast(128)), w=[negA])
    dma(P, lambda e: e.dma_start(out=dtb.ap.rearrange("p a b -> p (a b)"),
                                 in_=W['a_dt_bias'][jl_:jl_ + 1].rearrange("o a b -> o (a b)").partition_broadcast(128)), w=[dtb])
    dma(P, lambda e: e.dma_start(out=nw_bc.ap, in_=W['a_norm_w'][jl_:jl_ + 1, :].partition_broadcast(128)), w=[nw_bc])
    act(P, lambda e: e.activation(out=negA.ap, in_=negA.ap, func=AF.Exp), r=[negA], w=[negA])
    dve(P, lambda e: e.tensor_scalar(out=negA.ap, in0=negA.ap, scalar1=-1.0, scalar2=None, op0=ALU.mult), r=[negA], w=[negA])
    cwrow = Alloc(C.arena, regR).new([4096], F32, parts=5)
    dma(P, lambda e: e.dma_start(out=cwrow.ap, in_=W['a_conv_w'][jl_]), w=[cwrow])
    for blk in range(32):
        pe(P, lambda e, blk=blk: e.transpose(banks[7].ap[:, blk * 5:(blk + 1) * 5], cwrow.ap[0:5, blk * 128:(blk + 1) * 128],
                                             C.ident_f.ap[0:5, 0:5]), r=[cwrow, C.ident_f], w=[banks[7]])
    dve(P, lambda e: e.tensor_copy(out=cw.ap.rearrange("p a b -> p (a b)"), in_=banks[7].ap[:, 0:160]), r=[banks[7]], w=[cw])

    def seq_body(sq):
        for tt in range(16):
            t = sq * 16 + tt
            s = tt % 2
            rows = slice(t * 128, (t + 1) * 128)
            dma(P, lambda e, s=s, rows=rows: e.dma_start(out=xa[s].ap, in_=X_ap[rows, :]), r=[(X_k, t)], w=[xa[s]])
            bA, bB = (banks[0], banks[1]) if s == 0 else (banks[2], banks[3])
            for c in range(8):
                bk = bA if c < 4 else bB
                pe(P, lambda e, c=c, bk=bk, s=s: e.transpose(bk.ap[:, (c % 4) * 128:(c % 4 + 1) * 128],
                                                           xa[s].ap[:, c * 128:(c + 1) * 128], C.ident_f.ap),
                   r=[xa[s], C.ident_f], w=[bk])
            act(P, lambda e, s=s, bk=bA: e.copy(out=xTf[s].ap[:, 0:512], in_=bk.ap), r=[bA, xTf[s]], w=[xTf[s]])
            act(P, lambda e, s=s, bk=bB: e.copy(out=xTf[s].ap[:, 512:1024], in_=bk.ap), r=[bB, xTf[s]], w=[xTf[s]])
            for c in range(8):
                dve(P, lambda e, c=c, s=s, tt=tt: e.tensor_copy(out=xT[c].ap[:, tt * 128:(tt + 1) * 128],
                                                              in_=xTf[s].ap[:, c * 128:(c + 1) * 128]),
                    r=[xTf[s]], w=[xT[c]])
            bg = banks[4 + s]
            for c in range(8):
                pe(P, lambda e, c=c, s=s, bg=bg: e.matmul(bg.ap[:, 0:64], lhsT=xTf[s].ap[:, c * 128:(c + 1) * 128],
                                                        rhs=wg.ap[:, c, :], start=(c == 0), stop=(c == 7)),
                   r=[xTf[s], wg], w=[bg])
            dve(P, lambda e, tt=tt, bg=bg: e.tensor_copy(out=graw.ap[:, tt, :], in_=bg.ap[:, 0:64]), r=[bg], w=[graw])
        g4 = graw.ap.rearrange("p t (d b h) -> p t d b h", d=2, b=2)
        for d in range(2):
            act(P, lambda e, d=d: e.activation(out=Bt.ap[:, :, d, :], in_=g4[:, :, d, 0, :], func=AF.Sigmoid),
                r=[graw], w=[Bt])
            dve(P, lambda e, d=d: e.tensor_tensor(out=Gt.ap[:, :, d, :], in0=g4[:, :, d, 1, :],
                                                  in1=bc_heads(dtb.ap[:, d, :], 16), op=ALU.add), r=[graw, dtb], w=[Gt])
        act(P, lambda e: e.activation(out=Gt.ap, in_=Gt.ap, func=AF.Exp), r=[Gt], w=[Gt])
        act(P, lambda e: e.activation(out=Gt.ap, in_=Gt.ap, func=AF.Ln, bias=1.0, scale=1.0), r=[Gt], w=[Gt])
        for d in range(2):
            dve(P, lambda e, d=d: e.tensor_tensor(out=Gt.ap[:, :, d, :], in0=Gt.ap[:, :, d, :],
                                                  in1=bc_heads(negA.ap[:, d, :], 16), op=ALU.mult), r=[Gt, negA], w=[Gt])
        for hg in range(4):
            hg_body(sq, hg)

    def hg_body(sq, hg):
        blocks = [('q', 0, 2 * hg, 2 * hg * 128), ('q', 1, 2 * hg + 1, (2 * hg + 1) * 128),
                  ('k', 0, 8 + 2 * hg, 1024 + 2 * hg * 128), ('k', 1, 8 + 2 * hg + 1, 1024 + (2 * hg + 1) * 128)]
        for vi in range(4):
            blocks.append(('v', vi, 16 + 4 * hg + vi, 2048 + (4 * hg + vi) * 128))
        for dc in range(8):
            dma(P, lambda e, dc=dc: e.dma_start(out=wz[dc].ap, in_=Win[jl_, dc * 128:(dc + 1) * 128,
                                                                    4096 + hg * 512:4096 + (hg + 1) * 512]),
                w=[wz[dc]], q='pool')
        for hb_ in range(2):
            dve(P, lambda e, hb_=hb_: e.memset(hT[hb_].ap[:, 0:2], 0.0), r=[hT[hb_]], w=[hT[hb_]])
            dve(P, lambda e, hb_=hb_: e.memset(hT[hb_].ap[:, SEQ + 2:SEQ + 4], 0.0), r=[hT[hb_]], w=[hT[hb_]])
        for bi, (kind, li_, blk, col0) in enumerate(blocks):
            ws = bi % 2
            for dc in range(8):
                dma(P, lambda e, dc=dc, ws=ws, col0=col0: e.dma_start(
                    out=wblk[ws][dc].ap, in_=Win[jl_, dc * 128:(dc + 1) * 128, col0:col0 + 128]),
                    w=[wblk[ws][dc]], q='pool')
            hb = hT[ws]
            for tq in range(4):
                bk = banks[4 + tq % 2]
                for dc in range(8):
                    pe(P, lambda e, dc=dc, tq=tq, bk=bk, ws=ws: e.matmul(
                        bk.ap, lhsT=wblk[ws][dc].ap, rhs=xT[dc].ap[:, tq * 512:(tq + 1) * 512],
                        start=(dc == 0), stop=(dc == 7)), r=[wblk[ws][dc], xT[dc]], w=[bk])
                act(P, lambda e, tq=tq, bk=bk, hb=hb: e.copy(out=hb.ap[:, 2 + tq * 512:2 + (tq + 1) * 512], in_=bk.ap),
                    r=[bk, hb], w=[hb])
            for jt in range(5):
                dve(P, lambda e, jt=jt, ws=ws, blk=blk: e.tensor_scalar(
                    out=dg[ws][jt].ap, in0=C.ident_b.ap, scalar1=cw.ap[:, blk, jt:jt + 1], scalar2=None, op0=ALU.mult),
                    r=[C.ident_b, cw], w=[dg[ws][jt]])
            if kind in ('q', 'k'):
                dst = qT[li_] if kind == 'q' else kT[li_]
                ebias = float(-0.5 * np.log(128.0)) if kind == 'q' else 0.0
                for tq in range(4):
                    bk = banks[6 + tq % 2]
                    r_ = tq % 2
                    for jt in range(5):
                        pe(P, lambda e, jt=jt, tq=tq, bk=bk, ws=ws, hb=hb: e.matmul(
                            bk.ap, lhsT=dg[ws][jt].ap, rhs=hb.ap[:, tq * 512 + jt:tq * 512 + jt + 512],
                            start=(jt == 0), stop=(jt == 4)), r=[dg[ws][jt], hb], w=[bk])
                    act(P, lambda e, bk=bk, r_=r_: e.activation(out=qf[r_].ap, in_=bk.ap, func=AF.Silu), r=[bk], w=[qf[r_]])
                    dve(P, lambda e, r_=r_: e.tensor_tensor(out=sqb[r_].ap, in0=qf[r_].ap, in1=qf[r_].ap, op=ALU.mult),
                        r=[qf[r_]], w=[sqb[r_]])
                    bo = banks[0 + tq % 2]
                    pe(P, lambda e, bo=bo, r_=r_: e.matmul(bo.ap, lhsT=C.OnesF.ap, rhs=sqb[r_].ap, start=True, stop=True),
                       r=[C.OnesF, sqb[r_]], w=[bo])
                    act(P, lambda e, bo=bo, r_=r_: e.activation(out=t1b[r_].ap, in_=bo.ap, func=AF.Ln, bias=1.0e-6, scale=1.0),
                        r=[bo], w=[t1b[r_]])
                    act(P, lambda e, r_=r_, ebias=ebias: e.activation(out=t1b[r_].ap, in_=t1b[r_].ap, func=AF.Exp,
                                                                    bias=ebias, scale=-0.5), r=[t1b[r_]], w=[t1b[r_]])
                    dve(P, lambda e, r_=r_, tq=tq, dst=dst: e.tensor_tensor(out=dst.ap[:, tq * 512:(tq + 1) * 512],
                                                                          in0=qf[r_].ap, in1=t1b[r_].ap, op=ALU.mult),
                        r=[qf[r_], t1b[r_]], w=[dst])
            if kind in ('k', 'v'):
                for t4 in range(4):
                    bk = banks[2 + t4 % 2]
                    r_ = t4 % 2
                    for ti in range(4):
                        tt = t4 * 4 + ti
                        for jt in range(5):
                            pe(P, lambda e, jt=jt, tt=tt, ti=ti, bk=bk, ws=ws, hb=hb: e.matmul(
                                bk.ap[:, ti * 128:(ti + 1) * 128], lhsT=hb.ap[:, tt * 128 + jt:tt * 128 + jt + 128],
                                rhs=dg[ws][jt].ap, start=(jt == 0), stop=(jt == 4)), r=[dg[ws][jt], hb], w=[bk])
                    if kind == 'v':
                        for ti in range(4):
                            tt = t4 * 4 + ti
                            act(P, lambda e, bk=bk, ti=ti, tt=tt, li_=li_: e.activation(
                                out=vtm[tt].ap[:, li_, :], in_=bk.ap[:, ti * 128:(ti + 1) * 128], func=AF.Silu),
                                r=[bk], w=[vtm[tt]])
                    else:
                        act(P, lambda e, bk=bk, r_=r_: e.activation(out=qf[r_].ap, in_=bk.ap, func=AF.Silu), r=[bk], w=[qf[r_]])
                        dve(P, lambda e, r_=r_: e.tensor_tensor(out=sqb[r_].ap, in0=qf[r_].ap, in1=qf[r_].ap, op=ALU.mult),
                            r=[qf[r_]], w=[sqb[r_]])
                        dve(P, lambda e, r_=r_: e.tensor_reduce(out=ss4[r_].ap, in_=sqb[r_].ap.rearrange("p (a b) -> p a b", a=4),
                                                                axis=AX.X, op=ALU.add), r=[sqb[r_]], w=[ss4[r_]])
                        dve(P, lambda e, r_=r_: e.tensor_scalar(out=ss4[r_].ap, in0=ss4[r_].ap, scalar1=1.0e-6, scalar2=None,
                                                                op0=ALU.add), r=[ss4[r_]], w=[ss4[r_]])
                        act(P, lambda e, r_=r_: e.activation(out=ss4[r_].ap, in_=ss4[r_].ap, func=AF.Sqrt), r=[ss4[r_]], w=[ss4[r_]])
                        dve(P, lambda e, r_=r_: e.reciprocal(out=ss4[r_].ap, in_=ss4[r_].ap), r=[ss4[r_]], w=[ss4[r_]])
                        for ti in range(4):
                            tt = t4 * 4 + ti
                            dve(P, lambda e, r_=r_, ti=ti, tt=tt, li_=li_: e.tensor_scalar(
                                out=ktm[tt].ap[:, li_, :], in0=qf[r_].ap[:, ti * 128:(ti + 1) * 128],
                                scalar1=ss4[r_].ap[:, ti:ti + 1], scalar2=None, op0=ALU.mult),
                                r=[qf[r_], ss4[r_]], w=[ktm[tt]])
        for d in range(2):
            dve(P, lambda e, d=d: e.memset(Sf[d].ap, 0.0), r=[Sf[d]], w=[Sf[d]])
            dve(P, lambda e, d=d: e.memset(Sb[d].ap, 0.0), r=[Sb[d]], w=[Sb[d]])
        from itertools import zip_longest
        for step in range(16):
            for _ in zip_longest(unit(sq, hg, 0, step), unit(sq, hg, 1, 15 - step)):
                pass
        for tt in range(16):
            t = sq * 16 + tt
            r_ = tt % 2
            rows = slice(t * 128, (t + 1) * 128)
            cols = slice(hg * 512, (hg + 1) * 512)
            dma(P, lambda e, r_=r_, rows=rows, cols=cols: e.dma_start(out=of_t[r_].ap, in_=OF[0, rows, cols]),
                r=[('OF', 0, t, hg)], w=[of_t[r_]])
            dma(P, lambda e, r_=r_, rows=rows, cols=cols: e.dma_start(out=ob_t[r_].ap, in_=OF[1, rows, cols]),
                r=[('OF', 1, t, hg)], w=[ob_t[r_]])
            bk = banks[tt % 2]
            for dc in range(8):
                pe(P, lambda e, dc=dc, tt=tt, bk=bk: e.matmul(bk.ap, lhsT=xT[dc].ap[:, tt * 128:(tt + 1) * 128], rhs=wz[dc].ap,
                                                              start=(dc == 0), stop=(dc == 7)), r=[xT[dc], wz[dc]], w=[bk])
            act(P, lambda e, bk=bk, r_=r_: e.activation(out=zs[r_].ap, in_=bk.ap, func=AF.Silu), r=[bk], w=[zs[r_]])
            dve(P, lambda e, r_=r_: e.tensor_tensor(out=of_t[r_].ap, in0=of_t[r_].ap, in1=ob_t[r_].ap, op=ALU.add),
                r=[of_t[r_], ob_t[r_]], w=[of_t[r_]])
            dve(P, lambda e, r_=r_: e.tensor_tensor(out=sq5[r_].ap, in0=of_t[r_].ap, in1=of_t[r_].ap, op=ALU.mult),
                r=[of_t[r_]], w=[sq5[r_]])
            dve(P, lambda e, r_=r_: e.tensor_reduce(out=ms5[r_].ap, in_=sq5[r_].ap.rearrange("p (a b) -> p a b", a=4),
                                                    axis=AX.X, op=ALU.add), r=[sq5[r_]], w=[ms5[r_]])
            dve(P, lambda e, r_=r_: e.tensor_scalar(out=ms5[r_].ap, in0=ms5[r_].ap, scalar1=1.0 / 128.0, scalar2=1.0e-6,
                                                    op0=ALU.mult, op1=ALU.add), r=[ms5[r_]], w=[ms5[r_]])
            act(P, lambda e, r_=r_: e.activation(out=ms5[r_].ap, in_=ms5[r_].ap, func=AF.Sqrt), r=[ms5[r_]], w=[ms5[r_]])
            dve(P, lambda e, r_=r_: e.reciprocal(out=ms5[r_].ap, in_=ms5[r_].ap), r=[ms5[r_]], w=[ms5[r_]])
            o3 = lambda b: b.ap.rearrange("p (a b) -> p a b", a=4)
            dve(P, lambda e, r_=r_: e.tensor_tensor(out=o3(of_t[r_]), in0=o3(of_t[r_]), in1=bc_mid(ms5[r_].ap, 128), op=ALU.mult),
                r=[of_t[r_], ms5[r_]], w=[of_t[r_]])
            pool(P, lambda e, r_=r_: e.tensor_tensor(out=o3(of_t[r_]), in0=o3(of_t[r_]), in1=bc_heads(nw_bc.ap, 4), op=ALU.mult),
                 r=[of_t[r_], nw_bc], w=[of_t[r_]])
            dve(P, lambda e, r_=r_: e.tensor_tensor(out=onb[r_].ap, in0=of_t[r_].ap, in1=zs[r_].ap, op=ALU.mult),
                r=[of_t[r_], zs[r_]], w=[onb[r_]])
            dma(P, lambda e, r_=r_, rows=rows, cols=cols: e.dma_start(out=ON[rows, cols], in_=onb[r_].ap),
                r=[onb[r_]], w=[('ON', t, hg)])

    def unit(sq, hg, d, tt):
        S = sets[d]
        b0, b1, b2, b3 = S.bk
        t = sq * 16 + tt
        tk = slice(tt * 128, (tt + 1) * 128)
        Tri, nTri = (C.Ui, C.nUi) if d == 0 else (C.Li, C.nLi)
        NMs = C.NM_Ls if d == 0 else C.NM_Us
        NMi = C.NM_Ui if d == 0 else C.NM_Li
        h0 = hg * 4
        g4 = Gt.ap[:, tt, d, h0:h0 + 4]
        be4 = Bt.ap[:, tt, d, h0:h0 + 4]
        v3 = lambda b: b.ap
        pool(P, lambda e: e.tensor_copy(out=S.Gb.ap, in_=bc_mid(g4, 128)), r=[Gt], w=[S.Gb])
        pool(P, lambda e: e.tensor_tensor(out=S.RG.ap, in0=S.Gb.ap, in1=bc_heads(Tri.ap, 4), op=ALU.mult),
             r=[S.Gb, Tri], w=[S.RG])
        f2 = lambda b: b.ap.rearrange("p a b -> p (a b)")
        pe(P, lambda e: e.matmul(b0.ap, lhsT=Tri.ap, rhs=f2(S.Gb), start=True, stop=False), r=[Tri, S.Gb], w=[b0])
        pe(P, lambda e: e.matmul(b0.ap, lhsT=C.NegOnesF.ap, rhs=f2(S.RG), start=False, stop=True), r=[C.NegOnesF, S.RG], w=[b0])
        pe(P, lambda e: e.matmul(b1.ap, lhsT=C.OnesF.ap, rhs=f2(S.RG), start=True, stop=False), r=[C.OnesF, S.RG], w=[b1])
        pe(P, lambda e: e.matmul(b1.ap, lhsT=nTri.ap, rhs=f2(S.Gb), start=False, stop=True), r=[nTri, S.Gb], w=[b1])
        for kh in range(2):
            pe(P, lambda e, kh=kh: e.matmul(b2.ap[:, kh * 128:(kh + 1) * 128], lhsT=kT[kh].ap[:, tk], rhs=kT[kh].ap[:, tk],
                                            start=True, stop=True), r=[kT[kh]], w=[b2])
            pe(P, lambda e, kh=kh: e.matmul(b2.ap[:, 256 + kh * 128:256 + (kh + 1) * 128], lhsT=kT[kh].ap[:, tk],
                                            rhs=qT[kh].ap[:, tk], start=True, stop=True), r=[kT[kh], qT[kh]], w=[b2])
        pe(P, lambda e: e.matmul(b3.ap[:, 0:4], lhsT=Tri.ap, rhs=g4, start=True, stop=True), r=[Tri, Gt], w=[b3])
        pe(P, lambda e: e.matmul(b3.ap[:, 4:8], lhsT=C.SC.ap, rhs=g4, start=True, stop=True), r=[C.SC, Gt], w=[b3])
        pe(P, lambda e: e.matmul(b3.ap[:, 8:12], lhsT=C.Half0.ap, rhs=g4, start=True, stop=True), r=[C.Half0, Gt], w=[b3])
        pe(P, lambda e: e.matmul(b3.ap[:, 12:16], lhsT=C.Half1.ap, rhs=g4, start=True, stop=True), r=[C.Half1, Gt], w=[b3])
        dve(P, lambda e: e.tensor_copy(out=S.gc.ap, in_=b3.ap[:, 0:4]), r=[b3], w=[S.gc])
        act(P, lambda e: e.activation(out=S.egc.ap, in_=b3.ap[:, 0:4], func=AF.Exp), r=[b3], w=[S.egc])
        dve(P, lambda e: e.tensor_tensor(out=S.ekd.ap, in0=b3.ap[:, 4:8], in1=S.gc.ap, op=ALU.subtract), r=[b3, S.gc], w=[S.ekd])
        act(P, lambda e: e.activation(out=S.ekd.ap, in_=S.ekd.ap, func=AF.Exp), r=[S.ekd], w=[S.ekd])
        act(P, lambda e: e.activation(out=S.gl.ap.rearrange("p a b -> p (a b)"), in_=b3.ap[:, 8:16], func=AF.Exp), r=[b3], w=[S.gl])
        dve(P, lambda e: e.tensor_tensor(out=S.bge.ap, in0=S.egc.ap, in1=be4, op=ALU.mult), r=[S.egc, Bt], w=[S.bge])
        dve(P, lambda e: e.tensor_scalar(out=S.nbeta.ap, in0=be4, scalar1=-1.0, scalar2=None, op0=ALU.mult), r=[Bt], w=[S.nbeta])
        dve(P, lambda e: e.tensor_tensor(out=S.dS.ap, in0=b0.ap.rearrange("p (a b) -> p a b", a=4), in1=bc_heads(NMs.ap, 4), op=ALU.add),
            r=[b0, NMs], w=[S.dS])
        act(P, lambda e: e.activation(out=S.dS.ap, in_=S.dS.ap, func=AF.Exp), r=[S.dS], w=[S.dS])
        dve(P, lambda e: e.tensor_tensor(out=S.dT.ap, in0=b1.ap.rearrange("p (a b) -> p a b", a=4), in1=bc_heads(NMi.ap, 4), op=ALU.add),
            r=[b1, NMi], w=[S.dT])
        act(P, lambda e: e.activation(out=S.dT.ap, in_=S.dT.ap, func=AF.Exp), r=[S.dT], w=[S.dT])
        yield
        for kh in range(2):
            dve(P, lambda e, kh=kh: e.tensor_tensor(out=S.qkT.ap[:, 2 * kh:2 * kh + 2, :], in0=S.dT.ap[:, 2 * kh:2 * kh + 2, :],
                                                   in1=bc_heads(b2.ap[:, 256 + kh * 128:256 + (kh + 1) * 128], 2), op=ALU.mult),
                r=[S.dT, b2], w=[S.qkT])
        for h in range(4):
            dve(P, lambda e, h=h: e.scalar_tensor_tensor(out=S.XA.ap[:, h, :], in0=S.dS.ap[:, h, :], scalar=S.nbeta.ap[:, h:h + 1],
                                                         in1=b2.ap[:, (h // 2) * 128:(h // 2 + 1) * 128],
                                                         op0=ALU.mult, op1=ALU.mult), r=[S.dS, S.nbeta, b2], w=[S.XA])
        pool(P, lambda e: e.tensor_tensor(out=S.vb.ap, in0=vtm[tt].ap, in1=bc_mid(be4, 128), op=ALU.mult), r=[vtm[tt], Bt], w=[S.vb])
        for h in range(4):
            pool(P, lambda e, h=h: e.tensor_scalar(out=S.kbg.ap[:, h, :], in0=ktm[tt].ap[:, h // 2, :], scalar1=S.bge.ap[:, h:h + 1],
                                                   scalar2=1.0, op0=ALU.mult, op1=ALU.mult), r=[ktm[tt], S.bge], w=[S.kbg])
            pool(P, lambda e, h=h: e.tensor_scalar(out=S.kd.ap[:, h, :], in0=ktm[tt].ap[:, h // 2, :], scalar1=S.ekd.ap[:, h:h + 1],
                                                   scalar2=1.0, op0=ALU.mult, op1=ALU.mult), r=[ktm[tt], S.ekd], w=[S.kd])
        yield
        for h in range(4):
            pe(P, lambda e, h=h: e.transpose(b0.ap[:, h * 128:(h + 1) * 128], S.XA.ap[:, h, :], C.ident_f.ap),
               r=[S.XA, C.ident_f], w=[b0])
        act(P, lambda e: e.copy(out=f2(S.XTA), in_=b0.ap), r=[b0], w=[S.XTA])
        dve(P, lambda e: e.tensor_tensor(out=S.PTA.ap, in0=S.XTA.ap, in1=bc_heads(C.ident_f.ap, 4), op=ALU.add),
            r=[S.XTA, C.ident_f], w=[S.PTA])
        yield
        Xc, XTc, PTc = S.XA, S.XTA, S.PTA
        Xn_, XTn_, PTn_ = S.XB, S.XTB, S.PTB
        for lvl in range(5):
            last = (lvl == 4)
            for h in range(4):
                pe(P, lambda e, h=h, Xc=Xc, XTc=XTc: e.matmul(b0.ap[:, h * 128:(h + 1) * 128], lhsT=XTc.ap[:, h, :], rhs=Xc.ap[:, h, :],
                                                             start=True, stop=True), r=[Xc, XTc], w=[b0])
            if not last:
                for h in range(4):
                    pe(P, lambda e, h=h, Xc=Xc, XTc=XTc: e.matmul(b1.ap[:, h * 128:(h + 1) * 128], lhsT=Xc.ap[:, h, :],
                                                                 rhs=XTc.ap[:, h, :], start=True, stop=True), r=[Xc, XTc], w=[b1])
            act(P, lambda e, Xn_=Xn_: e.copy(out=f2(Xn_), in_=b0.ap), r=[b0], w=[Xn_])
            if not last:
                dve(P, lambda e, XTn_=XTn_: e.tensor_copy(out=f2(XTn_), in_=b1.ap), r=[b1], w=[XTn_])
            yield
            for h in range(4):
                pe(P, lambda e, h=h, Xn_=Xn_, PTc=PTc: e.matmul(b2.ap[:, h * 128:(h + 1) * 128], lhsT=Xn_.ap[:, h, :],
                                                               rhs=PTc.ap[:, h, :], start=True, stop=True), r=[Xn_, PTc], w=[b2])
            dve(P, lambda e, PTn_=PTn_, PTc=PTc: e.tensor_tensor(out=f2(PTn_), in0=b2.ap, in1=f2(PTc), op=ALU.add),
                r=[b2, PTc], w=[PTn_])
            yield
            Xc, Xn_ = Xn_, Xc
            XTc, XTn_ = XTn_, XTc
            PTc, PTn_ = PTn_, PTc
        act(P, lambda e, PTc=PTc: e.copy(out=S.Tb.ap, in_=PTc.ap), r=[PTc], w=[S.Tb])
        for h in range(4):
            pe(P, lambda e, h=h: e.matmul(b0.ap[:, h * 128:(h + 1) * 128], lhsT=S.Tb.ap[:, h, :], rhs=S.vb.ap[:, h, :],
                                          start=True, stop=True), r=[S.Tb, S.vb], w=[b0])
        for h in range(4):
            pe(P, lambda e, h=h: e.matmul(b1.ap[:, h * 128:(h + 1) * 128], lhsT=S.kbg.ap[:, h, :], rhs=S.Tb.ap[:, h, :],
                                          start=True, stop=True), r=[S.kbg, S.Tb], w=[b1])
        dve(P, lambda e: e.tensor_copy(out=f2(S.u), in_=b0.ap), r=[b0], w=[S.u])
        act(P, lambda e: e.copy(out=f2(S.wT), in_=b1.ap), r=[b1], w=[S.wT])
        yield
        for c in ((0, 1) if d == 0 else (1, 0)):
            cs = slice(c * 64, (c + 1) * 64)
            ck = (b2.keys[0], c)
            for h in range(4):
                pe(P, lambda e, h=h, cs=cs: e.matmul(b2.ap[cs, h * 128:(h + 1) * 128], lhsT=S.wT.ap[:, h, cs], rhs=Sb[d].ap[:, h, :],
                                                     start=True, stop=True), r=[S.wT, Sb[d]], w=[b2])
            for h in range(4):
                pe(P, lambda e, h=h, cs=cs, c=c: e.matmul(b3.ap[cs, h * 128:(h + 1) * 128], lhsT=qT[h // 2].ap[:, tt * 128 + c * 64:tt * 128 + (c + 1) * 64],
                                                     rhs=Sb[d].ap[:, h, :], start=True, stop=True), r=[qT[h // 2], Sb[d]], w=[b3])
            dve(P, lambda e, cs=cs: e.tensor_tensor(out=f2(S.vn)[cs, :], in0=f2(S.u)[cs, :], in1=b2.ap[cs, :], op=ALU.subtract),
                r=[S.u, b2], w=[S.vn])
            dbg = getattr(C, 'dbg_unit', None)
            if dbg and (sq, hg, d, tt) == (0, 0, 0, 1) and c == 0:
                dma(P, lambda e: e.dma_start(out=dbg['Sb'], in_=f2(Sb[d])), r=[Sb[d]], w=['dbgSb'])
                dma(P, lambda e: e.dma_start(out=dbg['vn'], in_=f2(S.vn)), r=[S.vn], w=['dbgvn'])
                dma(P, lambda e: e.dma_start(out=dbg['u'], in_=f2(S.u)), r=[S.u], w=['dbgu'])
                dma(P, lambda e: e.dma_start(out=dbg['egc'], in_=S.egc.ap), r=[S.egc], w=['dbgegc'])
                dma(P, lambda e: e.dma_start(out=dbg['qkT'], in_=f2(S.qkT)), r=[S.qkT], w=['dbgqkT'])
                dve(P, lambda e: e.tensor_copy(out=f2(S.osb), in_=b3.ap), r=[b3], w=[S.osb])
                dma(P, lambda e: e.dma_start(out=dbg['po1'], in_=f2(S.osb)), r=[S.osb], w=['dbgpo1'])
            yield
            for h in range(4):
                pe(P, lambda e, h=h, cs=cs: e.matmul(b0.ap[cs, h * 128:(h + 1) * 128], lhsT=S.qkT.ap[cs, h, cs], rhs=S.vn.ap[cs, h, :],
                                                     start=True, stop=True), r=[S.qkT, S.vn], w=[b0])
            for h in range(4):
                pe(P, lambda e, h=h, cs=cs: e.matmul(b1.ap[:, h * 128:(h + 1) * 128], lhsT=S.kd.ap[cs, h, :], rhs=S.vn.ap[cs, h, :],
                                                     start=True, stop=True), r=[S.kd, S.vn], w=[b1])
            dve(P, lambda e, cs=cs: e.tensor_tensor(out=S.otmp.ap[cs], in0=b3.ap[cs, :].rearrange("p (a b) -> p a b", a=4),
                                                    in1=bc_mid(S.egc.ap[cs, :], 128), op=ALU.mult), r=[b3, S.egc], w=[S.otmp])
            if dbg and (sq, hg, d, tt) == (0, 0, 0, 1) and c == 0:
                dve(P, lambda e: e.tensor_copy(out=f2(S.osb), in_=b0.ap), r=[b0], w=[S.osb])
                dma(P, lambda e: e.dma_start(out=dbg['po2'], in_=f2(S.osb)), r=[S.osb], w=['dbgpo2'])
                dma(P, lambda e: e.dma_start(out=dbg['otmp'], in_=f2(S.otmp)), r=[S.otmp], w=['dbgotmp'])
            dve(P, lambda e, cs=cs: e.tensor_tensor(out=f2(S.osb)[cs, :], in0=f2(S.otmp)[cs, :], in1=b0.ap[cs, :], op=ALU.add),
                r=[S.otmp, b0], w=[S.osb])
            for h in range(4):
                dve(P, lambda e, h=h, c=c: e.scalar_tensor_tensor(out=Sf[d].ap[:, h, :], in0=Sf[d].ap[:, h, :],
                                                                  scalar=S.gl.ap[:, c, h:h + 1], in1=b1.ap[:, h * 128:(h + 1) * 128],
                                                                  op0=ALU.mult, op1=ALU.add), r=[Sf[d], S.gl, b1], w=[Sf[d]])
            act(P, lambda e: e.copy(out=Sb[d].ap, in_=Sf[d].ap), r=[Sf[d]], w=[Sb[d]])
            yield
        dma(P, lambda e: e.dma_start(out=OF[d, t * 128:(t + 1) * 128, hg * 512:(hg + 1) * 512], in_=f2(S.osb)),
            r=[S.osb], w=[('OF', d, t, hg)])

    for sq in range(2):
        seq_body(sq)
    a = Alloc(C.arena, gdn_dyn)
    wo = [a.new([D], BF16) for _ in range(16)]
    ont = [a.new([2048], BF16) for _ in range(2)]
    onT = [a.new([16, 128], BF16) for _ in range(2)]
    ysb = [a.new([D], F32) for _ in range(2)]
    for c in range(16):
        dma(P, lambda e, c=c: e.dma_start(out=wo[c].ap, in_=W['a_w_out'][jl_, c * 128:(c + 1) * 128, :]), w=[wo[c]], q='pool')
    for t in range(NT):
        s = t % 2
        rows = slice(t * 128, (t + 1) * 128)
        dma(P, lambda e, s=s, rows=rows: e.dma_start(out=ont[s].ap, in_=ON[rows, :]), r=[('ON', t, hg) for hg in range(4)], w=[ont[s]])
        for c4 in range(4):
            bk = banks[c4 % 2]
            bkb = bk.ap.bitcast(BF16)
            for ci in range(4):
                c = c4 * 4 + ci
                pe(P, lambda e, c=c, ci=ci, s=s, bkb=bkb: e.transpose(bkb[:, ci * 128:(ci + 1) * 128], ont[s].ap[:, c * 128:(c + 1) * 128],
                                                                     C.ident_b.ap), r=[ont[s], C.ident_b], w=[bk])
            act(P, lambda e, c4=c4, s=s, bkb=bkb: e.copy(out=onT[s].ap[:, c4 * 4:(c4 + 1) * 4, :].rearrange("p a b -> p (a b)"),
                                                       in_=bkb[:, 0:512]), r=[bk, onT[s]], w=[onT[s]])
        for half in range(2):
            bk = banks[2 + half]
            for c in range(16):
                pe(P, lambda e, c=c, half=half, s=s, bk=bk: e.matmul(bk.ap, lhsT=onT[s].ap[:, c, :], rhs=wo[c].ap[:, half * 512:(half + 1) * 512],
                                                                   start=(c == 0), stop=(c == 15)), r=[onT[s], wo[c]], w=[bk])
            act(P, lambda e, half=half, s=s, bk=bk: e.copy(out=ysb[s].ap[:, half * 512:(half + 1) * 512], in_=bk.ap),
                r=[bk, ysb[s]], w=[ysb[s]])
        dma(P, lambda e, s=s, rows=rows: e.dma_start(out=H_ap[rows, :], in_=ysb[s].ap), r=[ysb[s]], w=[(H_k, t)])


_W_SPECS = [
    ('a_w_in', [2, 1024, 6208]), ('a_conv_w', [2, 5, 4096]), ('a_A_log', [2, 2, 16]), ('a_dt_bias', [2, 2, 16]),
    ('a_norm_w', [2, 128]), ('a_w_out', [2, 2048, 1024]), ('b_w_in', [2, 1024, 1536]), ('b_b_in', [2, 1536]),
    ('b_sinks', [2, 16]), ('b_w_out', [2, 1024, 1024]), ('b_b_out', [2, 1024]), ('router_w', [4, 1024, 32]),
    ('router_b', [4, 32]), ('exp_w_up', [4, 32, 1024, 2048]), ('exp_b_up', [4, 32, 2048]),
    ('exp_w_down', [4, 32, 1024, 1024]), ('exp_b_down', [4, 32, 1024]), ('ln_g', [4, 2, 1024]), ('ln_b', [4, 2, 1024]),
]


def build_program(depth=DEPTH):
    nc = bass.Bass("TRN2", target_bir_lowering=False)
    dt = lambda n, s, k="ExternalInput", d=F32: nc.dram_tensor(n, s, d, kind=k).ap()
    x = dt("x", [NTOK, D])
    W = {n: dt(n, s) for n, s in _W_SPECS}
    out = dt("out", [NTOK, D], "ExternalOutput")
    with ExitStack() as st:
        P = Prog(nc)
        C = Ctx()
        C.nc = nc
        C.XS = dt("XS", [NE * CAP, D], "Internal", BF16)
        C.Y = dt("Y", [NE * CAP, D], "Internal", F32)
        C.OF = dt("OF", [2, NTOK, 2048], "Internal", F32)
        C.ON = dt("ON", [NTOK, 2048], "Internal", BF16)
        XA = dt("XA", [NTOK, D], "Internal")
        XB = dt("XB", [NTOK, D], "Internal")
        Hh = dt("Hh", [NTOK, D], "Internal")
        C.arena = Arena(nc, st, 176 * 1024)
        C.banks = []
        for i in range(8):
            t = st.enter_context(nc.psum_tensor("bank%d" % i, [128, 512], F32))
            C.banks.append(Buf(t[:], [('ps', i)]))
        al = Alloc(C.arena, 0)
        setup_consts(P, C, al)
        C.dyn_start = al.off
        cur = (x, 'x')
        for li in range(depth):
            if li % 2 == 0:
                gdn_stage(P, C, li // 2, cur, (Hh, 'H'), W)
            else:
                attn_stage(P, C, li // 2, cur, (Hh, 'H'), W)
            dst = (out, 'out') if li == depth - 1 else (XB, 'XB')
            moe_stage(P, C, li, cur, (XA, 'XA'), dst, (Hh, 'H'), W)
            cur = dst
        P.emit(st)
    return nc


_LAYER_W = {
    'a': ['a_w_in', 'a_conv_w', 'a_A_log', 'a_dt_bias', 'a_norm_w', 'a_w_out'],
    'b': ['b_w_in', 'b_b_in', 'b_sinks', 'b_w_out', 'b_b_out'],
    'm': ['router_w', 'router_b', 'exp_w_up', 'exp_b_up', 'exp_w_down', 'exp_b_down', 'ln_g', 'ln_b'],
}


def build_layer_program(kind):
    nc = bass.Bass("TRN2", target_bir_lowering=False)
    dt = lambda n, s, k="ExternalInput", d=F32: nc.dram_tensor(n, s, d, kind=k).ap()
    x = dt("x", [NTOK, D])
    out = dt("out", [NTOK, D], "ExternalOutput")
    spec = dict(_W_SPECS)
    names = {'a': _LAYER_W['a'] + _LAYER_W['m'], 'b': _LAYER_W['b'], 'm': _LAYER_W['m']}[kind]
    W = {n: dt(n, [1] + spec[n][1:]) for n in names}
    if kind == 'm':
        Hh = dt("h", [NTOK, D])
    elif kind == 'b':
        Hh = None
    else:
        Hh = dt("Hh", [NTOK, D], "Internal")
    with ExitStack() as st:
        P = Prog(nc)
        C = Ctx()
        C.nc = nc
        if kind in ('a', 'm'):
            C.XS = dt("XS", [NE * CAP, D], "Internal", BF16)
            C.Y = dt("Y", [NE * CAP, D], "Internal", F32)
            XA = dt("XA", [NTOK, D], "Internal")
        if kind == 'a':
            C.OF = dt("OF", [2, NTOK, 2048], "Internal", F32)
            C.ON = dt("ON", [NTOK, 2048], "Internal", BF16)
        C.arena = Arena(nc, st, 176 * 1024)
        C.banks = []
        for i in range(8):
            t = st.enter_context(nc.psum_tensor("bank%d" % i, [128, 512], F32))
            C.banks.append(Buf(t[:], [('ps', i)]))
        al = Alloc(C.arena, 0)
        setup_consts(P, C, al)
        C.dyn_start = al.off
        if kind == 'a':
            gdn_stage(P, C, 0, (x, 'x'), (Hh, 'H'), W)
            moe_stage(P, C, 0, (x, 'x'), (XA, 'XA'), (out, 'out'), (Hh, 'H'), W)
        elif kind == 'b':
            attn_stage(P, C, 0, (x, 'x'), (out, 'out'), W)
        else:
            moe_stage(P, C, 0, (x, 'x'), (XA, 'XA'), (out, 'out'), (Hh, 'H'), W)
        P.emit(st)
    return nc


def kernel(**inputs):
    n = 8
    x = np.ascontiguousarray(np.asarray(inputs['x'], dtype=np.float32))
    xs = x.reshape(n, NTOK, D)
    nc = build_program()
    wmap = {name: np.ascontiguousarray(np.asarray(inputs[name], dtype=np.float32)) for name, _ in _W_SPECS}
    in_maps = []
    for c in range(n):
        m = dict(wmap)
        m['x'] = xs[c]
        in_maps.append(m)
    res = run_bass_kernel_spmd(nc, in_maps, core_ids=list(range(n)))
    outs = [np.asarray(r['out']) for r in res.results]
    return np.stack(outs, 0).reshape(16, SEQ, D).astype(np.float32)
```
